# Optimizing a Trainium2 kernel written in Bass

```python
import jax, jax.numpy as jnp
from jax import lax
import numpy as np

D_MODEL = 4096
BATCH = 4
SEQ = 2048
DEPTH = 2

MIX_WIDTH = D_MODEL
HEAD_DIM = 128
MOBA_HEADS = (3 * MIX_WIDTH // 8) // HEAD_DIM
MOBA_BLOCK = 256
MOBA_TOPK = 3
MOBA_QCHUNK = 16
GLA_DV = 128
GLA_HEADS = (MIX_WIDTH // 4) // GLA_DV
GLA_DK = GLA_DV // 2
GLA_LOWRANK = 16
GLA_TAU = 16.0
GLA_CHUNK = 64
SWA_HEADS = (MIX_WIDTH - MOBA_HEADS * HEAD_DIM - GLA_HEADS * GLA_DV) // HEAD_DIM
SWA_KV_HEADS = 4
SWA_WINDOW = 128
D_FF = 4 * D_MODEL
ALIBI_MAX_EXP = 8.0
EPS = 1e-6

SPLIT_SIZES = (
    MOBA_HEADS * HEAD_DIM, MOBA_HEADS * HEAD_DIM, MOBA_HEADS * HEAD_DIM,
    GLA_HEADS * GLA_DK, GLA_HEADS * GLA_DK, GLA_HEADS * GLA_DV,
    GLA_HEADS * GLA_DV, GLA_LOWRANK,
    SWA_HEADS * HEAD_DIM, SWA_KV_HEADS * HEAD_DIM, SWA_KV_HEADS * HEAD_DIM,
)
D_IN = sum(SPLIT_SIZES)

kernel_name = "hybrid_moba_gla_swa_adaln_block"


def rms_norm(x, gain):
    xf = x.astype(jnp.float32)
    y = xf * lax.rsqrt(jnp.mean(xf * xf, axis=-1, keepdims=True) + EPS)
    return (y * gain.astype(jnp.float32)).astype(x.dtype)


def alibi_slopes(n_heads):
    return jnp.exp2(-ALIBI_MAX_EXP * jnp.arange(1, n_heads + 1, dtype=jnp.float32) / n_heads)


def moba_attention(q, k, v, slopes):
    B, H, S, D = q.shape
    nb = -(-S // MOBA_BLOCK)
    pad = nb * MOBA_BLOCK - S
    kb = jnp.pad(k, ((0, 0), (0, 0), (0, pad), (0, 0))).reshape(B, H, nb, MOBA_BLOCK, D)
    vb = jnp.pad(v, ((0, 0), (0, 0), (0, pad), (0, 0))).reshape(B, H, nb, MOBA_BLOCK, D)
    kmean = jnp.mean(kb.astype(jnp.float32), axis=3)
    pos = jnp.arange(S)
    qblk = pos // MOBA_BLOCK
    gate = jnp.einsum('bhsd,bhnd->bhsn', q.astype(jnp.float32), kmean)
    past = jnp.arange(nb)[None, :] < qblk[:, None]
    gate = jnp.where(past[None, None], gate, -jnp.inf)
    n_sel = min(MOBA_TOPK, nb)
    _, sel = lax.top_k(gate, n_sel)
    sel_valid = sel < qblk[None, None, :, None]
    scale = D ** -0.5
    bi = jnp.arange(B)[:, None, None, None]
    hi = jnp.arange(H)[None, :, None, None]
    offs = jnp.arange(MOBA_BLOCK)
    n_chunks = S // MOBA_QCHUNK

    def chunk(ci):
        t0 = ci * MOBA_QCHUNK
        qc = lax.dynamic_slice_in_dim(q, t0, MOBA_QCHUNK, axis=2)
        selc = lax.dynamic_slice_in_dim(sel, t0, MOBA_QCHUNK, axis=2)
        validc = lax.dynamic_slice_in_dim(sel_valid, t0, MOBA_QCHUNK, axis=2)
        tq = t0 + jnp.arange(MOBA_QCHUNK)
        own = t0 // MOBA_BLOCK
        k_sel = kb[bi, hi, selc]
        v_sel = vb[bi, hi, selc]
        k_own = lax.dynamic_index_in_dim(kb, own, axis=2, keepdims=False)
        v_own = lax.dynamic_index_in_dim(vb, own, axis=2, keepdims=False)
        s_sel = selc[..., None] * MOBA_BLOCK + offs
        s_own = own * MOBA_BLOCK + offs
        d_sel = (tq[None, None, :, None, None] - s_sel).astype(jnp.float32)
        l_sel = (jnp.einsum('bhqd,bhqnkd->bhqnk', qc, k_sel).astype(jnp.float32) * scale
                 - slopes[None, :, None, None, None] * d_sel)
        l_sel = jnp.where(validc[..., None], l_sel, -jnp.inf)
        d_own = (tq[:, None] - s_own[None, :]).astype(jnp.float32)
        l_own = (jnp.einsum('bhqd,bhkd->bhqk', qc, k_own).astype(jnp.float32) * scale
                 - slopes[None, :, None, None] * d_own[None, None])
        l_own = jnp.where((d_own >= 0)[None, None], l_own, -jnp.inf)
        n_k = n_sel * MOBA_BLOCK
        logits = jnp.concatenate([l_sel.reshape(B, H, MOBA_QCHUNK, n_k), l_own], axis=-1)
        p = jax.nn.softmax(logits, axis=-1).astype(v.dtype)
        p_sel = p[..., :n_k].reshape(B, H, MOBA_QCHUNK, n_sel, MOBA_BLOCK)
        p_own = p[..., n_k:]
        return (jnp.einsum('bhqnk,bhqnkd->bhqd', p_sel, v_sel)
                + jnp.einsum('bhqk,bhkd->bhqd', p_own, v_own))

    out = lax.map(chunk, jnp.arange(n_chunks))
    return out.transpose(1, 2, 0, 3, 4).reshape(B, H, S, D)


def gla_mixer(q, k, v, g_out, a_lr, w_a, b_a, out_gain):
    B, S, H, DK = q.shape
    DV = v.shape[-1]
    n = S // GLA_CHUNK
    C = GLA_CHUNK
    log_a = jax.nn.log_sigmoid((a_lr @ w_a + b_a).astype(jnp.float32)) / GLA_TAU

    def to_chunks(t, d):
        return t.astype(jnp.float32).reshape(B, n, C, H, d).transpose(0, 3, 1, 2, 4)

    qc = to_chunks(q, DK) * (DK ** -0.5)
    kc = to_chunks(k, DK)
    vc = to_chunks(v, DV)
    lam = jnp.cumsum(to_chunks(log_a, DK), axis=3)
    lam_last = lam[:, :, :, -1:, :]
    qe = qc * jnp.exp(lam)
    ke = kc * jnp.exp(-lam)
    kd = kc * jnp.exp(lam_last - lam)
    causal = jnp.tril(jnp.ones((C, C), dtype=bool))
    att = jnp.where(causal, jnp.einsum('bhncd,bhnsd->bhncs', qe, ke), 0.0)
    o_intra = jnp.einsum('bhncs,bhnsv->bhncv', att, vc)

    def step(state, inp):
        qe_n, kd_n, v_n, decay_n = inp
        o = jnp.einsum('bhcd,bhdv->bhcv', qe_n, state)
        state = state * decay_n[..., None] + jnp.einsum('bhcd,bhcv->bhdv', kd_n, v_n)
        return state, o

    xs = (jnp.moveaxis(qe, 2, 0), jnp.moveaxis(kd, 2, 0), jnp.moveaxis(vc, 2, 0),
          jnp.moveaxis(jnp.exp(lam_last[:, :, :, 0, :]), 2, 0))
    state0 = jnp.zeros((B, H, DK, DV), jnp.float32)
    _, o_inter = lax.scan(step, state0, xs)
    o = o_intra + jnp.moveaxis(o_inter, 0, 2)
    o = o.transpose(0, 2, 3, 1, 4).reshape(B, S, H, DV)
    o = rms_norm(o, out_gain) * jax.nn.silu(g_out.astype(jnp.float32))
    return o.astype(v.dtype).reshape(B, S, H * DV)


def swa_attention(q, k, v, slopes, sinks):
    B, Hq, S, D = q.shape
    Hkv = k.shape[1]
    G = Hq // Hkv
    W = SWA_WINDOW
    nb = S // W
    qb = q.reshape(B, Hkv, G, nb, W, D)
    kb = k.reshape(B, Hkv, nb, W, D)
    vb = v.reshape(B, Hkv, nb, W, D)
    kprev = jnp.pad(kb, ((0, 0), (0, 0), (1, 0), (0, 0), (0, 0)))[:, :, :-1]
    vprev = jnp.pad(vb, ((0, 0), (0, 0), (1, 0), (0, 0), (0, 0)))[:, :, :-1]
    kw = jnp.concatenate([kprev, kb], axis=3)
    vw = jnp.concatenate([vprev, vb], axis=3)
    i = jnp.arange(W)[:, None]
    j = jnp.arange(2 * W)[None, :]
    dist = W + i - j
    band = (dist >= 0) & (dist < W)
    has_prev = (jnp.arange(nb) > 0)[:, None, None] | (j >= W)[None]
    mask = band[None] & has_prev
    sl = slopes.reshape(Hkv, G)[None, :, :, None, None, None]
    logits = (jnp.einsum('bkgnqd,bknsd->bkgnqs', qb, kw).astype(jnp.float32) * (D ** -0.5)
              - sl * dist.astype(jnp.float32))
    logits = jnp.where(mask, logits, -jnp.inf)
    sink = jnp.broadcast_to(sinks.astype(jnp.float32).reshape(Hkv, G)[None, :, :, None, None, None],
                            (B, Hkv, G, nb, W, 1))
    p = jax.nn.softmax(jnp.concatenate([logits, sink], axis=-1), axis=-1)[..., :2 * W]
    out = jnp.einsum('bkgnqs,bknsd->bkgnqd', p.astype(v.dtype), vw)
    return out.reshape(B, Hq, S, D)


def setup_inputs(seed: int = 0) -> dict:
    key = jax.random.key(seed)
    ks = jax.random.split(key, 18)

    def nrm(k, shape, scale):
        return jax.random.normal(k, shape, jnp.float32) * scale

    def gain(k, shape):
        return 1.0 + 0.1 * jax.random.normal(k, shape, jnp.float32)

    return {
        "x": nrm(ks[0], (BATCH, SEQ, D_MODEL), 1.0),
        "c": nrm(ks[1], (BATCH, D_MODEL), 1.0),
        "w_ada": nrm(ks[2], (DEPTH, D_MODEL, 6 * D_MODEL), 0.5 * D_MODEL ** -0.5),
        "b_ada": nrm(ks[3], (DEPTH, 6 * D_MODEL), 0.02),
        "norm_attn": gain(ks[4], (DEPTH, D_MODEL)),
        "w_in": nrm(ks[5], (DEPTH, D_MODEL, D_IN), D_MODEL ** -0.5),
        "moba_q_norm": gain(ks[6], (DEPTH, HEAD_DIM)),
        "moba_k_norm": gain(ks[7], (DEPTH, HEAD_DIM)),
        "gla_w_a": nrm(ks[8], (DEPTH, GLA_LOWRANK, GLA_HEADS * GLA_DK), GLA_LOWRANK ** -0.5),
        "gla_b_a": nrm(ks[9], (DEPTH, GLA_HEADS * GLA_DK), 0.02),
        "gla_out_norm": gain(ks[10], (DEPTH, GLA_DV)),
        "swa_q_norm": gain(ks[11], (DEPTH, HEAD_DIM)),
        "swa_k_norm": gain(ks[12], (DEPTH, HEAD_DIM)),
        "swa_sinks": nrm(ks[13], (DEPTH, SWA_HEADS), 0.5),
        "w_out": nrm(ks[14], (DEPTH, MIX_WIDTH, D_MODEL), MIX_WIDTH ** -0.5),
        "norm_mlp": gain(ks[15], (DEPTH, D_MODEL)),
        "w_mlp_in": nrm(ks[16], (DEPTH, D_MODEL, D_FF), D_MODEL ** -0.5),
        "w_mlp_out": nrm(ks[17], (DEPTH, D_FF, D_MODEL), D_FF ** -0.5),
    }


def reference(x, c, w_ada, b_ada, norm_attn, w_in, moba_q_norm, moba_k_norm, gla_w_a, gla_b_a,
              gla_out_norm, swa_q_norm, swa_k_norm, swa_sinks, w_out, norm_mlp, w_mlp_in, w_mlp_out):
    B, S, _ = x.shape
    bounds = np.cumsum(SPLIT_SIZES)[:-1].tolist()
    moba_slopes = alibi_slopes(MOBA_HEADS)
    swa_slopes = alibi_slopes(SWA_HEADS)
    cond = jax.nn.silu(c)
    for l in range(DEPTH):
        mod = (cond @ w_ada[l] + b_ada[l])[:, None, :]
        sh_a, sc_a, g_a, sh_m, sc_m, g_m = jnp.split(mod, 6, axis=-1)
        h = rms_norm(x, norm_attn[l]) * (1.0 + sc_a) + sh_a
        proj = h @ w_in[l]
        mq, mk, mv, gq, gk, gv, gg, ga, sq, sk, sv = jnp.split(proj, bounds, axis=-1)
        mq = rms_norm(mq.reshape(B, S, MOBA_HEADS, HEAD_DIM), moba_q_norm[l]).transpose(0, 2, 1, 3)
        mk = rms_norm(mk.reshape(B, S, MOBA_HEADS, HEAD_DIM), moba_k_norm[l]).transpose(0, 2, 1, 3)
        mv = mv.reshape(B, S, MOBA_HEADS, HEAD_DIM).transpose(0, 2, 1, 3)
        y_moba = moba_attention(mq, mk, mv, moba_slopes).transpose(0, 2, 1, 3).reshape(B, S, -1)
        y_gla = gla_mixer(gq.reshape(B, S, GLA_HEADS, GLA_DK), gk.reshape(B, S, GLA_HEADS, GLA_DK),
                          gv.reshape(B, S, GLA_HEADS, GLA_DV), gg.reshape(B, S, GLA_HEADS, GLA_DV),
                          ga, gla_w_a[l], gla_b_a[l], gla_out_norm[l])
        sq = rms_norm(sq.reshape(B, S, SWA_HEADS, HEAD_DIM), swa_q_norm[l]).transpose(0, 2, 1, 3)
        sk = rms_norm(sk.reshape(B, S, SWA_KV_HEADS, HEAD_DIM), swa_k_norm[l]).transpose(0, 2, 1, 3)
        sv = sv.reshape(B, S, SWA_KV_HEADS, HEAD_DIM).transpose(0, 2, 1, 3)
        y_swa = swa_attention(sq, sk, sv, swa_slopes, swa_sinks[l]).transpose(0, 2, 1, 3).reshape(B, S, -1)
        mix = jnp.concatenate([y_moba, y_gla, y_swa], axis=-1)
        x = x + g_a * (mix @ w_out[l])
        h = rms_norm(x, norm_mlp[l]) * (1.0 + sc_m) + sh_m
        x = x + g_m * (jnp.square(jax.nn.relu(h @ w_mlp_in[l])) @ w_mlp_out[l])
    return x
```

```python
import os
import numpy as np
import concourse.bass as bass
import concourse.mybir as mybir
from concourse.bass_utils import run_bass_kernel_spmd
import contextlib

F32 = mybir.dt.float32
BF16 = mybir.dt.bfloat16
AF = mybir.ActivationFunctionType
ALU = mybir.AluOpType
AX = mybir.AxisListType

ENGS = ("pe", "act", "dve", "pool", "sp")
SEM_WRAP = 30000


class Buf:
    __slots__ = ("name", "t", "writers", "readers", "dsem", "dcount")

    def __init__(self, name, t=None):
        self.name = name
        self.t = t
        self.writers = {}
        self.readers = {}
        self.dsem = None
        self.dcount = 0

    def __getitem__(self, idx):
        return self.t[idx]


class Op:
    __slots__ = ("eng", "fn", "deps", "sig", "is_dma", "needs_sig", "dma_sig", "idx")

    def __init__(self, eng, fn, is_dma):
        self.eng = eng
        self.fn = fn
        self.deps = []
        self.sig = None
        self.is_dma = is_dma
        self.needs_sig = False
        self.dma_sig = None
        self.idx = 0


class Prog:
    def __init__(self, nc, same_engine_sync=True):
        self.nc = nc
        self.ops = {e: [] for e in ENGS}
        self.stack = contextlib.ExitStack()
        self.same_engine_sync = same_engine_sync
        self.nsem = 0
        self.n_ops = 0
        self.pending = {e: [] for e in ENGS}
        self.open_dmas = {}
        import os
        self.chain_engs = [x for x in os.environ.get("MK_CHAIN", "act").split(",") if x]
        self.scopes = []
        self.scope_bufs = []
        self.free_dsems = []

    def sem(self, name):
        self.nsem += 1
        return self.stack.enter_context(self.nc.semaphore(name))

    def sbuf(self, name, shape, dt):
        st = self.scopes[-1] if self.scopes else self.stack
        self.nalloc = getattr(self, "nalloc", 0) + 1
        t = st.enter_context(self.nc.sbuf_tensor(f"{name}_{self.nalloc}", list(shape), dt))
        b = Buf(name, t)
        if self.scope_bufs:
            self.scope_bufs[-1].append(b)
        return b

    @contextlib.contextmanager
    def scope(self):
        st = contextlib.ExitStack()
        self.scopes.append(st)
        self.scope_bufs.append([])
        try:
            yield
        finally:
            self.barrier()
            self.scopes.pop()
            for b in self.scope_bufs.pop():
                if b.dsem is not None:
                    self.free_dsems.append((b.dsem, b.dcount))
                    b.dsem = None
            st.close()

    def barrier(self):
        lasts = [self.ops[e][-1] for e in ENGS if self.ops[e]]
        dmas = list(self.open_dmas.values())
        self.open_dmas = {}
        for e in ENGS:
            self.pending[e] = [o for o in lasts if o.eng != e or o.is_dma] + dmas

    def psum(self, name, shape, dt):
        t = self.stack.enter_context(self.nc.psum_tensor(name, list(shape), dt))
        return Buf(name, t)

    def dram(self, name, shape, dt, kind="Internal"):
        t = self.nc.dram_tensor(name, list(shape), dt, kind=kind)
        return Buf(name, t.ap())

    def view(self, name, ap):
        return Buf(name, ap)

    def _track(self, op, reads, writes, key):
        deps = op.deps
        for b in reads:
            for k, w in b.writers.items():
                deps.append(w)
            b.readers[key] = op
        for b in writes:
            if b.readers:
                for k, r in b.readers.items():
                    if r is not op:
                        deps.append(r)
                for k, w in b.writers.items():
                    deps.append(w)
                b.readers = {}
                b.writers = {key: op}
            else:
                for k, w in b.writers.items():
                    if not (op.is_dma and w.is_dma):
                        deps.append(w)
                b.writers[key] = op

    def op(self, eng, fn, reads=(), writes=()):
        o = Op(eng, fn, False)
        if self.pending[eng]:
            o.deps.extend(self.pending[eng])
            self.pending[eng] = []
        self._track(o, reads, writes, eng)
        if eng in self.chain_engs and self.ops[eng]:
            o.deps.append(self.ops[eng][-1])
        self.ops[eng].append(o)
        self.n_ops += 1
        return o

    def dma(self, eng, out_ap, in_ap, sb, reads=(), writes=(), custom=None, **kw):
        o = Op(eng, None, True)
        if sb.dsem is None:
            if self.free_dsems:
                sb.dsem, sb.dcount = self.free_dsems.pop()
            else:
                sb.dsem = self.sem(f"d_{sb.name}_{self.nsem}")
        sb.dcount += 16
        o.dma_sig = (sb.dsem, sb.dcount)
        dsem = sb.dsem

        def fn(e, out_ap=out_ap, in_ap=in_ap, kw=kw, dsem=dsem):
            if custom is not None:
                return custom(e).then_inc(dsem, 16)
            return e.dma_start(out=out_ap, in_=in_ap, **kw).then_inc(dsem, 16)
        o.fn = fn
        if self.pending[eng]:
            o.deps.extend(self.pending[eng])
            self.pending[eng] = []
        self.open_dmas[id(dsem)] = o
        self._track(o, reads, writes, ("dma", id(sb)))
        self.ops[eng].append(o)
        self.n_ops += 1
        return o

    def emit(self, final_waits=()):
        nc = self.nc
        final_waits = list(final_waits) + list(self.pending["sp"])
        for e in ENGS:
            for o in self.ops[e]:
                for d in o.deps:
                    if d.is_dma:
                        continue
                    if d.eng == o.eng and not o.is_dma and (not self.same_engine_sync or d.eng == "pe"):
                        continue
                    d.needs_sig = True
        for o in final_waits:
            if not o.is_dma:
                o.needs_sig = True
        for e in ENGS:
            cur = None
            cnt = 0
            for o in self.ops[e]:
                if o.is_dma or not o.needs_sig:
                    continue
                if cur is None or cnt >= SEM_WRAP:
                    cur = self.sem(f"s_{e}_{self.nsem}")
                    cnt = 0
                cnt += 1
                o.sig = (cur, cnt)
        engmap = {"pe": "tensor", "act": "scalar", "dve": "vector", "pool": "gpsimd", "sp": "sync"}
        with nc.Block() as block:
            for e in ENGS:
                ops = self.ops[e]
                if e == "sp":
                    ops = ops + []
                dec = getattr(block, engmap[e])

                def body(eng, ops=ops, e=e, last=(e == "sp")):
                    waited = {}

                    def wait(sem, val):
                        k = id(sem)
                        if waited.get(k, 0) >= val:
                            return
                        waited[k] = val
                        eng.wait_ge(sem, val)
                    for o in ops:
                        for d in o.deps:
                            if d.is_dma:
                                wait(*d.dma_sig)
                            else:
                                if d.sig is None:
                                    continue
                                wait(*d.sig)
                        ins = o.fn(eng)
                        if o.sig is not None:
                            ins.then_inc(o.sig[0], 1)
                    if last:
                        for o in final_waits:
                            if o.is_dma:
                                wait(*o.dma_sig)
                            else:
                                wait(*o.sig)
                dec(body)
        self.stack.close()


D = 4096
KC = 32
S = 2048
NT = 16
DIN = 10256
DFF = 16384
G = 1024
NGRP = S // G
EPS = 1e-6
DEPTH = 2
SCALE = 128 ** -0.5
BIG = 1.0e9

C_MQ, C_MK, C_MV, C_GQ, C_GK, C_GV, C_GG, C_GA, C_SQ, C_SK, C_SV = (
    0, 1536, 3072, 4608, 5120, 5632, 6656, 7680, 7696, 9232, 9744)

MOBA_SLOPES = [2.0 ** (-8.0 * h / 12) for h in range(1, 13)]
SWA_SLOPES = [2.0 ** (-8.0 * h / 12) for h in range(1, 13)]

CO_ID, CO_ONES, CO_TRI, CO_LTRI, CO_OH2, CO_PAST, CO_GMASK, CO_END = 0, 128, 256, 384, 512, 514, 642, 770
MO_DPL, MO_DBIG, MO_SEL, MO_END = 0, 512, 1408, 2432


def make_consts():
    c = np.zeros((128, CO_END), np.float32)
    c[:, CO_ID:CO_ID + 128] = np.eye(128, dtype=np.float32)
    c[:, CO_ONES:CO_ONES + 128] = 1.0
    s = np.arange(128)[:, None]
    t = np.arange(128)[None, :]
    tri = ((s // 64) == (t // 64)) & (s <= t)
    c[:, CO_TRI:CO_TRI + 128] = tri.astype(np.float32)
    c[:, CO_LTRI:CO_LTRI + 128] = -tri.astype(np.float32) / 16.0
    c[63, CO_OH2] = 1.0
    c[127, CO_OH2 + 1] = 1.0
    tt = np.arange(16)[:, None]
    nn = np.arange(8)[None, :]
    past = ((nn < tt // 2) & (tt >= 8)).astype(np.float32).reshape(1, 128)
    c[:, CO_PAST:CO_PAST + 128] = past
    gm = np.where(nn >= tt // 2, -1.0e30, 0.0).astype(np.float32).reshape(1, 128)
    c[:, CO_GMASK:CO_GMASK + 128] = gm
    m = np.zeros((128, MO_END), np.float32)
    sp = np.arange(128)[:, None].astype(np.float64)
    tp = np.arange(512)[None, :].astype(np.float64)
    m[:, MO_DPL:MO_DPL + 512] = tp - sp
    u = np.arange(896)[None, :].astype(np.float64)
    db = u - 384 - sp
    m[:, MO_DBIG:MO_DBIG + 896] = np.where(db >= 0, db, BIG)
    for n in range(8):
        m[n, MO_SEL + n * 128:MO_SEL + (n + 1) * 128] = 1.0
    sw = np.zeros((128, 12, 2, 128), np.float64)
    s1 = np.arange(128)[:, None]
    t1 = np.arange(128)[None, :]
    dprev = np.where(t1 < s1, 128.0 + t1 - s1, BIG)
    ddiag = np.where(t1 >= s1, (t1 - s1).astype(np.float64), BIG)
    for h in range(12):
        sw[:, h, 0, :] = -SWA_SLOPES[h] / SCALE * dprev
        sw[:, h, 1, :] = -SWA_SLOPES[h] / SCALE * ddiag
    return c, m, sw.reshape(128, 12 * 2 * 128).astype(np.float32)


class MK:
    def __init__(self, debug=False, upto="all", depth=DEPTH, only=None, v2=False):
        self.v2 = v2
        self.only = only
        self.debug = debug
        self.upto = upto
        self.depth = depth
        nc = bass.Bass("TRN2", target_bir_lowering=False)
        self.nc = nc
        P = Prog(nc)
        self.P = P
        self.outs = []
        self.build()

    def din(self, name, shape, dt=F32):
        if self.only is not None and name in ("w_ada", "w_in", "w_out", "w_mlp_in", "w_mlp_out", "x", "b_ada"):
            shape = [2, 2]
        return self.P.dram(name, shape, dt, kind="ExternalInput")

    def dscr(self, name, shape, dt):
        if self.only == "gla" and name in ("gq", "gk", "gv", "sgT", "gaT"):
            return self.P.dram(name, shape, dt, kind="ExternalInput")
        if self.debug:
            self.outs.append(name)
            return self.P.dram(name, shape, dt, kind="ExternalOutput")
        return self.P.dram(name, shape, dt, kind="Internal")

    def wload(self, src_ap, a, b, eng=None):
        P = self.P
        i = self.wi
        self.wi += 1
        st = self.wst[i % len(self.wst)]
        bf = self.wbf[i % len(self.wbf)]
        n = a * b
        stv = st.t[:, 0:n].rearrange("p (a b) -> p a b", b=b)
        bfv = bf.t[:, 0:n].rearrange("p (a b) -> p a b", b=b)
        P.dma("sp", stv, src_ap, st, writes=[st])
        ce = self.cast_engs[i % len(self.cast_engs)]
        if eng is not None:
            ce = eng[i % len(eng)]
        if ce == "act":
            P.op("act", lambda e: e.copy(out=bf.t[:, 0:n], in_=st.t[:, 0:n]), reads=[st], writes=[bf])
        else:
            P.op(ce, lambda e: e.tensor_copy(out=bf.t[:, 0:n], in_=st.t[:, 0:n]), reads=[st], writes=[bf])
        return bf, bfv

    def barrier(self):
        self.P.barrier()

    def build(self):
        P = self.P
        dbg = self.debug
        self.x_in = self.din("x", [S, D])
        self.cT = self.din("cT", [128, KC])
        self.w_ada = self.din("w_ada", [DEPTH, D, 6 * D])
        self.b_ada = self.din("b_ada", [DEPTH, 6 * D])
        self.normT = self.din("normT", [DEPTH, 128, 2 * KC])
        self.w_in = self.din("w_in", [DEPTH, D, DIN])
        self.gains = self.din("gains", [DEPTH, 128, 5])
        self.w_a_aug = self.din("w_a_aug", [DEPTH, 17, 512])
        self.esink_in = self.din("sinks_rep", [DEPTH, 128, 12])
        self.w_out = self.din("w_out", [DEPTH, D, D])
        self.w_mi = self.din("w_mlp_in", [DEPTH, D, DFF])
        self.w_mo = self.din("w_mlp_out", [DEPTH, DFF, D])
        self.consts_d = self.din("consts", [128, CO_END])
        self.mconsts_d = self.din("mconsts", [128, MO_END])
        self.swb_d = self.din("swbias", [128, 12 * 2 * 128])
        self.flags_d = self.din("flags", [128, 128 + 128 + 1 + 12 * 128])
        self.out = P.dram("out", [G if self.v2 else S, D], F32, kind="ExternalOutput")
        self.qmT = self.dscr("qmT", [12 * 128, S], BF16)
        self.kmT = self.dscr("kmT", [12 * 128, S], BF16)
        self.vm = self.dscr("vm", [S, 1536], BF16)
        self.gq = self.dscr("gq", [S, 512], F32)
        self.gk = self.dscr("gk", [S, 512], F32)
        self.gv = self.dscr("gv", [S, 1024], BF16)
        self.sgT = self.dscr("sgT", [8 * 128, S], F32)
        self.gaT = self.dscr("gaT", [16, S], F32)
        self.sqT = self.dscr("sqT", [12 * 128, S], BF16)
        self.skT = self.dscr("skT", [4 * 128, S], BF16)
        self.sv = self.dscr("sv", [S, 512], BF16)
        self.mixT = self.dscr("mixT", [32 * 128, S], BF16)
        self.xa = self.dscr("xa", [S, D], F32)
        self.xb = self.dscr("xb", [S, D], F32)
        if dbg:
            self.modT_d = self.dscr("modT_d", [DEPTH, 128, 6 * KC], F32)
            self.hT_d = self.dscr("hT_d", [KC * 128, G], BF16)
        self.consts = P.sbuf("consts", [128, CO_END], F32)
        self.ones_bf = P.sbuf("ones_bf", [128, 128], BF16)
        self.wst = [P.sbuf(f"wst{i}", [128, 2048], F32) for i in range(3)]
        self.wbf = [P.sbuf(f"wbf{i}", [128, 2048], BF16) for i in range(4)]
        self.wi = 0
        self.cast_engs = ["pool", "pool", "pool", "dve"]
        self.hT = P.sbuf("hT", [128, KC, G], BF16)
        self.modT = [P.sbuf(f"modT{l}", [128, 6, KC], F32) for l in range(DEPTH)]
        self.gsc = [P.sbuf(f"gsc{l}", [128, 2, KC], F32) for l in range(DEPTH)]
        self.gn = [P.sbuf(f"gn{l}", [128, 5], F32) for l in range(DEPTH)]
        self.kmean = P.sbuf("kmean", [128, 12, 8], F32)
        self.ps = [P.psum(f"ps{i}", [128, 512], F32) for i in range(8)]
        c = self.consts
        self.ident = c.t[:, CO_ID:CO_ID + 128]
        self.ones_f = c.t[:, CO_ONES:CO_ONES + 128]
        self.tri = c.t[:, CO_TRI:CO_TRI + 128]
        self.ltri = c.t[:, CO_LTRI:CO_LTRI + 128]
        self.oh2 = c.t[:, CO_OH2:CO_OH2 + 2]
        self.pastm = c.t[:, CO_PAST:CO_PAST + 128]
        self.gmask = c.t[:, CO_GMASK:CO_GMASK + 128]
        self.flags = P.sbuf("flags", [128, 257], F32)
        P.dma("sp", self.flags.t[:], self.flags_d.t[:, 0:257], self.flags, writes=[self.flags])
        self.gmask_p = self.flags.t[:, 0:128]
        self.flagb = self.flags.t[:, 128:256]
        self.fcol = self.flags.t[:, 256:257]

        P.dma("sp", c.t[:], self.consts_d[:], c, writes=[c])
        P.op("dve", lambda e: e.tensor_copy(out=self.ones_bf.t[:], in_=self.ones_f), reads=[c], writes=[self.ones_bf])
        for l in range(DEPTH):
            P.dma("sp", self.gn[l].t[:], self.gains.t[l], self.gn[l], writes=[self.gn[l]])

        if self.only == "gla":
            self.phase_gla(0)
            return self.finish()
        self.phase_mod()
        if self.upto == "mod":
            return self.finish()
        xsrc = self.x_in
        for l in range(self.depth):
            last = (l == self.depth - 1)
            xdst = self.out if last else self.xb
            half = self.v2 and last
            grps = [1] if half else list(range(NGRP))
            self.phase_proj(l, xsrc, a_kv_only=half)
            if self.upto == "proj":
                return self.finish()
            self.phase_moba(l, qgroups=(2, 3) if half else (0, 1, 2, 3))
            self.phase_gla(l, a_state_only=half)
            self.phase_swa(l, tiles=range(8, 16) if half else range(16))
            self.phase_oproj(l, xsrc, self.xa, grps)
            self.phase_mlp(l, self.xa, xdst, grps, dst_off=(G if half else 0))
            xsrc = xdst
        self.finish()

    def finish(self):
        P = self.P
        P.barrier()
        P.emit()

    def phase_mod(self):
        P = self.P
        ps = self.ps
        with P.scope():
            cT = P.sbuf("cT", [128, KC], F32)
            cond = P.sbuf("cond", [128, KC], BF16)
            row = [P.sbuf(f"mrow{i}", [1, 2048], F32) for i in range(2)]
            brow = [P.sbuf(f"brow{i}", [1, 2048], F32) for i in range(2)]
            nT = P.sbuf("nT", [128, 2 * KC], F32)
            P.dma("sp", cT.t[:], self.cT.t[:], cT, writes=[cT])
            P.op("act", lambda e: e.activation(out=cond.t[:], in_=cT.t[:], func=AF.Silu), reads=[cT], writes=[cond])
            for l in range(self.depth):
                modT = self.modT[l]
                for g in range(12):
                    r = row[g % 2]
                    br = brow[g % 2]
                    P.dma("sp", br.t[:], self.b_ada.t[l:l + 1, g * 2048:(g + 1) * 2048], br, writes=[br])
                    for k in range(KC):
                        src = self.w_ada.t[l, k * 128:(k + 1) * 128, g * 2048:(g + 1) * 2048].rearrange("p (a b) -> p a b", b=512)
                        bf, bv = self.wload(src, 4, 512, eng=("dve", "act", "dve", "act", "pool"))
                        for n in range(4):
                            P.op("pe", lambda e, n=n, k=k, bv=bv: e.matmul(ps[n].t[0:1, :], lhsT=cond.t[:, k:k + 1], rhs=bv[:, n, :],
                                                                              start=(k == 0), stop=(k == KC - 1)),
                                 reads=[cond, bf], writes=[ps[n]])
                    for n in range(4):
                        P.op("dve", lambda e, n=n, r=r, br=br: e.tensor_tensor(out=r.t[0:1, n * 512:(n + 1) * 512], in0=ps[n].t[0:1, :],
                                                                                in1=br.t[0:1, n * 512:(n + 1) * 512], op=ALU.add),
                             reads=[ps[n], br], writes=[r])
                    kind, half = g // 2, g % 2
                    for j in range(16):
                        P.op("pe", lambda e, j=j, r=r: e.matmul(ps[4].t[:, j:j + 1], lhsT=r.t[0:1, j * 128:(j + 1) * 128],
                                                                 rhs=self.ones_f[0:1, 0:1], start=True, stop=True),
                             reads=[r, self.consts], writes=[ps[4]])
                    P.op("dve", lambda e, kind=kind, half=half, modT=modT: e.tensor_copy(out=modT.t[:, kind, half * 16:(half + 1) * 16],
                                                                                      in_=ps[4].t[:, 0:16]),
                         reads=[ps[4]], writes=[modT])
                P.dma("sp", nT.t[:], self.normT.t[l], nT, writes=[nT])
                for w, kind in ((0, 1), (1, 4)):
                    P.op("dve", lambda e, w=w, kind=kind, modT=modT, l=l: e.scalar_tensor_tensor(
                        out=self.gsc[l].t[:, w, :], in0=modT.t[:, kind, :], scalar=1.0, in1=nT.t[:, w * KC:(w + 1) * KC],
                        op0=ALU.add, op1=ALU.mult), reads=[modT, nT], writes=[self.gsc[l]])
                if self.debug:
                    P.dma("act", self.modT_d.t[l], modT.t[:].rearrange("p a b -> p (a b)"), modT, reads=[modT])
        P.barrier()

    def norm_group(self, xsrc, grp, gsc_ap, sh_ap):
        P = self.P
        ps = self.ps
        with P.scope():
            xts = [P.sbuf(f"xt{i}", [128, D], F32) for i in range(2)]
            junk = P.sbuf("junk", [128, D], BF16)
            st = P.sbuf("nstat", [128, 3, 8], F32)
            for t in range(G // 128):
                tok0 = grp * G + t * 128
                xt = xts[t % 2]
                P.dma("sp", xt.t[:], xsrc.t[tok0:tok0 + 128, :], xt, writes=[xt])
                P.op("act", lambda e, xt=xt, t=t: e.activation(out=junk.t[:], in_=xt.t[:], func=AF.Square, accum_out=st.t[:, 0, t:t + 1]),
                     reads=[xt], writes=[junk, st])
                P.op("act", lambda e, t=t: e.activation(out=st.t[:, 1, t:t + 1], in_=st.t[:, 0, t:t + 1], func=AF.Sqrt, bias=EPS, scale=1.0 / D),
                     reads=[st], writes=[st])
                P.op("dve", lambda e, t=t: e.reciprocal(out=st.t[:, 2, t:t + 1], in_=st.t[:, 1, t:t + 1]), reads=[st], writes=[st])
                P.op("dve", lambda e, xt=xt, t=t: e.tensor_scalar(out=xt.t[:], in0=xt.t[:], scalar1=st.t[:, 2, t:t + 1], scalar2=None, op0=ALU.mult),
                     reads=[xt, st], writes=[xt])
                for jb in range(8):
                    bank = ps[jb % 2]
                    for q in range(4):
                        j = jb * 4 + q
                        P.op("pe", lambda e, bank=bank, q=q, j=j, xt=xt: e.transpose(bank.t[:, q * 128:(q + 1) * 128], xt.t[:, j * 128:(j + 1) * 128], self.ident),
                             reads=[xt, self.consts], writes=[bank])
                    for q in range(4):
                        j = jb * 4 + q
                        if q % 2 == 0:
                            P.op("act", lambda e, bank=bank, q=q, j=j, t=t: e.activation(
                                out=self.hT.t[:, j, t * 128:(t + 1) * 128], in_=bank.t[:, q * 128:(q + 1) * 128], func=AF.Identity,
                                scale=gsc_ap[:, j:j + 1], bias=sh_ap[:, j:j + 1]), reads=[bank] + self.modbufs, writes=[self.hT])
                        else:
                            P.op("dve", lambda e, bank=bank, q=q, j=j, t=t: e.tensor_scalar(
                                out=self.hT.t[:, j, t * 128:(t + 1) * 128], in0=bank.t[:, q * 128:(q + 1) * 128],
                                scalar1=gsc_ap[:, j:j + 1], scalar2=sh_ap[:, j:j + 1], op0=ALU.mult, op1=ALU.add),
                                reads=[bank] + self.modbufs, writes=[self.hT])

    def fm_job(self, banks, wsrc_fn, nk, ncols, rhs_fn, rhs_bufs):
        P = self.P
        kk_per = 2048 // 128
        k = 0
        while k < nk:
            nkk = min(kk_per, nk - k)
            bf, bv = self.wload(wsrc_fn(k, nkk), nkk, ncols)
            for kk in range(nkk):
                kg = k + kk
                for hf in range(2):
                    P.op("pe", lambda e, hf=hf, kg=kg, kk=kk, bv=bv: e.matmul(banks[hf].t[0:ncols, :], lhsT=bv[:, kk, :], rhs=rhs_fn(kg, hf),
                                                                             start=(kg == 0), stop=(kg == nk - 1)),
                         reads=[bf] + rhs_bufs, writes=[banks[hf]])
            k += nkk

    def phase_proj(self, l, xsrc, a_kv_only=False):
        P = self.P
        ps = self.ps
        self.modbufs = [self.modT[l], self.gsc[l]]
        gn = self.gn[l]
        jobs = []
        for h in range(12):
            jobs.append(("fmn", C_MQ + h * 128, 128, (self.qmT, h, 0, None)))
        for h in range(12):
            jobs.append(("fmn", C_MK + h * 128, 128, (self.kmT, h, 1, h)))
        for h in range(12):
            jobs.append(("tm", C_MV + h * 128, 128, (self.vm, h * 128, BF16)))
        for j in range(4):
            jobs.append(("tm", C_GQ + j * 128, 128, (self.gq, j * 128, F32)))
        for j in range(4):
            jobs.append(("tm", C_GK + j * 128, 128, (self.gk, j * 128, F32)))
        for j in range(8):
            jobs.append(("tm", C_GV + j * 128, 128, (self.gv, j * 128, BF16)))
        for j in range(8):
            jobs.append(("fms", C_GG + j * 128, 128, (self.sgT, j)))
        jobs.append(("fma", C_GA, 16, None))
        for h in range(12):
            jobs.append(("fmn", C_SQ + h * 128, 128, (self.sqT, h, 2, None)))
        for h in range(4):
            jobs.append(("fmn", C_SK + h * 128, 128, (self.skT, h, 3, None)))
        for j in range(4):
            jobs.append(("tm", C_SV + j * 128, 128, (self.sv, j * 128, BF16)))
        if self.upto == "projq":
            jobs = jobs[:2]
        w = self.w_in
        all_jobs = jobs
        kv_cols = [(C_MK, C_GQ), (C_GK, C_GG), (C_GA, C_SQ), (C_SK, DIN)]
        kv_jobs = [j for j in all_jobs if any(a <= j[1] < b for a, b in kv_cols)]
        for grp in range(NGRP):
            jobs = kv_jobs if (a_kv_only and grp == 0) else all_jobs
            self.norm_group(xsrc, grp, self.gsc[l].t[:, 0, :], self.modT[l].t[:, 0, :])
            if self.debug and grp == 0 and l == 0:
                P.barrier()
                P.dma("act", self.hT_d.t[:].rearrange("(j p) t -> p j t", p=128), self.hT.t[:], self.hT, reads=[self.hT])
            t0 = grp * G
            with P.scope():
                sq = [P.sbuf(f"sq{i}", [128, 512], BF16) for i in range(2)]
                sd = [P.sbuf(f"sd{i}", [128, 512], F32) for i in range(2)]
                stg_bf = [P.sbuf(f"stgb{i}", [128, 1024], BF16) for i in range(2)]
                stg_f = [P.sbuf(f"stgf{i}", [128, 1024], F32) for i in range(2)]
                ytf = [P.sbuf(f"ytf{i}", [128, 1024], F32) for i in range(2)]
                pending = None
                for ji, (kind, c0, ncols, meta) in enumerate(jobs):
                    banks = (ps[2 * (ji % 3)], ps[2 * (ji % 3) + 1])

                    def wsrc(k0, nkk, c0=c0, ncols=ncols):
                        return w.t[l, k0 * 128:(k0 + nkk) * 128, c0:c0 + ncols].rearrange("(k p) c -> p k c", p=128)
                    self.fm_job(banks, wsrc, KC, ncols, lambda kg, hf: self.hT.t[:, kg, hf * 512:(hf + 1) * 512], [self.hT])
                    if pending is not None:
                        pending()
                        pending = None
                    sb = stg_bf[ji % 2]
                    sf = stg_f[ji % 2]
                    if kind == "fmn":
                        dst, h, gi, kmh = meta
                        for hf in range(2):
                            P.op("act", lambda e, hf=hf, banks=banks: e.activation(out=sq[hf].t[:], in_=banks[hf].t[:], func=AF.Square),
                                 reads=[banks[hf]], writes=[sq[hf]])

                        def fin(banks=banks, sb=sb, dst=dst, h=h, gi=gi, kmh=kmh, t0=t0, grp=grp):
                            for hf in range(2):
                                P.op("pe", lambda e, hf=hf: e.matmul(ps[6 + hf].t[:], lhsT=self.ones_bf.t[:], rhs=sq[hf].t[:], start=True, stop=True),
                                     reads=[self.ones_bf, sq[hf]], writes=[ps[6 + hf]])
                                P.op("act", lambda e, hf=hf: e.activation(out=sd[hf].t[:], in_=ps[6 + hf].t[:], func=AF.Sqrt, bias=EPS, scale=1.0 / 128),
                                     reads=[ps[6 + hf]], writes=[sd[hf]])
                                P.op("dve", lambda e, hf=hf: e.reciprocal(out=sd[hf].t[:], in_=sd[hf].t[:]), reads=[sd[hf]], writes=[sd[hf]])
                                P.op("dve", lambda e, hf=hf: e.scalar_tensor_tensor(out=sb.t[:, hf * 512:(hf + 1) * 512], in0=banks[hf].t[:],
                                                                                   scalar=gn.t[:, gi:gi + 1], in1=sd[hf].t[:], op0=ALU.mult, op1=ALU.mult),
                                     reads=[banks[hf], sd[hf], gn], writes=[sb])
                            if kmh is not None:
                                P.op("dve", lambda e: e.tensor_reduce(out=self.kmean.t[:, kmh, grp * 4:(grp + 1) * 4],
                                                                      in_=sb.t[:].rearrange("p (n k) -> p n k", k=256), axis=AX.X, op=ALU.add),
                                     reads=[sb], writes=[self.kmean])
                            P.dma("act", dst.t[h * 128:(h + 1) * 128, t0:t0 + G], sb.t[:], sb, reads=[sb])
                        pending = fin
                    elif kind == "fms":
                        dst, j = meta
                        for hf in range(2):
                            P.op("act", lambda e, hf=hf, banks=banks, sf=sf: e.activation(out=sf.t[:, hf * 512:(hf + 1) * 512], in_=banks[hf].t[:], func=AF.Silu),
                                 reads=[banks[hf]], writes=[sf])
                        P.dma("act", dst.t[j * 128:(j + 1) * 128, t0:t0 + G], sf.t[:], sf, reads=[sf])
                    elif kind == "fma":
                        for hf in range(2):
                            P.op("dve", lambda e, hf=hf, banks=banks, sf=sf: e.tensor_copy(out=sf.t[0:16, hf * 512:(hf + 1) * 512], in_=banks[hf].t[0:16, :]),
                                 reads=[banks[hf]], writes=[sf])
                        P.dma("act", self.gaT.t[:, t0:t0 + G], sf.t[0:16, :], sf, reads=[sf])
                    elif kind == "tm":
                        dst, dc0, dt = meta
                        stg = sb if dt == BF16 else sf
                        yt = ytf[ji % 2]
                        for hf in range(2):
                            if hf == 0:
                                P.op("act", lambda e, hf=hf, banks=banks, yt=yt: e.copy(out=yt.t[:, hf * 512:(hf + 1) * 512], in_=banks[hf].t[:]),
                                     reads=[banks[hf]], writes=[yt])
                            else:
                                P.op("dve", lambda e, hf=hf, banks=banks, yt=yt: e.tensor_copy(out=yt.t[:, hf * 512:(hf + 1) * 512], in_=banks[hf].t[:]),
                                     reads=[banks[hf]], writes=[yt])
                        for t in range(8):
                            P.op("pe", lambda e, t=t, yt=yt: e.transpose(ps[6 + t // 4].t[:, (t % 4) * 128:(t % 4 + 1) * 128], yt.t[:, t * 128:(t + 1) * 128], self.ident),
                                 reads=[yt, self.consts], writes=[ps[6 + t // 4]])
                        for b2 in range(2):
                            if b2 == 0:
                                P.op("act", lambda e, b2=b2, stg=stg: e.copy(out=stg.t[:, b2 * 512:(b2 + 1) * 512], in_=ps[6 + b2].t[:]),
                                     reads=[ps[6 + b2]], writes=[stg])
                            else:
                                P.op("dve", lambda e, b2=b2, stg=stg: e.tensor_copy(out=stg.t[:, b2 * 512:(b2 + 1) * 512], in_=ps[6 + b2].t[:]),
                                     reads=[ps[6 + b2]], writes=[stg])
                        P.dma("act", dst.t[t0:t0 + G, dc0:dc0 + 128].rearrange("(t p) c -> p t c", p=128),
                              stg.t[:].rearrange("p (t c) -> p t c", c=128), stg, reads=[stg])
                if pending is not None:
                    pending()
            P.barrier()

    def phase_moba(self, l, qgroups=(0, 1, 2, 3)):
        P = self.P
        ps = self.ps
        with P.scope():
            mc = P.sbuf("mconst", [128, MO_END], F32)
            P.dma("sp", mc.t[:], self.mconsts_d.t[:], mc, writes=[mc])
            sel_bf = P.sbuf("sel_bf", [8, 1024], BF16)
            P.op("dve", lambda e: e.tensor_copy(out=sel_bf.t[:], in_=mc.t[0:8, MO_SEL:MO_SEL + 1024]), reads=[mc], writes=[sel_bf])
            kmean_bf = P.sbuf("kmean_bf", [128, 12, 8], BF16)
            P.op("dve", lambda e: e.tensor_scalar(out=kmean_bf.t[:], in0=self.kmean.t[:], scalar1=1.0 / 256, scalar2=None, op0=ALU.mult),
                 reads=[self.kmean], writes=[kmean_bf])
            qT = [P.sbuf(f"mq{i}", [128, S], BF16) for i in range(2)]
            kT = [P.sbuf(f"mk{i}", [128, S], BF16) for i in range(2)]
            vt = [P.sbuf(f"mv{i}", [128, 16, 128], BF16) for i in range(2)]
            gate = P.sbuf("gate", [128, 128], F32)
            top8 = P.sbuf("top8", [128, 8, 8], F32)
            mb = P.sbuf("mbias", [128, 128], F32)
            maskT = P.sbuf("maskT", [8, 1024], BF16)
            pTs = [P.sbuf(f"pT{i}", [128, 512], BF16) for i in range(4)]
            tmps = [P.sbuf(f"mtmp{i}", [128, 512], F32) for i in range(4)]
            rec = P.sbuf("mrec", [128, 512], F32)
            osts = [P.sbuf(f"most{i}", [128, 512], BF16) for i in range(2)]
            P.op("dve", lambda e: e.memset(mb.t[:], 0.0), writes=[mb])
            it = 0
            for h in range(12):
                q, k, v = qT[h % 2], kT[h % 2], vt[h % 2]
                P.dma("sp", q.t[:], self.qmT.t[h * 128:(h + 1) * 128, :], q, writes=[q])
                P.dma("sp", k.t[:], self.kmT.t[h * 128:(h + 1) * 128, :], k, writes=[k])
                P.dma("sp", v.t[:], self.vm.t[:, h * 128:(h + 1) * 128].rearrange("(t p) c -> p t c", p=128), v, writes=[v])
                for t in range(16):
                    P.op("pe", lambda e, t=t, q=q, h=h: e.matmul(ps[7].t[:, t * 8:(t + 1) * 8], lhsT=q.t[:, t * 128:(t + 1) * 128],
                                                                 rhs=kmean_bf.t[:, h, :], start=True, stop=True),
                         reads=[q, kmean_bf], writes=[ps[7]])
                P.op("dve", lambda e: e.tensor_tensor(out=gate.t[:], in0=ps[7].t[:, 0:128], in1=self.gmask_p, op=ALU.add),
                     reads=[ps[7], self.flags], writes=[gate])
                for t in range(8, 16):
                    P.op("dve", lambda e, t=t: e.max(out=top8.t[:, t - 8, :], in_=gate.t[:, t * 8:(t + 1) * 8]), reads=[gate], writes=[top8])
                    P.op("dve", lambda e, t=t: e.tensor_scalar(out=mb.t[:, t * 8:(t + 1) * 8], in0=gate.t[:, t * 8:(t + 1) * 8],
                                                              scalar1=top8.t[:, t - 8, 2:3], scalar2=-30000.0, op0=ALU.is_lt, op1=ALU.mult),
                         reads=[gate, top8], writes=[mb])
                P.op("dve", lambda e: e.tensor_tensor(out=mb.t[:], in0=mb.t[:], in1=self.pastm, op=ALU.mult), reads=[mb, self.consts], writes=[mb])
                P.op("dve", lambda e: e.tensor_tensor(out=mb.t[:], in0=mb.t[:], in1=self.flagb, op=ALU.add), reads=[mb, self.flags], writes=[mb])
                for r in range(2):
                    for qq in range(4):
                        t = 8 + r * 4 + qq
                        P.op("pe", lambda e, t=t, qq=qq: e.transpose(ps[6].t[0:8, qq * 128:(qq + 1) * 128], mb.t[:, t * 8:(t + 1) * 8], self.ident),
                             reads=[mb, self.consts], writes=[ps[6]])
                    P.op("act", lambda e, r=r: e.copy(out=maskT.t[:, r * 512:(r + 1) * 512], in_=ps[6].t[0:8, :]), reads=[ps[6]], writes=[maskT])
                slope = MOBA_SLOPES[h]
                pairs = [(g, kt) for g in qgroups for kt in range(4 * g + 4)]
                LOOK = 3
                ctx = {}

                def emit_score(idx, h=h, q=q, k=k, slope=slope, ctx=ctx):
                    g, kt = pairs[idx]
                    sb = ps[2 + (idx % 4)]
                    tmp = tmps[idx % 4]
                    pT = pTs[idx % 4]
                    ctx[idx] = pT
                    n = kt // 2
                    need_mask = (g >= 2) and (n <= 2 * g)
                    P.op("pe", lambda e, sb=sb, kt=kt, g=g, k=k, q=q, need_mask=need_mask: e.matmul(
                        sb.t[:], lhsT=k.t[:, kt * 128:(kt + 1) * 128], rhs=q.t[:, g * 512:(g + 1) * 512], start=True, stop=not need_mask),
                        reads=[k, q], writes=[sb])
                    if need_mask:
                        P.op("pe", lambda e, sb=sb, n=n, g=g: e.matmul(sb.t[:], lhsT=sel_bf.t[0:8, n * 128:(n + 1) * 128],
                                                                       rhs=maskT.t[0:8, (g - 2) * 512:(g - 1) * 512], start=False, stop=True),
                             reads=[sel_bf, maskT], writes=[sb])
                    j = kt - 4 * g
                    if j >= 0:
                        c0 = MO_DBIG + 384 - 128 * j
                        delta = 0.0
                    else:
                        c0 = MO_DPL
                        delta = float(512 * g - 128 * kt)
                    P.op("dve", lambda e, tmp=tmp, sb=sb, c0=c0, slope=slope: e.scalar_tensor_tensor(
                        out=tmp.t[:], in0=mc.t[:, c0:c0 + 512], scalar=-slope / SCALE, in1=sb.t[:], op0=ALU.mult, op1=ALU.add),
                        reads=[mc, sb], writes=[tmp])
                    P.op("act", lambda e, tmp=tmp, pT=pT, slope=slope, delta=delta: e.activation(
                        out=pT.t[:], in_=tmp.t[:], func=AF.Exp, scale=SCALE, bias=-slope * delta), reads=[tmp], writes=[pT])

                def emit_pv(idx, h=h, v=v, ctx=ctx):
                    g, kt = pairs[idx]
                    nkt = 4 * g + 4
                    pT = ctx.pop(idx)
                    oT, den = ps[0], ps[1]
                    P.op("pe", lambda e, kt=kt, pT=pT, nkt=nkt: e.matmul(oT.t[:], lhsT=v.t[:, kt, :], rhs=pT.t[:],
                                                                        start=(kt == 0), stop=(kt == nkt - 1)),
                         reads=[v, pT], writes=[oT])
                    P.op("pe", lambda e, kt=kt, pT=pT, nkt=nkt: e.matmul(den.t[:], lhsT=self.ones_bf.t[:], rhs=pT.t[:],
                                                                        start=(kt == 0), stop=(kt == nkt - 1)),
                         reads=[self.ones_bf, pT], writes=[den])
                    if kt == nkt - 1:
                        ost = osts[g % 2]
                        P.op("dve", lambda e: e.reciprocal(out=rec.t[:], in_=den.t[:]), reads=[den], writes=[rec])
                        P.op("dve", lambda e, ost=ost: e.tensor_tensor(out=ost.t[:], in0=oT.t[:], in1=rec.t[:], op=ALU.mult),
                             reads=[oT, rec], writes=[ost])
                        P.dma("act", self.mixT.t[h * 128:(h + 1) * 128, g * 512:(g + 1) * 512], ost.t[:], ost, reads=[ost])

                for idx in range(len(pairs) + LOOK):
                    if idx < len(pairs):
                        emit_score(idx)
                    if idx >= LOOK:
                        emit_pv(idx - LOOK)

    def phase_swa(self, l, tiles=range(16)):
        P = self.P
        ps = self.ps
        with P.scope():
            swb = P.sbuf("swb", [128, 12 * 2 * 128], F32)
            P.dma("sp", swb.t[:], self.swb_d.t[:], swb, writes=[swb])
            swv = swb.t[:].rearrange("p (h w q) -> p h w q", w=2, q=128)
            swb8t = P.sbuf("swb8", [128, 12 * 128], F32)
            P.dma("sp", swb8t.t[:], self.flags_d.t[:, 257:257 + 12 * 128], swb8t, writes=[swb8t])
            swb8 = swb8t.t[:].rearrange("p (h q) -> p h q", q=128)
            esk = P.sbuf("esk", [128, 12], F32)
            P.dma("sp", esk.t[:], self.esink_in.t[l], esk, writes=[esk])
            P.op("act", lambda e: e.activation(out=esk.t[:], in_=esk.t[:], func=AF.Exp), reads=[esk], writes=[esk])
            qTs = [P.sbuf(f"sq{i}", [128, 3, S], BF16) for i in range(2)]
            kTs = [P.sbuf(f"sk{i}", [128, S], BF16) for i in range(2)]
            vts = [P.sbuf(f"sv{i}", [128, 16, 128], BF16) for i in range(2)]
            msts = [P.sbuf(f"smst{i}", [128, 3, S], BF16) for i in range(2)]
            tmps = [P.sbuf(f"stmp{i}", [128, 384], F32) for i in range(4)]
            pTs = [P.sbuf(f"spT{i}", [128, 384], BF16) for i in range(4)]
            rec = P.sbuf("srec", [128, 384], F32)
            it = 0
            for kv in range(4):
                q, k, v, mst = qTs[kv % 2], kTs[kv % 2], vts[kv % 2], msts[kv % 2]
                P.dma("sp", q.t[:], self.sqT.t[3 * kv * 128:(3 * kv + 3) * 128, :].rearrange("(g p) t -> p g t", p=128), q, writes=[q])
                P.dma("sp", k.t[:], self.skT.t[kv * 128:(kv + 1) * 128, :], k, writes=[k])
                P.dma("sp", v.t[:], self.sv.t[:, kv * 128:(kv + 1) * 128].rearrange("(t p) c -> p t c", p=128), v, writes=[v])
                parts = []
                for i in tiles:
                    pl = ([(i - 1, 0)] if i > 0 else []) + [(i, 1)]
                    for pi, (kt, which) in enumerate(pl):
                        parts.append((i, kt, which, pi == 0, pi == len(pl) - 1))
                LOOK = 2
                ctx = {}

                def emit_score(idx, q=q, k=k, kv=kv, ctx=ctx):
                    i, kt, which, first, lastp = parts[idx]
                    sb = ps[idx % 4]
                    tmp = tmps[idx % 4]
                    pT = pTs[idx % 4]
                    ctx[idx] = pT
                    qi = q.t[:, :, i * 128:(i + 1) * 128]
                    P.op("pe", lambda e, sb=sb, kt=kt, qi=qi, k=k: e.matmul(sb.t[:, 0:384], lhsT=k.t[:, kt * 128:(kt + 1) * 128], rhs=qi,
                                                                            start=True, stop=True), reads=[k, q], writes=[sb])
                    bias_ap = swb8[:, 3 * kv:3 * kv + 3, :] if (i == 8 and which == 0) else swv[:, 3 * kv:3 * kv + 3, which, :]
                    P.op("dve", lambda e, sb=sb, tmp=tmp, bias_ap=bias_ap: e.tensor_tensor(
                        out=tmp.t[:].rearrange("p (g q) -> p g q", q=128), in0=sb.t[:, 0:384].rearrange("p (g q) -> p g q", q=128),
                        in1=bias_ap, op=ALU.add), reads=[sb, swb, swb8t], writes=[tmp])
                    P.op("act", lambda e, tmp=tmp, pT=pT: e.activation(out=pT.t[:], in_=tmp.t[:], func=AF.Exp, scale=SCALE),
                         reads=[tmp], writes=[pT])

                def emit_pv(idx, v=v, kv=kv, mst=mst, ctx=ctx):
                    i, kt, which, first, lastp = parts[idx]
                    pT = ctx.pop(idx)
                    oT = ps[4 + (i % 2)]
                    den = ps[6 + (i % 2)]
                    P.op("pe", lambda e, oT=oT, kt=kt, pT=pT, first=first, lastp=lastp: e.matmul(
                        oT.t[:, 0:384], lhsT=v.t[:, kt, :], rhs=pT.t[:], start=first, stop=lastp), reads=[v, pT], writes=[oT])
                    P.op("pe", lambda e, den=den, pT=pT, first=first, lastp=lastp: e.matmul(
                        den.t[:, 0:384], lhsT=self.ones_bf.t[:], rhs=pT.t[:], start=first, stop=lastp), reads=[self.ones_bf, pT], writes=[den])
                    if lastp:
                        for g in range(3):
                            P.op("dve", lambda e, g=g, den=den: e.tensor_scalar(
                                out=rec.t[:, g * 128:(g + 1) * 128], in0=den.t[:, g * 128:(g + 1) * 128],
                                scalar1=esk.t[:, 3 * kv + g:3 * kv + g + 1], scalar2=None, op0=ALU.add), reads=[den, esk], writes=[rec])
                        P.op("dve", lambda e: e.reciprocal(out=rec.t[:], in_=rec.t[:]), reads=[rec], writes=[rec])
                        P.op("dve", lambda e, i=i, oT=oT: e.tensor_tensor(
                            out=mst.t[:, :, i * 128:(i + 1) * 128], in0=oT.t[:, 0:384].rearrange("p (g q) -> p g q", q=128),
                            in1=rec.t[:].rearrange("p (g q) -> p g q", q=128), op=ALU.mult), reads=[oT, rec], writes=[mst])

                for idx in range(len(parts) + LOOK):
                    if idx < len(parts):
                        emit_score(idx)
                    if idx >= LOOK:
                        emit_pv(idx - LOOK)
                r0 = (20 + 3 * kv) * 128
                c_lo, c_hi = tiles[0] * 128, (tiles[-1] + 1) * 128
                P.dma("act", self.mixT.t[r0:r0 + 384, c_lo:c_hi].rearrange("(g p) t -> p g t", p=128), mst.t[:, :, c_lo:c_hi], mst, reads=[mst])

    def phase_gla(self, l, a_state_only=False):
        P = self.P
        ps = self.ps
        gn = self.gn[l]
        with P.scope():
            waug = P.sbuf("waug", [17, 512], F32)
            P.dma("sp", waug.t[:], self.w_a_aug.t[l], waug, writes=[waug])
            ga1 = P.sbuf("ga1", [32, S], F32)
            P.op("dve", lambda e: e.memset(ga1.t[:], 1.0), writes=[ga1])
            P.dma("sp", ga1.t[0:16, :], self.gaT.t[:, :], ga1, writes=[ga1])
            state = [P.sbuf(f"gst{h}", [64, 128], F32) for h in range(8)]
            state_bf = [P.sbuf(f"gsb{h}", [64, 128], BF16) for h in range(8)]
            for h in range(8):
                P.op("dve", lambda e, h=h: e.memset(state[h].t[:], 0.0), writes=[state[h]])
                P.op("dve", lambda e, h=h: e.memset(state_bf[h].t[:], 0.0), writes=[state_bf[h]])

            def mk(name, shape, dt, n=2):
                return [P.sbuf(f"{name}{i}", shape, dt) for i in range(n)]
            q_sb = mk("gqs", [128, 512], F32)
            k_sb = mk("gks", [128, 512], F32)
            v_sb = mk("gvs", [128, 1024], BF16)
            sg_sb = mk("gsg", [128, 8, 128], F32)
            e_sb = mk("ges", [128, 512], F32, 1)[0]
            sp_sb = mk("gsp", [128, 512], F32, 1)[0]
            lam_sb = mk("glam", [128, 512], F32)
            elam = mk("gel", [128, 512], F32, 1)[0]
            enlam = mk("gen", [128, 512], F32, 1)[0]
            qe = mk("gqe", [128, 512], F32)
            ke = mk("gke", [128, 512], F32)
            ke_bf = mk("gkeb", [128, 512], BF16)
            E_sb = mk("gE", [64, 16], F32)
            qkT = mk("gqkT", [64, 256], BF16)
            attT = mk("gatt", [128, 128], BF16)
            tmpst = mk("gtmp", [64, 128], F32)
            osq = mk("gosq", [128, 128], BF16)
            ord_ = mk("gord", [128, 128], F32)
            ytmp = mk("gyt", [128, 128], F32)
            yst = mk("gyst", [128, 8, 128], BF16)
            hh = 0
            import os
            GSTOP = int(os.environ.get("MK_GLA_STOP", "9"))
            for t in range(int(os.environ.get("MK_GLA_TILES", "16"))):
                tb = t % 2
                r0 = t * 128
                so = a_state_only and t < 8
                if t == 8:
                    for h in range(8):
                        P.op("dve", lambda e, h=h: e.tensor_scalar(out=state[h].t[:], in0=state[h].t[:], scalar1=self.fcol[0:64, :], scalar2=None, op0=ALU.mult),
                             reads=[state[h], self.flags], writes=[state[h]])
                        P.op("dve", lambda e, h=h: e.tensor_scalar(out=state_bf[h].t[:], in0=state_bf[h].t[:], scalar1=self.fcol[0:64, :], scalar2=None, op0=ALU.mult),
                             reads=[state_bf[h], self.flags], writes=[state_bf[h]])
                if not so:
                    P.dma("sp", q_sb[tb].t[:], self.gq.t[r0:r0 + 128, :], q_sb[tb], writes=[q_sb[tb]])
                    P.dma("sp", sg_sb[tb].t[:], self.sgT.t[:, r0:r0 + 128].rearrange("(h p) t -> p h t", p=128), sg_sb[tb], writes=[sg_sb[tb]])
                P.dma("sp", k_sb[tb].t[:], self.gk.t[r0:r0 + 128, :], k_sb[tb], writes=[k_sb[tb]])
                P.dma("sp", v_sb[tb].t[:], self.gv.t[r0:r0 + 128, :], v_sb[tb], writes=[v_sb[tb]])
                P.op("pe", lambda e, r0=r0: e.matmul(ps[0].t[:], lhsT=ga1.t[0:17, r0:r0 + 128], rhs=waug.t[0:17, :], start=True, stop=True),
                     reads=[ga1, waug], writes=[ps[0]])
                P.op("act", lambda e: e.activation(out=e_sb.t[:], in_=ps[0].t[:], func=AF.Exp, scale=-1.0), reads=[ps[0]], writes=[e_sb])
                P.op("act", lambda e: e.activation(out=sp_sb.t[:], in_=e_sb.t[:], func=AF.Ln, bias=1.0, scale=1.0), reads=[e_sb], writes=[sp_sb])
                if GSTOP <= 0:
                    continue
                SUB = int(os.environ.get("MK_GLA_SUB", "99"))
                lam = lam_sb[tb]
                E = E_sb[tb]
                if SUB >= 1:
                    P.op("pe", lambda e: e.matmul(ps[1].t[:], lhsT=self.ltri, rhs=sp_sb.t[:], start=True, stop=True),
                         reads=[self.consts, sp_sb], writes=[ps[1]])
                if SUB >= 2:
                    P.op("dve", lambda e, lam=lam: e.tensor_copy(out=lam.t[:], in_=ps[1].t[:]), reads=[ps[1]], writes=[lam])
                if SUB >= 3:
                    FV = os.environ.get("MK_GLA_F", "expsb")
                    if FV == "exp":
                        P.op("act", lambda e: e.activation(out=elam.t[:], in_=ps[1].t[:], func=AF.Exp), reads=[ps[1]], writes=[elam])
                    elif FV == "exps":
                        P.op("act", lambda e: e.activation(out=elam.t[:], in_=ps[1].t[:], func=AF.Exp, scale=1.0, bias=0.0), reads=[ps[1]], writes=[elam])
                    elif FV == "copy":
                        P.op("act", lambda e: e.activation(out=elam.t[:], in_=ps[1].t[:], func=AF.Identity), reads=[ps[1]], writes=[elam])
                    elif FV == "toesb":
                        P.op("act", lambda e: e.activation(out=e_sb.t[:], in_=lam.t[:], func=AF.Exp), reads=[lam], writes=[e_sb])
                    elif FV == "dvew":
                        P.op("dve", lambda e: e.memset(elam.t[:], 1.0), writes=[elam])
                    elif FV == "expsb":
                        P.op("act", lambda e, lam=lam: e.activation(out=elam.t[:], in_=lam.t[:], func=AF.Exp), reads=[lam], writes=[elam])
                if SUB >= 4:
                    P.op("act", lambda e, lam=lam: e.activation(out=enlam.t[:], in_=lam.t[:], func=AF.Exp, scale=-1.0), reads=[lam], writes=[enlam])
                if SUB >= 5 and not so:
                    P.op("dve", lambda e, tb=tb: e.scalar_tensor_tensor(out=qe[tb].t[:], in0=q_sb[tb].t[:], scalar=0.125, in1=elam.t[:],
                                                                         op0=ALU.mult, op1=ALU.mult), reads=[q_sb[tb], elam], writes=[qe[tb]])
                if SUB >= 6:
                    P.op("dve", lambda e, tb=tb: e.tensor_tensor(out=ke[tb].t[:], in0=k_sb[tb].t[:], in1=enlam.t[:], op=ALU.mult),
                         reads=[k_sb[tb], enlam], writes=[ke[tb]])
                if SUB >= 7:
                    P.op("pool", lambda e, tb=tb: e.tensor_copy(out=ke_bf[tb].t[:], in_=ke[tb].t[:]), reads=[ke[tb]], writes=[ke_bf[tb]])
                if SUB >= 8:
                    for h in range(8):
                        P.op("pe", lambda e, h=h, lam=lam: e.matmul(ps[2].t[0:64, 2 * h:2 * h + 2], lhsT=lam.t[:, h * 64:(h + 1) * 64], rhs=self.oh2,
                                                                    start=True, stop=True), reads=[lam, self.consts], writes=[ps[2]])
                if SUB >= 9:
                    P.op("act", lambda e, E=E: e.activation(out=E.t[:], in_=ps[2].t[0:64, 0:16], func=AF.Exp), reads=[ps[2]], writes=[E])
                if GSTOP <= 1:
                    continue
                v = v_sb[tb]

                def front(h, hb, tb=tb):
                    qk = qkT[hb]
                    P.op("pe", lambda e, h=h, tb=tb: e.transpose(ps[3].t[0:64, 0:128], qe[tb].t[:, h * 64:(h + 1) * 64], self.ident),
                         reads=[qe[tb], self.consts], writes=[ps[3]])
                    P.op("pe", lambda e, h=h, tb=tb: e.transpose(ps[3].t[0:64, 128:256], ke[tb].t[:, h * 64:(h + 1) * 64], self.ident),
                         reads=[ke[tb], self.consts], writes=[ps[3]])
                    P.op("act", lambda e, qk=qk: e.copy(out=qk.t[:], in_=ps[3].t[0:64, 0:256]), reads=[ps[3]], writes=[qk])
                    P.op("pe", lambda e, qk=qk: e.matmul(ps[4].t[:, 0:128], lhsT=qk.t[:, 128:256], rhs=qk.t[:, 0:128], start=True, stop=True),
                         reads=[qk], writes=[ps[4]])
                    at = attT[hb]
                    P.op("dve", lambda e, at=at: e.tensor_tensor(out=at.t[:], in0=ps[4].t[:, 0:128], in1=self.tri, op=ALU.mult),
                         reads=[ps[4], self.consts], writes=[at])
                    return qk, at

                def half_step(h, hb, qk, half, tb=tb, v=v, E=E):
                    bank = ps[5 + hb]
                    c0 = half * 64
                    if qk is not None:
                        P.op("pe", lambda e, bank=bank, h=h, qk=qk, c0=c0, half=half: e.matmul(
                            bank.t[:, c0:c0 + 64], lhsT=state_bf[h].t[:], rhs=qk.t[:, c0:c0 + 64], start=False, stop=(half == 1)),
                            reads=[state_bf[h], qk], writes=[bank])
                    P.op("pe", lambda e, h=h, tb=tb, v=v, c0=c0: e.matmul(
                        ps[7].t[0:64, 0:128], lhsT=ke_bf[tb].t[c0:c0 + 64, h * 64:(h + 1) * 64], rhs=v.t[c0:c0 + 64, h * 128:(h + 1) * 128],
                        start=True, stop=True), reads=[ke_bf[tb], v], writes=[ps[7]])
                    tm = tmpst[half]
                    P.op("dve", lambda e, h=h, tm=tm: e.tensor_tensor(out=tm.t[:], in0=ps[7].t[0:64, 0:128], in1=state[h].t[:], op=ALU.add),
                         reads=[ps[7], state[h]], writes=[tm])
                    P.op("dve", lambda e, h=h, tm=tm, E=E, half=half: e.tensor_scalar(
                        out=state_bf[h].t[:], in0=tm.t[:], scalar1=E.t[:, 2 * h + half:2 * h + half + 1], scalar2=None, op0=ALU.mult),
                        reads=[tm, E], writes=[state_bf[h]])
                    P.op("dve", lambda e, h=h, tm=tm, E=E, half=half: e.tensor_scalar(
                        out=state[h].t[:], in0=tm.t[:], scalar1=E.t[:, 2 * h + half:2 * h + half + 1], scalar2=None, op0=ALU.mult),
                        reads=[tm, E], writes=[state[h]])

                def mid(h, hb, qk, at, v=v):
                    bank = ps[5 + hb]
                    P.op("pe", lambda e, bank=bank, v=v, h=h, at=at: e.matmul(bank.t[:, 0:128], lhsT=v.t[:, h * 128:(h + 1) * 128], rhs=at.t[:],
                                                                             start=True, stop=False), reads=[v, at], writes=[bank])
                    half_step(h, hb, qk, 0)

                def back(h, hb, qk, tb=tb):
                    bank = ps[5 + hb]
                    half_step(h, hb, qk, 1)
                    P.op("act", lambda e, bank=bank, hb=hb: e.activation(out=osq[hb].t[:], in_=bank.t[:, 0:128], func=AF.Square),
                         reads=[bank], writes=[osq[hb]])
                    P.op("pe", lambda e, hb=hb: e.matmul(ps[2].t[:, 256:384], lhsT=self.ones_bf.t[:], rhs=osq[hb].t[:], start=True, stop=True),
                         reads=[self.ones_bf, osq[hb]], writes=[ps[2]])
                    P.op("act", lambda e, hb=hb: e.activation(out=ord_[hb].t[:], in_=ps[2].t[:, 256:384], func=AF.Sqrt, bias=EPS, scale=1.0 / 128),
                         reads=[ps[2]], writes=[ord_[hb]])
                    P.op("dve", lambda e, hb=hb: e.reciprocal(out=ord_[hb].t[:], in_=ord_[hb].t[:]), reads=[ord_[hb]], writes=[ord_[hb]])
                    P.op("dve", lambda e, hb=hb, bank=bank: e.scalar_tensor_tensor(out=ytmp[hb].t[:], in0=bank.t[:, 0:128], scalar=gn.t[:, 4:5],
                                                                                     in1=ord_[hb].t[:], op0=ALU.mult, op1=ALU.mult),
                         reads=[bank, ord_[hb], gn], writes=[ytmp[hb]])
                    P.op("pool", lambda e, hb=hb, h=h, tb=tb: e.tensor_tensor(out=yst[tb].t[:, h, :], in0=ytmp[hb].t[:], in1=sg_sb[tb].t[:, h, :], op=ALU.mult),
                         reads=[ytmp[hb], sg_sb[tb]], writes=[yst[tb]])

                if so:
                    for h in range(8):
                        half_step(h, h % 2, None, 0)
                        half_step(h, h % 2, None, 1)
                    continue
                fr = {0: front(0, 0)}
                for h in range(8):
                    hb = h % 2
                    mid(h, hb, *fr[h])
                    if h + 1 < 8:
                        fr[h + 1] = front(h + 1, (h + 1) % 2)
                    back(h, hb, fr[h][0])
                P.dma("act", self.mixT.t[12 * 128:20 * 128, r0:r0 + 128].rearrange("(h p) t -> p h t", p=128), yst[tb].t[:], yst[tb], reads=[yst[tb]])

    def resid_epilogue(self, banks, gcol, xsrc, xdst, t0, c0, ji, xbuf, src_off=0, dst_off=0):
        P = self.P
        ps = self.ps
        yT, xin, xo = self.yT[ji % 2], self.xin[ji % 2], self.xo[ji % 2]
        P.dma("sp", xin.t[:], xsrc.t[t0 - src_off:t0 - src_off + G, c0:c0 + 128].rearrange("(t p) c -> p t c", p=128), xin, reads=[xbuf] if xbuf else [], writes=[xin])
        for hf in range(2):
            P.op("act", lambda e, hf=hf: e.activation(out=yT.t[:, hf * 512:(hf + 1) * 512], in_=banks[hf].t[:], func=AF.Identity, scale=gcol),
                 reads=[banks[hf]] + self.modbufs, writes=[yT])
        for t in range(8):
            P.op("pe", lambda e, t=t: e.transpose(ps[6 + t // 4].t[:, (t % 4) * 128:(t % 4 + 1) * 128], yT.t[:, t * 128:(t + 1) * 128], self.ident),
                 reads=[yT, self.consts], writes=[ps[6 + t // 4]])
        for b2 in range(2):
            P.op("dve", lambda e, b2=b2: e.tensor_tensor(out=xo.t[:, b2 * 4:(b2 + 1) * 4, :], in0=ps[6 + b2].t[:].rearrange("p (t c) -> p t c", c=128),
                                                          in1=xin.t[:, b2 * 4:(b2 + 1) * 4, :], op=ALU.add), reads=[ps[6 + b2], xin], writes=[xo])
        P.dma("act", xdst.t[t0 - dst_off:t0 - dst_off + G, c0:c0 + 128].rearrange("(t p) c -> p t c", p=128), xo.t[:], xo, reads=[xo], writes=[xbuf] if xbuf else [])

    def phase_oproj(self, l, xsrc, xdst, grps):
        P = self.P
        ps = self.ps
        self.modbufs = [self.modT[l], self.gsc[l]]
        w = self.w_out
        for grp in grps:
            t0 = grp * G
            with P.scope():
                self.yT = [P.sbuf(f"yT{i}", [128, G], F32) for i in range(2)]
                self.xin = [P.sbuf(f"xin{i}", [128, 8, 128], F32) for i in range(2)]
                self.xo = [P.sbuf(f"xo{i}", [128, 8, 128], F32) for i in range(2)]
                P.dma("sp", self.hT.t[:], self.mixT.t[:, t0:t0 + G].rearrange("(j p) t -> p j t", p=128), self.hT, writes=[self.hT])
                for j in range(KC):
                    banks = (ps[2 * (j % 3)], ps[2 * (j % 3) + 1])

                    def wsrc(k0, nkk, j=j):
                        return w.t[l, k0 * 128:(k0 + nkk) * 128, j * 128:(j + 1) * 128].rearrange("(k p) c -> p k c", p=128)
                    self.fm_job(banks, wsrc, KC, 128, lambda kg, hf: self.hT.t[:, kg, hf * 512:(hf + 1) * 512], [self.hT])
                    self.resid_epilogue(banks, self.modT[l].t[:, 2, j:j + 1], xsrc, xdst, t0, j * 128, j, None)
            P.barrier()

    def phase_mlp(self, l, xsrc, xdst, grps, dst_off=0):
        P = self.P
        ps = self.ps
        self.modbufs = [self.modT[l], self.gsc[l]]
        NE = 4
        CE = DFF // 128 // NE
        for grp in grps:
            t0 = grp * G
            self.norm_group(xsrc, grp, self.gsc[l].t[:, 1, :], self.modT[l].t[:, 3, :])
            with P.scope():
                h1T = P.sbuf("h1T", [128, CE, G], BF16)
                sqs = [P.sbuf(f"fsq{i}", [128, 512], F32) for i in range(2)]
                self.yT = [P.sbuf(f"yT{i}", [128, G], F32) for i in range(2)]
                self.xin = [P.sbuf(f"xin{i}", [128, 8, 128], F32) for i in range(2)]
                self.xo = [P.sbuf(f"xo{i}", [128, 8, 128], F32) for i in range(2)]
                xcols = [Buf(f"xcol{j}") for j in range(KC)]
                ji = 0
                for ep in range(NE):
                    for c in range(CE):
                        banks = (ps[2 * (ji % 3)], ps[2 * (ji % 3) + 1])
                        ff0 = (ep * CE + c) * 128

                        def wsrc(k0, nkk, ff0=ff0):
                            return self.w_mi.t[l, k0 * 128:(k0 + nkk) * 128, ff0:ff0 + 128].rearrange("(k p) c -> p k c", p=128)
                        self.fm_job(banks, wsrc, KC, 128, lambda kg, hf: self.hT.t[:, kg, hf * 512:(hf + 1) * 512], [self.hT])
                        for hf in range(2):
                            sq = sqs[hf]
                            P.op("act", lambda e, sq=sq, hf=hf, banks=banks: e.activation(out=sq.t[:], in_=banks[hf].t[:], func=AF.Square),
                                 reads=[banks[hf]], writes=[sq])
                            P.op("dve", lambda e, sq=sq, hf=hf, banks=banks, c=c: e.scalar_tensor_tensor(
                                out=h1T.t[:, c, hf * 512:(hf + 1) * 512], in0=banks[hf].t[:], scalar=0.0, in1=sq.t[:], op0=ALU.is_gt, op1=ALU.mult),
                                reads=[banks[hf], sq], writes=[h1T])
                        ji += 1
                    for j in range(KC):
                        banks = (ps[2 * (ji % 3)], ps[2 * (ji % 3) + 1])

                        def wsrc2(k0, nkk, j=j, ep=ep):
                            r0 = (ep * CE + k0) * 128
                            return self.w_mo.t[l, r0:r0 + nkk * 128, j * 128:(j + 1) * 128].rearrange("(k p) c -> p k c", p=128)
                        self.fm_job(banks, wsrc2, CE, 128, lambda kg, hf: h1T.t[:, kg, hf * 512:(hf + 1) * 512], [h1T])
                        self.resid_epilogue(banks, self.modT[l].t[:, 5, j:j + 1], xsrc if ep == 0 else xdst, xdst, t0, j * 128, ji, xcols[j],
                                            src_off=(0 if ep == 0 else dst_off), dst_off=dst_off)
                        ji += 1
            P.barrier()


NCORES = 8


def _make_flags(p, sw):
    fl = np.zeros((128, 257 + 12 * 128), np.float32)
    tt = np.arange(16)[:, None]
    nn = np.arange(8)[None, :]
    gm = np.where(nn >= tt // 2, -1.0e30, 0.0).astype(np.float32)
    sel = (tt >= 8) & (nn < 4)
    fb = np.zeros((16, 8), np.float32)
    if p == 0:
        gm = np.where(sel, -1.0e30, gm).astype(np.float32)
        fb = np.where(sel, -30000.0, 0.0).astype(np.float32)
    fl[:, 0:128] = gm.reshape(1, 128)
    fl[:, 128:256] = fb.reshape(1, 128)
    fl[:, 256] = float(p)
    swp = sw.reshape(128, 12, 2, 128)[:, :, 0, :].copy()
    if p == 0:
        swp = swp - 1.0e10
    fl[:, 257:] = swp.reshape(128, 12 * 128)
    return fl


def _prep(inp, b, p, consts):
    c, m, sw = consts
    d = {}
    xb = inp["x"][b]
    d["x"] = np.ascontiguousarray(xb if p == 1 else np.concatenate([xb[G:], xb[:G]], axis=0))
    d["cT"] = np.ascontiguousarray(inp["c"][b].reshape(32, 128).T)
    for k in ("w_ada", "b_ada", "w_in", "w_out", "w_mlp_in", "w_mlp_out"):
        d[k] = inp[k]
    na = inp["norm_attn"].reshape(DEPTH, 32, 128).transpose(0, 2, 1)
    nm = inp["norm_mlp"].reshape(DEPTH, 32, 128).transpose(0, 2, 1)
    d["normT"] = np.ascontiguousarray(np.concatenate([na, nm], axis=2))
    d["gains"] = np.ascontiguousarray(np.stack([inp["moba_q_norm"], inp["moba_k_norm"], inp["swa_q_norm"], inp["swa_k_norm"],
                                                inp["gla_out_norm"]], axis=2))
    d["w_a_aug"] = np.ascontiguousarray(np.concatenate([inp["gla_w_a"], inp["gla_b_a"][:, None, :]], axis=1))
    d["sinks_rep"] = np.ascontiguousarray(np.broadcast_to(inp["swa_sinks"][:, None, :], (DEPTH, 128, 12)))
    d["consts"] = c
    d["mconsts"] = m
    d["swbias"] = sw
    d["flags"] = _make_flags(p, sw)
    return d


def kernel(**inputs):
    inp = {k: np.asarray(v, dtype=np.float32) for k, v in inputs.items()}
    consts = make_consts()
    K = MK(debug=False, v2=True)
    in_maps = [_prep(inp, i // 2, i % 2, consts) for i in range(NCORES)]
    res = run_bass_kernel_spmd(K.nc, in_maps, core_ids=list(range(NCORES)))
    B = inp["x"].shape[0]
    out = np.empty((B, S, D), np.float32)
    for i in range(NCORES):
        b, p = i // 2, i % 2
        out[b, p * G:(p + 1) * G] = np.asarray(res.results[i]["out"])
    return out
```

```python
import os
import numpy as np
import concourse.bass as bass
import concourse.mybir as mybir
from concourse.bass_utils import run_bass_kernel_spmd
import contextlib

F32 = mybir.dt.float32
BF16 = mybir.dt.bfloat16
AF = mybir.ActivationFunctionType
ALU = mybir.AluOpType
AX = mybir.AxisListType

ENGS = ("pe", "act", "dve", "pool", "sp")
SEM_WRAP = 30000


class Buf:
    __slots__ = ("name", "t", "writers", "readers", "dsem", "dcount")

    def __init__(self, name, t=None):
        self.name = name
        self.t = t
        self.writers = {}
        self.readers = {}
        self.dsem = None
        self.dcount = 0

    def __getitem__(self, idx):
        return self.t[idx]


class Op:
    __slots__ = ("eng", "fn", "deps", "sig", "is_dma", "needs_sig", "dma_sig", "idx")

    def __init__(self, eng, fn, is_dma):
        self.eng = eng
        self.fn = fn
        self.deps = []
        self.sig = None
        self.is_dma = is_dma
        self.needs_sig = False
        self.dma_sig = None
        self.idx = 0


class Prog:
    def __init__(self, nc, same_engine_sync=True):
        self.nc = nc
        self.ops = {e: [] for e in ENGS}
        self.stack = contextlib.ExitStack()
        self.same_engine_sync = same_engine_sync
        self.nsem = 0
        self.n_ops = 0
        self.pending = {e: [] for e in ENGS}
        self.open_dmas = {}
        import os
        self.chain_engs = [x for x in os.environ.get("MK_CHAIN", "act").split(",") if x]
        self.scopes = []
        self.scope_bufs = []
        self.free_dsems = []

    def sem(self, name):
        self.nsem += 1
        return self.stack.enter_context(self.nc.semaphore(name))

    def sbuf(self, name, shape, dt):
        st = self.scopes[-1] if self.scopes else self.stack
        self.nalloc = getattr(self, "nalloc", 0) + 1
        t = st.enter_context(self.nc.sbuf_tensor(f"{name}_{self.nalloc}", list(shape), dt))
        b = Buf(name, t)
        if self.scope_bufs:
            self.scope_bufs[-1].append(b)
        return b

    @contextlib.contextmanager
    def scope(self):
        st = contextlib.ExitStack()
        self.scopes.append(st)
        self.scope_bufs.append([])
        try:
            yield
        finally:
            self.barrier()
            self.scopes.pop()
            for b in self.scope_bufs.pop():
                if b.dsem is not None:
                    self.free_dsems.append((b.dsem, b.dcount))
                    b.dsem = None
            st.close()

    def barrier(self):
        lasts = [self.ops[e][-1] for e in ENGS if self.ops[e]]
        dmas = list(self.open_dmas.values())
        self.open_dmas = {}
        for e in ENGS:
            self.pending[e] = [o for o in lasts if o.eng != e or o.is_dma] + dmas

    def psum(self, name, shape, dt):
        t = self.stack.enter_context(self.nc.psum_tensor(name, list(shape), dt))
        return Buf(name, t)

    def dram(self, name, shape, dt, kind="Internal"):
        t = self.nc.dram_tensor(name, list(shape), dt, kind=kind)
        return Buf(name, t.ap())

    def view(self, name, ap):
        return Buf(name, ap)

    def _track(self, op, reads, writes, key):
        deps = op.deps
        for b in reads:
            for k, w in b.writers.items():
                deps.append(w)
            b.readers[key] = op
        for b in writes:
            if b.readers:
                for k, r in b.readers.items():
                    if r is not op:
                        deps.append(r)
                for k, w in b.writers.items():
                    deps.append(w)
                b.readers = {}
                b.writers = {key: op}
            else:
                for k, w in b.writers.items():
                    if not (op.is_dma and w.is_dma):
                        deps.append(w)
                b.writers[key] = op

    def op(self, eng, fn, reads=(), writes=()):
        o = Op(eng, fn, False)
        if self.pending[eng]:
            o.deps.extend(self.pending[eng])
            self.pending[eng] = []
        self._track(o, reads, writes, eng)
        if eng in self.chain_engs and self.ops[eng]:
            o.deps.append(self.ops[eng][-1])
        self.ops[eng].append(o)
        self.n_ops += 1
        return o

    def dma(self, eng, out_ap, in_ap, sb, reads=(), writes=(), custom=None, **kw):
        o = Op(eng, None, True)
        if sb.dsem is None:
            if self.free_dsems:
                sb.dsem, sb.dcount = self.free_dsems.pop()
            else:
                sb.dsem = self.sem(f"d_{sb.name}_{self.nsem}")
        sb.dcount += 16
        o.dma_sig = (sb.dsem, sb.dcount)
        dsem = sb.dsem

        def fn(e, out_ap=out_ap, in_ap=in_ap, kw=kw, dsem=dsem):
            if custom is not None:
                return custom(e).then_inc(dsem, 16)
            return e.dma_start(out=out_ap, in_=in_ap, **kw).then_inc(dsem, 16)
        o.fn = fn
        if self.pending[eng]:
            o.deps.extend(self.pending[eng])
            self.pending[eng] = []
        self.open_dmas[id(dsem)] = o
        self._track(o, reads, writes, ("dma", id(sb)))
        self.ops[eng].append(o)
        self.n_ops += 1
        return o

    def emit(self, final_waits=()):
        nc = self.nc
        final_waits = list(final_waits) + list(self.pending["sp"])
        for e in ENGS:
            for o in self.ops[e]:
                for d in o.deps:
                    if d.is_dma:
                        continue
                    if d.eng == o.eng and not o.is_dma and (not self.same_engine_sync or d.eng == "pe"):
                        continue
                    d.needs_sig = True
        for o in final_waits:
            if not o.is_dma:
                o.needs_sig = True
        for e in ENGS:
            cur = None
            cnt = 0
            for o in self.ops[e]:
                if o.is_dma or not o.needs_sig:
                    continue
                if cur is None or cnt >= SEM_WRAP:
                    cur = self.sem(f"s_{e}_{self.nsem}")
                    cnt = 0
                cnt += 1
                o.sig = (cur, cnt)
        engmap = {"pe": "tensor", "act": "scalar", "dve": "vector", "pool": "gpsimd", "sp": "sync"}
        with nc.Block() as block:
            for e in ENGS:
                ops = self.ops[e]
                if e == "sp":
                    ops = ops + []
                dec = getattr(block, engmap[e])

                def body(eng, ops=ops, e=e, last=(e == "sp")):
                    waited = {}

                    def wait(sem, val):
                        k = id(sem)
                        if waited.get(k, 0) >= val:
                            return
                        waited[k] = val
                        eng.wait_ge(sem, val)
                    for o in ops:
                        for d in o.deps:
                            if d.is_dma:
                                wait(*d.dma_sig)
                            else:
                                if d.sig is None:
                                    continue
                                wait(*d.sig)
                        ins = o.fn(eng)
                        if o.sig is not None:
                            ins.then_inc(o.sig[0], 1)
                    if last:
                        for o in final_waits:
                            if o.is_dma:
                                wait(*o.dma_sig)
                            else:
                                wait(*o.sig)
                dec(body)
        self.stack.close()


D = 4096
KC = 32
S = 2048
NT = 16
DIN = 10256
DFF = 16384
G = 1024
NGRP = S // G
EPS = 1e-6
DEPTH = 2
SCALE = 128 ** -0.5
BIG = 1.0e9

C_MQ, C_MK, C_MV, C_GQ, C_GK, C_GV, C_GG, C_GA, C_SQ, C_SK, C_SV = (
    0, 1536, 3072, 4608, 5120, 5632, 6656, 7680, 7696, 9232, 9744)

MOBA_SLOPES = [2.0 ** (-8.0 * h / 12) for h in range(1, 13)]
SWA_SLOPES = [2.0 ** (-8.0 * h / 12) for h in range(1, 13)]

CO_ID, CO_ONES, CO_TRI, CO_LTRI, CO_OH2, CO_PAST, CO_GMASK, CO_END = 0, 128, 256, 384, 512, 514, 642, 770
MO_DPL, MO_DBIG, MO_SEL, MO_END = 0, 512, 1408, 2432


def make_consts():
    c = np.zeros((128, CO_END), np.float32)
    c[:, CO_ID:CO_ID + 128] = np.eye(128, dtype=np.float32)
    c[:, CO_ONES:CO_ONES + 128] = 1.0
    s = np.arange(128)[:, None]
    t = np.arange(128)[None, :]
    tri = ((s // 64) == (t // 64)) & (s <= t)
    c[:, CO_TRI:CO_TRI + 128] = tri.astype(np.float32)
    c[:, CO_LTRI:CO_LTRI + 128] = -tri.astype(np.float32) / 16.0
    c[63, CO_OH2] = 1.0
    c[127, CO_OH2 + 1] = 1.0
    tt = np.arange(16)[:, None]
    nn = np.arange(8)[None, :]
    past = ((nn < tt // 2) & (tt >= 8)).astype(np.float32).reshape(1, 128)
    c[:, CO_PAST:CO_PAST + 128] = past
    gm = np.where(nn >= tt // 2, -1.0e30, 0.0).astype(np.float32).reshape(1, 128)
    c[:, CO_GMASK:CO_GMASK + 128] = gm
    m = np.zeros((128, MO_END), np.float32)
    sp = np.arange(128)[:, None].astype(np.float64)
    tp = np.arange(512)[None, :].astype(np.float64)
    m[:, MO_DPL:MO_DPL + 512] = tp - sp
    u = np.arange(896)[None, :].astype(np.float64)
    db = u - 384 - sp
    m[:, MO_DBIG:MO_DBIG + 896] = np.where(db >= 0, db, BIG)
    for n in range(8):
        m[n, MO_SEL + n * 128:MO_SEL + (n + 1) * 128] = 1.0
    sw = np.zeros((128, 12, 2, 128), np.float64)
    s1 = np.arange(128)[:, None]
    t1 = np.arange(128)[None, :]
    dprev = np.where(t1 < s1, 128.0 + t1 - s1, BIG)
    ddiag = np.where(t1 >= s1, (t1 - s1).astype(np.float64), BIG)
    for h in range(12):
        sw[:, h, 0, :] = -SWA_SLOPES[h] / SCALE * dprev
        sw[:, h, 1, :] = -SWA_SLOPES[h] / SCALE * ddiag
    return c, m, sw.reshape(128, 12 * 2 * 128).astype(np.float32)


class MK:
    def __init__(self, debug=False, upto="all", depth=DEPTH, only=None, v2=False):
        self.v2 = v2
        self.only = only
        self.debug = debug
        self.upto = upto
        self.depth = depth
        nc = bass.Bass("TRN2", target_bir_lowering=False)
        self.nc = nc
        P = Prog(nc)
        self.P = P
        self.outs = []
        self.build()

    def din(self, name, shape, dt=F32):
        if self.only is not None and name in ("w_ada", "w_in", "w_out", "w_mlp_in", "w_mlp_out", "x", "b_ada"):
            shape = [2, 2]
        return self.P.dram(name, shape, dt, kind="ExternalInput")

    def dscr(self, name, shape, dt):
        if self.only == "gla" and name in ("gq", "gk", "gv", "sgT", "gaT"):
            return self.P.dram(name, shape, dt, kind="ExternalInput")
        if self.debug:
            self.outs.append(name)
            return self.P.dram(name, shape, dt, kind="ExternalOutput")
        return self.P.dram(name, shape, dt, kind="Internal")

    def wload(self, src_ap, a, b, eng=None):
        P = self.P
        i = self.wi
        self.wi += 1
        st = self.wst[i % len(self.wst)]
        bf = self.wbf[i % len(self.wbf)]
        n = a * b
        stv = st.t[:, 0:n].rearrange("p (a b) -> p a b", b=b)
        bfv = bf.t[:, 0:n].rearrange("p (a b) -> p a b", b=b)
        P.dma("sp", stv, src_ap, st, writes=[st])
        ce = self.cast_engs[i % len(self.cast_engs)]
        if eng is not None:
            ce = eng[i % len(eng)]
        if ce == "act":
            P.op("act", lambda e: e.copy(out=bf.t[:, 0:n], in_=st.t[:, 0:n]), reads=[st], writes=[bf])
        else:
            P.op(ce, lambda e: e.tensor_copy(out=bf.t[:, 0:n], in_=st.t[:, 0:n]), reads=[st], writes=[bf])
        return bf, bfv

    def barrier(self):
        self.P.barrier()

    def build(self):
        P = self.P
        dbg = self.debug
        self.x_in = self.din("x", [S, D])
        self.cT = self.din("cT", [128, KC])
        self.w_ada = self.din("w_ada", [DEPTH, D, 6 * D])
        self.b_ada = self.din("b_ada", [DEPTH, 6 * D])
        self.normT = self.din("normT", [DEPTH, 128, 2 * KC])
        self.w_in = self.din("w_in", [DEPTH, D, DIN])
        self.gains = self.din("gains", [DEPTH, 128, 5])
        self.w_a_aug = self.din("w_a_aug", [DEPTH, 17, 512])
        self.esink_in = self.din("sinks_rep", [DEPTH, 128, 12])
        self.w_out = self.din("w_out", [DEPTH, D, D])
        self.w_mi = self.din("w_mlp_in", [DEPTH, D, DFF])
        self.w_mo = self.din("w_mlp_out", [DEPTH, DFF, D])
        self.consts_d = self.din("consts", [128, CO_END])
        self.mconsts_d = self.din("mconsts", [128, MO_END])
        self.swb_d = self.din("swbias", [128, 12 * 2 * 128])
        self.flags_d = self.din("flags", [128, 128 + 128 + 1 + 12 * 128])
        self.out = P.dram("out", [G if self.v2 else S, D], F32, kind="ExternalOutput")
        self.qmT = self.dscr("qmT", [12 * 128, S], BF16)
        self.kmT = self.dscr("kmT", [12 * 128, S], BF16)
        self.vm = self.dscr("vm", [S, 1536], BF16)
        self.gq = self.dscr("gq", [S, 512], F32)
        self.gk = self.dscr("gk", [S, 512], F32)
        self.gv = self.dscr("gv", [S, 1024], BF16)
        self.sgT = self.dscr("sgT", [8 * 128, S], F32)
        self.gaT = self.dscr("gaT", [16, S], F32)
        self.sqT = self.dscr("sqT", [12 * 128, S], BF16)
        self.skT = self.dscr("skT", [4 * 128, S], BF16)
        self.sv = self.dscr("sv", [S, 512], BF16)
        self.mixT = self.dscr("mixT", [32 * 128, S], BF16)
        self.xa = self.dscr("xa", [S, D], F32)
        self.xb = self.dscr("xb", [S, D], F32)
        if dbg:
            self.modT_d = self.dscr("modT_d", [DEPTH, 128, 6 * KC], F32)
            self.hT_d = self.dscr("hT_d", [KC * 128, G], BF16)
        self.consts = P.sbuf("consts", [128, CO_END], F32)
        self.ones_bf = P.sbuf("ones_bf", [128, 128], BF16)
        self.wst = [P.sbuf(f"wst{i}", [128, 2048], F32) for i in range(3)]
        self.wbf = [P.sbuf(f"wbf{i}", [128, 2048], BF16) for i in range(4)]
        self.wi = 0
        self.cast_engs = ["pool"]
        self.hT = P.sbuf("hT", [128, KC, G], BF16)
        self.modT = [P.sbuf(f"modT{l}", [128, 6, KC], F32) for l in range(DEPTH)]
        self.gsc = [P.sbuf(f"gsc{l}", [128, 2, KC], F32) for l in range(DEPTH)]
        self.gn = [P.sbuf(f"gn{l}", [128, 5], F32) for l in range(DEPTH)]
        self.kmean = P.sbuf("kmean", [128, 12, 8], F32)
        self.ps = [P.psum(f"ps{i}", [128, 512], F32) for i in range(8)]
        c = self.consts
        self.ident = c.t[:, CO_ID:CO_ID + 128]
        self.ones_f = c.t[:, CO_ONES:CO_ONES + 128]
        self.tri = c.t[:, CO_TRI:CO_TRI + 128]
        self.ltri = c.t[:, CO_LTRI:CO_LTRI + 128]
        self.oh2 = c.t[:, CO_OH2:CO_OH2 + 2]
        self.pastm = c.t[:, CO_PAST:CO_PAST + 128]
        self.gmask = c.t[:, CO_GMASK:CO_GMASK + 128]
        self.flags = P.sbuf("flags", [128, 257], F32)
        P.dma("sp", self.flags.t[:], self.flags_d.t[:, 0:257], self.flags, writes=[self.flags])
        self.gmask_p = self.flags.t[:, 0:128]
        self.flagb = self.flags.t[:, 128:256]
        self.fcol = self.flags.t[:, 256:257]

        P.dma("sp", c.t[:], self.consts_d[:], c, writes=[c])
        P.op("dve", lambda e: e.tensor_copy(out=self.ones_bf.t[:], in_=self.ones_f), reads=[c], writes=[self.ones_bf])
        for l in range(DEPTH):
            P.dma("sp", self.gn[l].t[:], self.gains.t[l], self.gn[l], writes=[self.gn[l]])

        if self.only == "gla":
            self.phase_gla(0)
            return self.finish()
        self.phase_mod()
        if self.upto == "mod":
            return self.finish()
        xsrc = self.x_in
        for l in range(self.depth):
            last = (l == self.depth - 1)
            xdst = self.out if last else self.xb
            half = self.v2 and last
            grps = [1] if half else list(range(NGRP))
            self.phase_proj(l, xsrc, a_kv_only=half)
            if self.upto == "proj":
                return self.finish()
            self.phase_moba(l, qgroups=(2, 3) if half else (0, 1, 2, 3))
            self.phase_gla(l, a_state_only=half)
            self.phase_swa(l, tiles=range(8, 16) if half else range(16))
            self.phase_oproj(l, xsrc, self.xa, grps)
            self.phase_mlp(l, self.xa, xdst, grps, dst_off=(G if half else 0))
            xsrc = xdst
        self.finish()

    def finish(self):
        P = self.P
        P.barrier()
        P.emit()

    def phase_mod(self):
        P = self.P
        ps = self.ps
        with P.scope():
            cT = P.sbuf("cT", [128, KC], F32)
            cond = P.sbuf("cond", [128, KC], BF16)
            row = [P.sbuf(f"mrow{i}", [1, 2048], F32) for i in range(2)]
            brow = [P.sbuf(f"brow{i}", [1, 2048], F32) for i in range(2)]
            nT = P.sbuf("nT", [128, 2 * KC], F32)
            g_wst, g_wbf = self.wst, self.wbf
            self.wst = g_wst + [P.sbuf(f"mwst{i}", [128, 2048], F32) for i in range(4)]
            self.wbf = g_wbf + [P.sbuf(f"mwbf{i}", [128, 2048], BF16) for i in range(3)]
            P.dma("sp", cT.t[:], self.cT.t[:], cT, writes=[cT])
            P.op("act", lambda e: e.activation(out=cond.t[:], in_=cT.t[:], func=AF.Silu), reads=[cT], writes=[cond])
            for l in range(self.depth):
                modT = self.modT[l]
                for g in range(12):
                    r = row[g % 2]
                    br = brow[g % 2]
                    P.dma("sp", br.t[:], self.b_ada.t[l:l + 1, g * 2048:(g + 1) * 2048], br, writes=[br])
                    for k in range(KC):
                        src = self.w_ada.t[l, k * 128:(k + 1) * 128, g * 2048:(g + 1) * 2048].rearrange("p (a b) -> p a b", b=512)
                        bf, bv = self.wload(src, 4, 512, eng=("dve", "act", "dve", "act", "pool"))
                        for n in range(4):
                            P.op("pe", lambda e, n=n, k=k, bv=bv: e.matmul(ps[n].t[0:1, :], lhsT=cond.t[:, k:k + 1], rhs=bv[:, n, :],
                                                                              start=(k == 0), stop=(k == KC - 1)),
                                 reads=[cond, bf], writes=[ps[n]])
                    for n in range(4):
                        P.op("dve", lambda e, n=n, r=r, br=br: e.tensor_tensor(out=r.t[0:1, n * 512:(n + 1) * 512], in0=ps[n].t[0:1, :],
                                                                                in1=br.t[0:1, n * 512:(n + 1) * 512], op=ALU.add),
                             reads=[ps[n], br], writes=[r])
                    kind, half = g // 2, g % 2
                    for j in range(16):
                        P.op("pe", lambda e, j=j, r=r: e.matmul(ps[4].t[:, j:j + 1], lhsT=r.t[0:1, j * 128:(j + 1) * 128],
                                                                 rhs=self.ones_f[0:1, 0:1], start=True, stop=True),
                             reads=[r, self.consts], writes=[ps[4]])
                    P.op("dve", lambda e, kind=kind, half=half, modT=modT: e.tensor_copy(out=modT.t[:, kind, half * 16:(half + 1) * 16],
                                                                                      in_=ps[4].t[:, 0:16]),
                         reads=[ps[4]], writes=[modT])
                P.dma("sp", nT.t[:], self.normT.t[l], nT, writes=[nT])
                for w, kind in ((0, 1), (1, 4)):
                    P.op("dve", lambda e, w=w, kind=kind, modT=modT, l=l: e.scalar_tensor_tensor(
                        out=self.gsc[l].t[:, w, :], in0=modT.t[:, kind, :], scalar=1.0, in1=nT.t[:, w * KC:(w + 1) * KC],
                        op0=ALU.add, op1=ALU.mult), reads=[modT, nT], writes=[self.gsc[l]])
                if self.debug:
                    P.dma("act", self.modT_d.t[l], modT.t[:].rearrange("p a b -> p (a b)"), modT, reads=[modT])
            self.wst, self.wbf = g_wst, g_wbf
            self.wi = 0
        P.barrier()

    def norm_group(self, xsrc, grp, gsc_ap, sh_ap):
        P = self.P
        ps = self.ps
        with P.scope():
            xts = [P.sbuf(f"xt{i}", [128, D], F32) for i in range(2)]
            junk = P.sbuf("junk", [128, D], BF16)
            st = P.sbuf("nstat", [128, 3, 8], F32)
            for t in range(G // 128):
                tok0 = grp * G + t * 128
                xt = xts[t % 2]
                P.dma("sp", xt.t[:], xsrc.t[tok0:tok0 + 128, :], xt, writes=[xt])
                P.op("act", lambda e, xt=xt, t=t: e.activation(out=junk.t[:], in_=xt.t[:], func=AF.Square, accum_out=st.t[:, 0, t:t + 1]),
                     reads=[xt], writes=[junk, st])
                P.op("act", lambda e, t=t: e.activation(out=st.t[:, 1, t:t + 1], in_=st.t[:, 0, t:t + 1], func=AF.Sqrt, bias=EPS, scale=1.0 / D),
                     reads=[st], writes=[st])
                P.op("dve", lambda e, t=t: e.reciprocal(out=st.t[:, 2, t:t + 1], in_=st.t[:, 1, t:t + 1]), reads=[st], writes=[st])
                P.op("dve", lambda e, xt=xt, t=t: e.tensor_scalar(out=xt.t[:], in0=xt.t[:], scalar1=st.t[:, 2, t:t + 1], scalar2=None, op0=ALU.mult),
                     reads=[xt, st], writes=[xt])
                for jb in range(8):
                    bank = ps[jb % 2]
                    for q in range(4):
                        j = jb * 4 + q
                        P.op("pe", lambda e, bank=bank, q=q, j=j, xt=xt: e.transpose(bank.t[:, q * 128:(q + 1) * 128], xt.t[:, j * 128:(j + 1) * 128], self.ident),
                             reads=[xt, self.consts], writes=[bank])
                    for q in range(4):
                        j = jb * 4 + q
                        if q % 2 == 0:
                            P.op("act", lambda e, bank=bank, q=q, j=j, t=t: e.activation(
                                out=self.hT.t[:, j, t * 128:(t + 1) * 128], in_=bank.t[:, q * 128:(q + 1) * 128], func=AF.Identity,
                                scale=gsc_ap[:, j:j + 1], bias=sh_ap[:, j:j + 1]), reads=[bank] + self.modbufs, writes=[self.hT])
                        else:
                            P.op("dve", lambda e, bank=bank, q=q, j=j, t=t: e.tensor_scalar(
                                out=self.hT.t[:, j, t * 128:(t + 1) * 128], in0=bank.t[:, q * 128:(q + 1) * 128],
                                scalar1=gsc_ap[:, j:j + 1], scalar2=sh_ap[:, j:j + 1], op0=ALU.mult, op1=ALU.add),
                                reads=[bank] + self.modbufs, writes=[self.hT])

    def fm_job(self, banks, wsrc_fn, nk, ncols, rhs_fn, rhs_bufs):
        P = self.P
        kk_per = 2048 // 128
        k = 0
        while k < nk:
            nkk = min(kk_per, nk - k)
            bf, bv = self.wload(wsrc_fn(k, nkk), nkk, ncols)
            for kk in range(nkk):
                kg = k + kk
                for hf in range(2):
                    P.op("pe", lambda e, hf=hf, kg=kg, kk=kk, bv=bv: e.matmul(banks[hf].t[0:ncols, :], lhsT=bv[:, kk, :], rhs=rhs_fn(kg, hf),
                                                                             start=(kg == 0), stop=(kg == nk - 1)),
                         reads=[bf] + rhs_bufs, writes=[banks[hf]])
            k += nkk

    def phase_proj(self, l, xsrc, a_kv_only=False):
        P = self.P
        ps = self.ps
        self.modbufs = [self.modT[l], self.gsc[l]]
        gn = self.gn[l]
        jobs = []
        for h in range(12):
            jobs.append(("fmn", C_MQ + h * 128, 128, (self.qmT, h, 0, None)))
        for h in range(12):
            jobs.append(("fmn", C_MK + h * 128, 128, (self.kmT, h, 1, h)))
        for h in range(12):
            jobs.append(("tm", C_MV + h * 128, 128, (self.vm, h * 128, BF16)))
        for j in range(4):
            jobs.append(("tm", C_GQ + j * 128, 128, (self.gq, j * 128, F32)))
        for j in range(4):
            jobs.append(("tm", C_GK + j * 128, 128, (self.gk, j * 128, F32)))
        for j in range(8):
            jobs.append(("tm", C_GV + j * 128, 128, (self.gv, j * 128, BF16)))
        for j in range(8):
            jobs.append(("fms", C_GG + j * 128, 128, (self.sgT, j)))
        jobs.append(("fma", C_GA, 16, None))
        for h in range(12):
            jobs.append(("fmn", C_SQ + h * 128, 128, (self.sqT, h, 2, None)))
        for h in range(4):
            jobs.append(("fmn", C_SK + h * 128, 128, (self.skT, h, 3, None)))
        for j in range(4):
            jobs.append(("tm", C_SV + j * 128, 128, (self.sv, j * 128, BF16)))
        if self.upto == "projq":
            jobs = jobs[:2]
        w = self.w_in
        all_jobs = jobs
        kv_cols = [(C_MK, C_GQ), (C_GK, C_GG), (C_GA, C_SQ), (C_SK, DIN)]
        kv_jobs = [j for j in all_jobs if any(a <= j[1] < b for a, b in kv_cols)]
        for grp in range(NGRP):
            jobs = kv_jobs if (a_kv_only and grp == 0) else all_jobs
            self.norm_group(xsrc, grp, self.gsc[l].t[:, 0, :], self.modT[l].t[:, 0, :])
            if self.debug and grp == 0 and l == 0:
                P.barrier()
                P.dma("act", self.hT_d.t[:].rearrange("(j p) t -> p j t", p=128), self.hT.t[:], self.hT, reads=[self.hT])
            t0 = grp * G
            with P.scope():
                sq = [P.sbuf(f"sq{i}", [128, 512], BF16) for i in range(2)]
                sd = [P.sbuf(f"sd{i}", [128, 512], F32) for i in range(2)]
                stg_bf = [P.sbuf(f"stgb{i}", [128, 1024], BF16) for i in range(2)]
                stg_f = [P.sbuf(f"stgf{i}", [128, 1024], F32) for i in range(2)]
                pending = None
                for ji, (kind, c0, ncols, meta) in enumerate(jobs):
                    banks = (ps[2 * (ji % 3)], ps[2 * (ji % 3) + 1])

                    def wsrc(k0, nkk, c0=c0, ncols=ncols):
                        return w.t[l, k0 * 128:(k0 + nkk) * 128, c0:c0 + ncols].rearrange("(k p) c -> p k c", p=128)
                    if kind != "tm":
                        self.fm_job(banks, wsrc, KC, ncols, lambda kg, hf: self.hT.t[:, kg, hf * 512:(hf + 1) * 512], [self.hT])
                    else:
                        wts = []
                        for k0 in (0, 16):
                            wts.append(self.wload(wsrc(k0, 16), 16, ncols))
                        for t in range(8):
                            for kg in range(KC):
                                bf, bv = wts[kg // 16]
                                P.op("pe", lambda e, t=t, kg=kg, bv=bv, banks=banks: e.matmul(
                                    banks[t // 4].t[:, (t % 4) * 128:(t % 4) * 128 + 128], lhsT=self.hT.t[:, kg, t * 128:(t + 1) * 128],
                                    rhs=bv[:, kg % 16, :], start=(kg == 0), stop=(kg == KC - 1)),
                                    reads=[bf, self.hT], writes=[banks[t // 4]])
                    if pending is not None:
                        pending()
                        pending = None
                    sb = stg_bf[ji % 2]
                    sf = stg_f[ji % 2]
                    if kind == "fmn":
                        dst, h, gi, kmh = meta
                        for hf in range(2):
                            P.op("act", lambda e, hf=hf, banks=banks: e.activation(out=sq[hf].t[:], in_=banks[hf].t[:], func=AF.Square),
                                 reads=[banks[hf]], writes=[sq[hf]])

                        def fin(banks=banks, sb=sb, dst=dst, h=h, gi=gi, kmh=kmh, t0=t0, grp=grp):
                            for hf in range(2):
                                P.op("pe", lambda e, hf=hf: e.matmul(ps[6 + hf].t[:], lhsT=self.ones_bf.t[:], rhs=sq[hf].t[:], start=True, stop=True),
                                     reads=[self.ones_bf, sq[hf]], writes=[ps[6 + hf]])
                                P.op("act", lambda e, hf=hf: e.activation(out=sd[hf].t[:], in_=ps[6 + hf].t[:], func=AF.Sqrt, bias=EPS, scale=1.0 / 128),
                                     reads=[ps[6 + hf]], writes=[sd[hf]])
                                P.op("dve", lambda e, hf=hf: e.reciprocal(out=sd[hf].t[:], in_=sd[hf].t[:]), reads=[sd[hf]], writes=[sd[hf]])
                                P.op("dve", lambda e, hf=hf: e.scalar_tensor_tensor(out=sb.t[:, hf * 512:(hf + 1) * 512], in0=banks[hf].t[:],
                                                                                   scalar=gn.t[:, gi:gi + 1], in1=sd[hf].t[:], op0=ALU.mult, op1=ALU.mult),
                                     reads=[banks[hf], sd[hf], gn], writes=[sb])
                            if kmh is not None:
                                P.op("dve", lambda e: e.tensor_reduce(out=self.kmean.t[:, kmh, grp * 4:(grp + 1) * 4],
                                                                      in_=sb.t[:].rearrange("p (n k) -> p n k", k=256), axis=AX.X, op=ALU.add),
                                     reads=[sb], writes=[self.kmean])
                            P.dma("act", dst.t[h * 128:(h + 1) * 128, t0:t0 + G], sb.t[:], sb, reads=[sb])
                        pending = fin
                    elif kind == "fms":
                        dst, j = meta
                        for hf in range(2):
                            P.op("act", lambda e, hf=hf, banks=banks, sf=sf: e.activation(out=sf.t[:, hf * 512:(hf + 1) * 512], in_=banks[hf].t[:], func=AF.Silu),
                                 reads=[banks[hf]], writes=[sf])
                        P.dma("act", dst.t[j * 128:(j + 1) * 128, t0:t0 + G], sf.t[:], sf, reads=[sf])
                    elif kind == "fma":
                        for hf in range(2):
                            P.op("dve", lambda e, hf=hf, banks=banks, sf=sf: e.tensor_copy(out=sf.t[0:16, hf * 512:(hf + 1) * 512], in_=banks[hf].t[0:16, :]),
                                 reads=[banks[hf]], writes=[sf])
                        P.dma("act", self.gaT.t[:, t0:t0 + G], sf.t[0:16, :], sf, reads=[sf])
                    elif kind == "tm":
                        dst, dc0, dt = meta
                        stg = sb if dt == BF16 else sf
                        for b2 in range(2):
                            eng = "act" if b2 == 0 else "dve"
                            if eng == "act":
                                P.op("act", lambda e, b2=b2, banks=banks, stg=stg: e.copy(out=stg.t[:, b2 * 512:(b2 + 1) * 512], in_=banks[b2].t[:]),
                                     reads=[banks[b2]], writes=[stg])
                            else:
                                P.op("dve", lambda e, b2=b2, banks=banks, stg=stg: e.tensor_copy(out=stg.t[:, b2 * 512:(b2 + 1) * 512], in_=banks[b2].t[:]),
                                     reads=[banks[b2]], writes=[stg])
                        P.dma("act", dst.t[t0:t0 + G, dc0:dc0 + 128].rearrange("(t p) c -> p t c", p=128),
                              stg.t[:].rearrange("p (t c) -> p t c", c=128), stg, reads=[stg])
                if pending is not None:
                    pending()
            P.barrier()

    def phase_moba(self, l, qgroups=(0, 1, 2, 3)):
        P = self.P
        ps = self.ps
        with P.scope():
            mc = P.sbuf("mconst", [128, MO_END], F32)
            P.dma("sp", mc.t[:], self.mconsts_d.t[:], mc, writes=[mc])
            sel_bf = P.sbuf("sel_bf", [8, 1024], BF16)
            P.op("dve", lambda e: e.tensor_copy(out=sel_bf.t[:], in_=mc.t[0:8, MO_SEL:MO_SEL + 1024]), reads=[mc], writes=[sel_bf])
            kmean_bf = P.sbuf("kmean_bf", [128, 12, 8], BF16)
            P.op("dve", lambda e: e.tensor_scalar(out=kmean_bf.t[:], in0=self.kmean.t[:], scalar1=1.0 / 256, scalar2=None, op0=ALU.mult),
                 reads=[self.kmean], writes=[kmean_bf])
            qT = [P.sbuf(f"mq{i}", [128, S], BF16) for i in range(2)]
            kT = [P.sbuf(f"mk{i}", [128, S], BF16) for i in range(2)]
            vt = [P.sbuf(f"mv{i}", [128, 16, 128], BF16) for i in range(2)]
            gate = P.sbuf("gate", [128, 128], F32)
            top8 = P.sbuf("top8", [128, 8, 8], F32)
            mb = P.sbuf("mbias", [128, 128], F32)
            maskT = P.sbuf("maskT", [8, 1024], BF16)
            pTs = [P.sbuf(f"pT{i}", [128, 512], BF16) for i in range(4)]
            tmps = [P.sbuf(f"mtmp{i}", [128, 512], F32) for i in range(4)]
            rec = P.sbuf("mrec", [128, 512], F32)
            osts = [P.sbuf(f"most{i}", [128, 512], BF16) for i in range(2)]
            P.op("dve", lambda e: e.memset(mb.t[:], 0.0), writes=[mb])
            it = 0
            for h in range(12):
                q, k, v = qT[h % 2], kT[h % 2], vt[h % 2]
                P.dma("sp", q.t[:], self.qmT.t[h * 128:(h + 1) * 128, :], q, writes=[q])
                P.dma("sp", k.t[:], self.kmT.t[h * 128:(h + 1) * 128, :], k, writes=[k])
                P.dma("sp", v.t[:], self.vm.t[:, h * 128:(h + 1) * 128].rearrange("(t p) c -> p t c", p=128), v, writes=[v])
                for t in range(16):
                    P.op("pe", lambda e, t=t, q=q, h=h: e.matmul(ps[7].t[:, t * 8:(t + 1) * 8], lhsT=q.t[:, t * 128:(t + 1) * 128],
                                                                 rhs=kmean_bf.t[:, h, :], start=True, stop=True),
                         reads=[q, kmean_bf], writes=[ps[7]])
                P.op("dve", lambda e: e.tensor_tensor(out=gate.t[:], in0=ps[7].t[:, 0:128], in1=self.gmask_p, op=ALU.add),
                     reads=[ps[7], self.flags], writes=[gate])
                for t in range(8, 16):
                    P.op("dve", lambda e, t=t: e.max(out=top8.t[:, t - 8, :], in_=gate.t[:, t * 8:(t + 1) * 8]), reads=[gate], writes=[top8])
                    P.op("dve", lambda e, t=t: e.tensor_scalar(out=mb.t[:, t * 8:(t + 1) * 8], in0=gate.t[:, t * 8:(t + 1) * 8],
                                                              scalar1=top8.t[:, t - 8, 2:3], scalar2=-30000.0, op0=ALU.is_lt, op1=ALU.mult),
                         reads=[gate, top8], writes=[mb])
                P.op("dve", lambda e: e.tensor_tensor(out=mb.t[:], in0=mb.t[:], in1=self.pastm, op=ALU.mult), reads=[mb, self.consts], writes=[mb])
                P.op("dve", lambda e: e.tensor_tensor(out=mb.t[:], in0=mb.t[:], in1=self.flagb, op=ALU.add), reads=[mb, self.flags], writes=[mb])
                for r in range(2):
                    for qq in range(4):
                        t = 8 + r * 4 + qq
                        P.op("pe", lambda e, t=t, qq=qq: e.transpose(ps[6].t[0:8, qq * 128:(qq + 1) * 128], mb.t[:, t * 8:(t + 1) * 8], self.ident),
                             reads=[mb, self.consts], writes=[ps[6]])
                    P.op("act", lambda e, r=r: e.copy(out=maskT.t[:, r * 512:(r + 1) * 512], in_=ps[6].t[0:8, :]), reads=[ps[6]], writes=[maskT])
                slope = MOBA_SLOPES[h]
                pairs = [(g, kt) for g in qgroups for kt in range(4 * g + 4)]
                LOOK = 3
                ctx = {}

                def emit_score(idx, h=h, q=q, k=k, slope=slope, ctx=ctx):
                    g, kt = pairs[idx]
                    sb = ps[2 + (idx % 4)]
                    tmp = tmps[idx % 4]
                    pT = pTs[idx % 4]
                    ctx[idx] = pT
                    n = kt // 2
                    need_mask = (g >= 2) and (n <= 2 * g)
                    P.op("pe", lambda e, sb=sb, kt=kt, g=g, k=k, q=q, need_mask=need_mask: e.matmul(
                        sb.t[:], lhsT=k.t[:, kt * 128:(kt + 1) * 128], rhs=q.t[:, g * 512:(g + 1) * 512], start=True, stop=not need_mask),
                        reads=[k, q], writes=[sb])
                    if need_mask:
                        P.op("pe", lambda e, sb=sb, n=n, g=g: e.matmul(sb.t[:], lhsT=sel_bf.t[0:8, n * 128:(n + 1) * 128],
                                                                       rhs=maskT.t[0:8, (g - 2) * 512:(g - 1) * 512], start=False, stop=True),
                             reads=[sel_bf, maskT], writes=[sb])
                    j = kt - 4 * g
                    if j >= 0:
                        c0 = MO_DBIG + 384 - 128 * j
                        delta = 0.0
                    else:
                        c0 = MO_DPL
                        delta = float(512 * g - 128 * kt)
                    P.op("dve", lambda e, tmp=tmp, sb=sb, c0=c0, slope=slope: e.scalar_tensor_tensor(
                        out=tmp.t[:], in0=mc.t[:, c0:c0 + 512], scalar=-slope / SCALE, in1=sb.t[:], op0=ALU.mult, op1=ALU.add),
                        reads=[mc, sb], writes=[tmp])
                    P.op("act", lambda e, tmp=tmp, pT=pT, slope=slope, delta=delta: e.activation(
                        out=pT.t[:], in_=tmp.t[:], func=AF.Exp, scale=SCALE, bias=-slope * delta), reads=[tmp], writes=[pT])

                def emit_pv(idx, h=h, v=v, ctx=ctx):
                    g, kt = pairs[idx]
                    nkt = 4 * g + 4
                    pT = ctx.pop(idx)
                    oT, den = ps[0], ps[1]
                    P.op("pe", lambda e, kt=kt, pT=pT, nkt=nkt: e.matmul(oT.t[:], lhsT=v.t[:, kt, :], rhs=pT.t[:],
                                                                        start=(kt == 0), stop=(kt == nkt - 1)),
                         reads=[v, pT], writes=[oT])
                    P.op("pe", lambda e, kt=kt, pT=pT, nkt=nkt: e.matmul(den.t[:], lhsT=self.ones_bf.t[:], rhs=pT.t[:],
                                                                        start=(kt == 0), stop=(kt == nkt - 1)),
                         reads=[self.ones_bf, pT], writes=[den])
                    if kt == nkt - 1:
                        ost = osts[g % 2]
                        P.op("dve", lambda e: e.reciprocal(out=rec.t[:], in_=den.t[:]), reads=[den], writes=[rec])
                        P.op("dve", lambda e, ost=ost: e.tensor_tensor(out=ost.t[:], in0=oT.t[:], in1=rec.t[:], op=ALU.mult),
                             reads=[oT, rec], writes=[ost])
                        P.dma("act", self.mixT.t[h * 128:(h + 1) * 128, g * 512:(g + 1) * 512], ost.t[:], ost, reads=[ost])

                for idx in range(len(pairs) + LOOK):
                    if idx < len(pairs):
                        emit_score(idx)
                    if idx >= LOOK:
                        emit_pv(idx - LOOK)

    def phase_swa(self, l, tiles=range(16)):
        P = self.P
        ps = self.ps
        with P.scope():
            swb = P.sbuf("swb", [128, 12 * 2 * 128], F32)
            P.dma("sp", swb.t[:], self.swb_d.t[:], swb, writes=[swb])
            swv = swb.t[:].rearrange("p (h w q) -> p h w q", w=2, q=128)
            swb8t = P.sbuf("swb8", [128, 12 * 128], F32)
            P.dma("sp", swb8t.t[:], self.flags_d.t[:, 257:257 + 12 * 128], swb8t, writes=[swb8t])
            swb8 = swb8t.t[:].rearrange("p (h q) -> p h q", q=128)
            esk = P.sbuf("esk", [128, 12], F32)
            P.dma("sp", esk.t[:], self.esink_in.t[l], esk, writes=[esk])
            P.op("act", lambda e: e.activation(out=esk.t[:], in_=esk.t[:], func=AF.Exp), reads=[esk], writes=[esk])
            qTs = [P.sbuf(f"sq{i}", [128, 3, S], BF16) for i in range(2)]
            kTs = [P.sbuf(f"sk{i}", [128, S], BF16) for i in range(2)]
            vts = [P.sbuf(f"sv{i}", [128, 16, 128], BF16) for i in range(2)]
            msts = [P.sbuf(f"smst{i}", [128, 3, S], BF16) for i in range(2)]
            tmps = [P.sbuf(f"stmp{i}", [128, 384], F32) for i in range(4)]
            pTs = [P.sbuf(f"spT{i}", [128, 384], BF16) for i in range(4)]
            rec = P.sbuf("srec", [128, 384], F32)
            it = 0
            for kv in range(4):
                q, k, v, mst = qTs[kv % 2], kTs[kv % 2], vts[kv % 2], msts[kv % 2]
                P.dma("sp", q.t[:], self.sqT.t[3 * kv * 128:(3 * kv + 3) * 128, :].rearrange("(g p) t -> p g t", p=128), q, writes=[q])
                P.dma("sp", k.t[:], self.skT.t[kv * 128:(kv + 1) * 128, :], k, writes=[k])
                P.dma("sp", v.t[:], self.sv.t[:, kv * 128:(kv + 1) * 128].rearrange("(t p) c -> p t c", p=128), v, writes=[v])
                parts = []
                for i in tiles:
                    pl = ([(i - 1, 0)] if i > 0 else []) + [(i, 1)]
                    for pi, (kt, which) in enumerate(pl):
                        parts.append((i, kt, which, pi == 0, pi == len(pl) - 1))
                LOOK = 2
                ctx = {}

                def emit_score(idx, q=q, k=k, kv=kv, ctx=ctx):
                    i, kt, which, first, lastp = parts[idx]
                    sb = ps[idx % 4]
                    tmp = tmps[idx % 4]
                    pT = pTs[idx % 4]
                    ctx[idx] = pT
                    qi = q.t[:, :, i * 128:(i + 1) * 128]
                    P.op("pe", lambda e, sb=sb, kt=kt, qi=qi, k=k: e.matmul(sb.t[:, 0:384], lhsT=k.t[:, kt * 128:(kt + 1) * 128], rhs=qi,
                                                                            start=True, stop=True), reads=[k, q], writes=[sb])
                    bias_ap = swb8[:, 3 * kv:3 * kv + 3, :] if (i == 8 and which == 0) else swv[:, 3 * kv:3 * kv + 3, which, :]
                    P.op("dve", lambda e, sb=sb, tmp=tmp, bias_ap=bias_ap: e.tensor_tensor(
                        out=tmp.t[:].rearrange("p (g q) -> p g q", q=128), in0=sb.t[:, 0:384].rearrange("p (g q) -> p g q", q=128),
                        in1=bias_ap, op=ALU.add), reads=[sb, swb, swb8t], writes=[tmp])
                    P.op("act", lambda e, tmp=tmp, pT=pT: e.activation(out=pT.t[:], in_=tmp.t[:], func=AF.Exp, scale=SCALE),
                         reads=[tmp], writes=[pT])

                def emit_pv(idx, v=v, kv=kv, mst=mst, ctx=ctx):
                    i, kt, which, first, lastp = parts[idx]
                    pT = ctx.pop(idx)
                    oT = ps[4 + (i % 2)]
                    den = ps[6 + (i % 2)]
                    P.op("pe", lambda e, oT=oT, kt=kt, pT=pT, first=first, lastp=lastp: e.matmul(
                        oT.t[:, 0:384], lhsT=v.t[:, kt, :], rhs=pT.t[:], start=first, stop=lastp), reads=[v, pT], writes=[oT])
                    P.op("pe", lambda e, den=den, pT=pT, first=first, lastp=lastp: e.matmul(
                        den.t[:, 0:384], lhsT=self.ones_bf.t[:], rhs=pT.t[:], start=first, stop=lastp), reads=[self.ones_bf, pT], writes=[den])
                    if lastp:
                        for g in range(3):
                            P.op("dve", lambda e, g=g, den=den: e.tensor_scalar(
                                out=rec.t[:, g * 128:(g + 1) * 128], in0=den.t[:, g * 128:(g + 1) * 128],
                                scalar1=esk.t[:, 3 * kv + g:3 * kv + g + 1], scalar2=None, op0=ALU.add), reads=[den, esk], writes=[rec])
                        P.op("dve", lambda e: e.reciprocal(out=rec.t[:], in_=rec.t[:]), reads=[rec], writes=[rec])
                        P.op("dve", lambda e, i=i, oT=oT: e.tensor_tensor(
                            out=mst.t[:, :, i * 128:(i + 1) * 128], in0=oT.t[:, 0:384].rearrange("p (g q) -> p g q", q=128),
                            in1=rec.t[:].rearrange("p (g q) -> p g q", q=128), op=ALU.mult), reads=[oT, rec], writes=[mst])

                for idx in range(len(parts) + LOOK):
                    if idx < len(parts):
                        emit_score(idx)
                    if idx >= LOOK:
                        emit_pv(idx - LOOK)
                r0 = (20 + 3 * kv) * 128
                c_lo, c_hi = tiles[0] * 128, (tiles[-1] + 1) * 128
                P.dma("act", self.mixT.t[r0:r0 + 384, c_lo:c_hi].rearrange("(g p) t -> p g t", p=128), mst.t[:, :, c_lo:c_hi], mst, reads=[mst])

    def phase_gla(self, l, a_state_only=False):
        P = self.P
        ps = self.ps
        gn = self.gn[l]
        with P.scope():
            waug = P.sbuf("waug", [17, 512], F32)
            P.dma("sp", waug.t[:], self.w_a_aug.t[l], waug, writes=[waug])
            ga1 = P.sbuf("ga1", [32, S], F32)
            P.op("dve", lambda e: e.memset(ga1.t[:], 1.0), writes=[ga1])
            P.dma("sp", ga1.t[0:16, :], self.gaT.t[:, :], ga1, writes=[ga1])
            state = [P.sbuf(f"gst{h}", [64, 128], F32) for h in range(8)]
            state_bf = [P.sbuf(f"gsb{h}", [64, 128], BF16) for h in range(8)]
            for h in range(8):
                P.op("dve", lambda e, h=h: e.memset(state[h].t[:], 0.0), writes=[state[h]])
                P.op("dve", lambda e, h=h: e.memset(state_bf[h].t[:], 0.0), writes=[state_bf[h]])

            def mk(name, shape, dt, n=2):
                return [P.sbuf(f"{name}{i}", shape, dt) for i in range(n)]
            q_sb = mk("gqs", [128, 512], F32)
            k_sb = mk("gks", [128, 512], F32)
            v_sb = mk("gvs", [128, 1024], BF16)
            sg_sb = mk("gsg", [128, 8, 128], F32)
            e_sb = mk("ges", [128, 512], F32, 1)[0]
            sp_sb = mk("gsp", [128, 512], F32, 1)[0]
            lam_sb = mk("glam", [128, 512], F32)
            elam = mk("gel", [128, 512], F32, 1)[0]
            enlam = mk("gen", [128, 512], F32, 1)[0]
            qe = mk("gqe", [128, 512], F32)
            ke = mk("gke", [128, 512], F32)
            ke_bf = mk("gkeb", [128, 512], BF16)
            E_sb = mk("gE", [64, 16], F32)
            qkT = mk("gqkT", [64, 256], BF16)
            attT = mk("gatt", [128, 128], BF16)
            tmpst = mk("gtmp", [64, 128], F32, 4)
            osq = mk("gosq", [128, 128], BF16)
            ord_ = mk("gord", [128, 128], F32)
            ytmp = mk("gyt", [128, 128], F32)
            yst = mk("gyst", [128, 8, 128], BF16)
            hh = 0
            import os
            GSTOP = int(os.environ.get("MK_GLA_STOP", "9"))
            for t in range(int(os.environ.get("MK_GLA_TILES", "16"))):
                tb = t % 2
                r0 = t * 128
                so = a_state_only and t < 8
                if t == 8:
                    for h in range(8):
                        P.op("dve", lambda e, h=h: e.tensor_scalar(out=state[h].t[:], in0=state[h].t[:], scalar1=self.fcol[0:64, :], scalar2=None, op0=ALU.mult),
                             reads=[state[h], self.flags], writes=[state[h]])
                        P.op("dve", lambda e, h=h: e.tensor_scalar(out=state_bf[h].t[:], in0=state_bf[h].t[:], scalar1=self.fcol[0:64, :], scalar2=None, op0=ALU.mult),
                             reads=[state_bf[h], self.flags], writes=[state_bf[h]])
                if not so:
                    P.dma("sp", q_sb[tb].t[:], self.gq.t[r0:r0 + 128, :], q_sb[tb], writes=[q_sb[tb]])
                    P.dma("sp", sg_sb[tb].t[:], self.sgT.t[:, r0:r0 + 128].rearrange("(h p) t -> p h t", p=128), sg_sb[tb], writes=[sg_sb[tb]])
                P.dma("sp", k_sb[tb].t[:], self.gk.t[r0:r0 + 128, :], k_sb[tb], writes=[k_sb[tb]])
                P.dma("sp", v_sb[tb].t[:], self.gv.t[r0:r0 + 128, :], v_sb[tb], writes=[v_sb[tb]])
                P.op("pe", lambda e, r0=r0: e.matmul(ps[0].t[:], lhsT=ga1.t[0:17, r0:r0 + 128], rhs=waug.t[0:17, :], start=True, stop=True),
                     reads=[ga1, waug], writes=[ps[0]])
                P.op("act", lambda e: e.activation(out=e_sb.t[:], in_=ps[0].t[:], func=AF.Exp, scale=-1.0), reads=[ps[0]], writes=[e_sb])
                P.op("act", lambda e: e.activation(out=sp_sb.t[:], in_=e_sb.t[:], func=AF.Ln, bias=1.0, scale=1.0), reads=[e_sb], writes=[sp_sb])
                if GSTOP <= 0:
                    continue
                SUB = int(os.environ.get("MK_GLA_SUB", "99"))
                lam = lam_sb[tb]
                E = E_sb[tb]
                if SUB >= 1:
                    P.op("pe", lambda e: e.matmul(ps[1].t[:], lhsT=self.ltri, rhs=sp_sb.t[:], start=True, stop=True),
                         reads=[self.consts, sp_sb], writes=[ps[1]])
                if SUB >= 2:
                    P.op("dve", lambda e, lam=lam: e.tensor_copy(out=lam.t[:], in_=ps[1].t[:]), reads=[ps[1]], writes=[lam])
                if SUB >= 3:
                    FV = os.environ.get("MK_GLA_F", "expsb")
                    if FV == "exp":
                        P.op("act", lambda e: e.activation(out=elam.t[:], in_=ps[1].t[:], func=AF.Exp), reads=[ps[1]], writes=[elam])
                    elif FV == "exps":
                        P.op("act", lambda e: e.activation(out=elam.t[:], in_=ps[1].t[:], func=AF.Exp, scale=1.0, bias=0.0), reads=[ps[1]], writes=[elam])
                    elif FV == "copy":
                        P.op("act", lambda e: e.activation(out=elam.t[:], in_=ps[1].t[:], func=AF.Identity), reads=[ps[1]], writes=[elam])
                    elif FV == "toesb":
                        P.op("act", lambda e: e.activation(out=e_sb.t[:], in_=lam.t[:], func=AF.Exp), reads=[lam], writes=[e_sb])
                    elif FV == "dvew":
                        P.op("dve", lambda e: e.memset(elam.t[:], 1.0), writes=[elam])
                    elif FV == "expsb":
                        P.op("act", lambda e, lam=lam: e.activation(out=elam.t[:], in_=lam.t[:], func=AF.Exp), reads=[lam], writes=[elam])
                if SUB >= 4:
                    P.op("act", lambda e, lam=lam: e.activation(out=enlam.t[:], in_=lam.t[:], func=AF.Exp, scale=-1.0), reads=[lam], writes=[enlam])
                if SUB >= 5 and not so:
                    P.op("dve", lambda e, tb=tb: e.scalar_tensor_tensor(out=qe[tb].t[:], in0=q_sb[tb].t[:], scalar=0.125, in1=elam.t[:],
                                                                         op0=ALU.mult, op1=ALU.mult), reads=[q_sb[tb], elam], writes=[qe[tb]])
                if SUB >= 6:
                    P.op("dve", lambda e, tb=tb: e.tensor_tensor(out=ke[tb].t[:], in0=k_sb[tb].t[:], in1=enlam.t[:], op=ALU.mult),
                         reads=[k_sb[tb], enlam], writes=[ke[tb]])
                if SUB >= 7:
                    P.op("pool", lambda e, tb=tb: e.tensor_copy(out=ke_bf[tb].t[:], in_=ke[tb].t[:]), reads=[ke[tb]], writes=[ke_bf[tb]])
                if SUB >= 8:
                    for h in range(8):
                        P.op("pe", lambda e, h=h, lam=lam: e.matmul(ps[2].t[0:64, 2 * h:2 * h + 2], lhsT=lam.t[:, h * 64:(h + 1) * 64], rhs=self.oh2,
                                                                    start=True, stop=True), reads=[lam, self.consts], writes=[ps[2]])
                if SUB >= 9:
                    P.op("act", lambda e, E=E: e.activation(out=E.t[:], in_=ps[2].t[0:64, 0:16], func=AF.Exp), reads=[ps[2]], writes=[E])
                if GSTOP <= 1:
                    continue
                v = v_sb[tb]

                def front(h, hb, tb=tb):
                    qk = qkT[hb]
                    P.op("pe", lambda e, h=h, tb=tb: e.transpose(ps[3].t[0:64, 0:128], qe[tb].t[:, h * 64:(h + 1) * 64], self.ident),
                         reads=[qe[tb], self.consts], writes=[ps[3]])
                    P.op("pe", lambda e, h=h, tb=tb: e.transpose(ps[3].t[0:64, 128:256], ke[tb].t[:, h * 64:(h + 1) * 64], self.ident),
                         reads=[ke[tb], self.consts], writes=[ps[3]])
                    P.op("act", lambda e, qk=qk: e.copy(out=qk.t[:], in_=ps[3].t[0:64, 0:256]), reads=[ps[3]], writes=[qk])
                    P.op("pe", lambda e, qk=qk: e.matmul(ps[4].t[:, 0:128], lhsT=qk.t[:, 128:256], rhs=qk.t[:, 0:128], start=True, stop=True),
                         reads=[qk], writes=[ps[4]])
                    at = attT[hb]
                    P.op("dve", lambda e, at=at: e.tensor_tensor(out=at.t[:], in0=ps[4].t[:, 0:128], in1=self.tri, op=ALU.mult),
                         reads=[ps[4], self.consts], writes=[at])
                    return qk, at

                def half_step(h, hb, qk, half, tb=tb, v=v, E=E):
                    bank = ps[5 + hb]
                    c0 = half * 64
                    if qk is not None:
                        P.op("pe", lambda e, bank=bank, h=h, qk=qk, c0=c0, half=half: e.matmul(
                            bank.t[:, c0:c0 + 64], lhsT=state_bf[h].t[:], rhs=qk.t[:, c0:c0 + 64], start=False, stop=(half == 1)),
                            reads=[state_bf[h], qk], writes=[bank])
                    P.op("pe", lambda e, h=h, tb=tb, v=v, c0=c0: e.matmul(
                        ps[7].t[0:64, 0:128], lhsT=ke_bf[tb].t[c0:c0 + 64, h * 64:(h + 1) * 64], rhs=v.t[c0:c0 + 64, h * 128:(h + 1) * 128],
                        start=True, stop=True), reads=[ke_bf[tb], v], writes=[ps[7]])
                    tm = tmpst[(2 * h + half) % 4]
                    P.op("dve", lambda e, h=h, tm=tm: e.tensor_tensor(out=tm.t[:], in0=ps[7].t[0:64, 0:128], in1=state[h].t[:], op=ALU.add),
                         reads=[ps[7], state[h]], writes=[tm])
                    P.op("dve", lambda e, h=h, tm=tm, E=E, half=half: e.tensor_scalar(
                        out=state_bf[h].t[:], in0=tm.t[:], scalar1=E.t[:, 2 * h + half:2 * h + half + 1], scalar2=None, op0=ALU.mult),
                        reads=[tm, E], writes=[state_bf[h]])
                    P.op("pool", lambda e, h=h, tm=tm, E=E, half=half: e.tensor_scalar(
                        out=state[h].t[:], in0=tm.t[:], scalar1=E.t[:, 2 * h + half:2 * h + half + 1], scalar2=1.0, op0=ALU.mult, op1=ALU.mult),
                        reads=[tm, E], writes=[state[h]])

                def mid(h, hb, qk, at, v=v):
                    bank = ps[5 + hb]
                    P.op("pe", lambda e, bank=bank, v=v, h=h, at=at: e.matmul(bank.t[:, 0:128], lhsT=v.t[:, h * 128:(h + 1) * 128], rhs=at.t[:],
                                                                             start=True, stop=False), reads=[v, at], writes=[bank])
                    half_step(h, hb, qk, 0)

                def back(h, hb, qk, tb=tb):
                    bank = ps[5 + hb]
                    half_step(h, hb, qk, 1)
                    P.op("act", lambda e, bank=bank, hb=hb: e.activation(out=osq[hb].t[:], in_=bank.t[:, 0:128], func=AF.Square),
                         reads=[bank], writes=[osq[hb]])
                    P.op("pe", lambda e, hb=hb: e.matmul(ps[2].t[:, 256:384], lhsT=self.ones_bf.t[:], rhs=osq[hb].t[:], start=True, stop=True),
                         reads=[self.ones_bf, osq[hb]], writes=[ps[2]])
                    P.op("act", lambda e, hb=hb: e.activation(out=ord_[hb].t[:], in_=ps[2].t[:, 256:384], func=AF.Sqrt, bias=EPS, scale=1.0 / 128),
                         reads=[ps[2]], writes=[ord_[hb]])
                    P.op("dve", lambda e, hb=hb: e.reciprocal(out=ord_[hb].t[:], in_=ord_[hb].t[:]), reads=[ord_[hb]], writes=[ord_[hb]])
                    P.op("dve", lambda e, hb=hb, bank=bank: e.scalar_tensor_tensor(out=ytmp[hb].t[:], in0=bank.t[:, 0:128], scalar=gn.t[:, 4:5],
                                                                                     in1=ord_[hb].t[:], op0=ALU.mult, op1=ALU.mult),
                         reads=[bank, ord_[hb], gn], writes=[ytmp[hb]])
                    P.op("pool", lambda e, hb=hb, h=h, tb=tb: e.tensor_tensor(out=yst[tb].t[:, h, :], in0=ytmp[hb].t[:], in1=sg_sb[tb].t[:, h, :], op=ALU.mult),
                         reads=[ytmp[hb], sg_sb[tb]], writes=[yst[tb]])

                if so:
                    for h in range(8):
                        half_step(h, h % 2, None, 0)
                        half_step(h, h % 2, None, 1)
                    continue
                fr = {0: front(0, 0)}
                for h in range(8):
                    hb = h % 2
                    mid(h, hb, *fr[h])
                    if h + 1 < 8:
                        fr[h + 1] = front(h + 1, (h + 1) % 2)
                    back(h, hb, fr[h][0])
                P.dma("act", self.mixT.t[12 * 128:20 * 128, r0:r0 + 128].rearrange("(h p) t -> p h t", p=128), yst[tb].t[:], yst[tb], reads=[yst[tb]])

    def resid_epilogue(self, banks, gcol, xsrc, xdst, t0, c0, ji, xbuf, src_off=0, dst_off=0):
        P = self.P
        ps = self.ps
        yT, xin, xo = self.yT[ji % 2], self.xin[ji % 2], self.xo[ji % 2]
        P.dma("sp", xin.t[:], xsrc.t[t0 - src_off:t0 - src_off + G, c0:c0 + 128].rearrange("(t p) c -> p t c", p=128), xin, reads=[xbuf] if xbuf else [], writes=[xin])
        for hf in range(2):
            P.op("act", lambda e, hf=hf: e.activation(out=yT.t[:, hf * 512:(hf + 1) * 512], in_=banks[hf].t[:], func=AF.Identity, scale=gcol),
                 reads=[banks[hf]] + self.modbufs, writes=[yT])
        for t in range(8):
            P.op("pe", lambda e, t=t: e.transpose(ps[6 + t // 4].t[:, (t % 4) * 128:(t % 4 + 1) * 128], yT.t[:, t * 128:(t + 1) * 128], self.ident),
                 reads=[yT, self.consts], writes=[ps[6 + t // 4]])
        for b2 in range(2):
            P.op("dve", lambda e, b2=b2: e.tensor_tensor(out=xo.t[:, b2 * 4:(b2 + 1) * 4, :], in0=ps[6 + b2].t[:].rearrange("p (t c) -> p t c", c=128),
                                                          in1=xin.t[:, b2 * 4:(b2 + 1) * 4, :], op=ALU.add), reads=[ps[6 + b2], xin], writes=[xo])
        P.dma("act", xdst.t[t0 - dst_off:t0 - dst_off + G, c0:c0 + 128].rearrange("(t p) c -> p t c", p=128), xo.t[:], xo, reads=[xo], writes=[xbuf] if xbuf else [])

    def phase_oproj(self, l, xsrc, xdst, grps):
        P = self.P
        ps = self.ps
        self.modbufs = [self.modT[l], self.gsc[l]]
        w = self.w_out
        for grp in grps:
            t0 = grp * G
            with P.scope():
                self.yT = [P.sbuf(f"yT{i}", [128, G], F32) for i in range(2)]
                self.xin = [P.sbuf(f"xin{i}", [128, 8, 128], F32) for i in range(2)]
                self.xo = [P.sbuf(f"xo{i}", [128, 8, 128], F32) for i in range(2)]
                P.dma("sp", self.hT.t[:], self.mixT.t[:, t0:t0 + G].rearrange("(j p) t -> p j t", p=128), self.hT, writes=[self.hT])
                for j in range(KC):
                    banks = (ps[2 * (j % 3)], ps[2 * (j % 3) + 1])

                    def wsrc(k0, nkk, j=j):
                        return w.t[l, k0 * 128:(k0 + nkk) * 128, j * 128:(j + 1) * 128].rearrange("(k p) c -> p k c", p=128)
                    self.fm_job(banks, wsrc, KC, 128, lambda kg, hf: self.hT.t[:, kg, hf * 512:(hf + 1) * 512], [self.hT])
                    self.resid_epilogue(banks, self.modT[l].t[:, 2, j:j + 1], xsrc, xdst, t0, j * 128, j, None)
            P.barrier()

    def phase_mlp(self, l, xsrc, xdst, grps, dst_off=0):
        P = self.P
        ps = self.ps
        self.modbufs = [self.modT[l], self.gsc[l]]
        NE = 4
        CE = DFF // 128 // NE
        for grp in grps:
            t0 = grp * G
            self.norm_group(xsrc, grp, self.gsc[l].t[:, 1, :], self.modT[l].t[:, 3, :])
            with P.scope():
                h1T = P.sbuf("h1T", [128, CE, G], BF16)
                sqs = [P.sbuf(f"fsq{i}", [128, 512], F32) for i in range(2)]
                self.yT = [P.sbuf(f"yT{i}", [128, G], F32) for i in range(2)]
                self.xin = [P.sbuf(f"xin{i}", [128, 8, 128], F32) for i in range(2)]
                self.xo = [P.sbuf(f"xo{i}", [128, 8, 128], F32) for i in range(2)]
                xcols = [Buf(f"xcol{j}") for j in range(KC)]
                ji = 0
                for ep in range(NE):
                    for c in range(CE):
                        banks = (ps[2 * (ji % 3)], ps[2 * (ji % 3) + 1])
                        ff0 = (ep * CE + c) * 128

                        def wsrc(k0, nkk, ff0=ff0):
                            return self.w_mi.t[l, k0 * 128:(k0 + nkk) * 128, ff0:ff0 + 128].rearrange("(k p) c -> p k c", p=128)
                        self.fm_job(banks, wsrc, KC, 128, lambda kg, hf: self.hT.t[:, kg, hf * 512:(hf + 1) * 512], [self.hT])
                        for hf in range(2):
                            sq = sqs[hf]
                            P.op("act", lambda e, sq=sq, hf=hf, banks=banks: e.activation(out=sq.t[:], in_=banks[hf].t[:], func=AF.Square),
                                 reads=[banks[hf]], writes=[sq])
                            P.op("dve", lambda e, sq=sq, hf=hf, banks=banks, c=c: e.scalar_tensor_tensor(
                                out=h1T.t[:, c, hf * 512:(hf + 1) * 512], in0=banks[hf].t[:], scalar=0.0, in1=sq.t[:], op0=ALU.is_gt, op1=ALU.mult),
                                reads=[banks[hf], sq], writes=[h1T])
                        ji += 1
                    for j in range(KC):
                        banks = (ps[2 * (ji % 3)], ps[2 * (ji % 3) + 1])

                        def wsrc2(k0, nkk, j=j, ep=ep):
                            r0 = (ep * CE + k0) * 128
                            return self.w_mo.t[l, r0:r0 + nkk * 128, j * 128:(j + 1) * 128].rearrange("(k p) c -> p k c", p=128)
                        self.fm_job(banks, wsrc2, CE, 128, lambda kg, hf: h1T.t[:, kg, hf * 512:(hf + 1) * 512], [h1T])
                        self.resid_epilogue(banks, self.modT[l].t[:, 5, j:j + 1], xsrc if ep == 0 else xdst, xdst, t0, j * 128, ji, xcols[j],
                                            src_off=(0 if ep == 0 else dst_off), dst_off=dst_off)
                        ji += 1
            P.barrier()


NCORES = 8


def _make_flags(p, sw):
    fl = np.zeros((128, 257 + 12 * 128), np.float32)
    tt = np.arange(16)[:, None]
    nn = np.arange(8)[None, :]
    gm = np.where(nn >= tt // 2, -1.0e30, 0.0).astype(np.float32)
    sel = (tt >= 8) & (nn < 4)
    fb = np.zeros((16, 8), np.float32)
    if p == 0:
        gm = np.where(sel, -1.0e30, gm).astype(np.float32)
        fb = np.where(sel, -30000.0, 0.0).astype(np.float32)
    fl[:, 0:128] = gm.reshape(1, 128)
    fl[:, 128:256] = fb.reshape(1, 128)
    fl[:, 256] = float(p)
    swp = sw.reshape(128, 12, 2, 128)[:, :, 0, :].copy()
    if p == 0:
        swp = swp - 1.0e10
    fl[:, 257:] = swp.reshape(128, 12 * 128)
    return fl


def _prep(inp, b, p, consts):
    c, m, sw = consts
    d = {}
    xb = inp["x"][b]
    d["x"] = np.ascontiguousarray(xb if p == 1 else np.concatenate([xb[G:], xb[:G]], axis=0))
    d["cT"] = np.ascontiguousarray(inp["c"][b].reshape(32, 128).T)
    for k in ("w_ada", "b_ada", "w_in", "w_out", "w_mlp_in", "w_mlp_out"):
        d[k] = inp[k]
    na = inp["norm_attn"].reshape(DEPTH, 32, 128).transpose(0, 2, 1)
    nm = inp["norm_mlp"].reshape(DEPTH, 32, 128).transpose(0, 2, 1)
    d["normT"] = np.ascontiguousarray(np.concatenate([na, nm], axis=2))
    d["gains"] = np.ascontiguousarray(np.stack([inp["moba_q_norm"], inp["moba_k_norm"], inp["swa_q_norm"], inp["swa_k_norm"],
                                                inp["gla_out_norm"]], axis=2))
    d["w_a_aug"] = np.ascontiguousarray(np.concatenate([inp["gla_w_a"], inp["gla_b_a"][:, None, :]], axis=1))
    d["sinks_rep"] = np.ascontiguousarray(np.broadcast_to(inp["swa_sinks"][:, None, :], (DEPTH, 128, 12)))
    d["consts"] = c
    d["mconsts"] = m
    d["swbias"] = sw
    d["flags"] = _make_flags(p, sw)
    return d


def kernel(**inputs):
    inp = {k: np.asarray(v, dtype=np.float32) for k, v in inputs.items()}
    consts = make_consts()
    K = MK(debug=False, v2=True)
    in_maps = [_prep(inp, i // 2, i % 2, consts) for i in range(NCORES)]
    res = run_bass_kernel_spmd(K.nc, in_maps, core_ids=list(range(NCORES)))
    B = inp["x"].shape[0]
    out = np.empty((B, S, D), np.float32)
    for i in range(NCORES):
        b, p = i // 2, i % 2
        out[b, p * G:(p + 1) * G] = np.asarray(res.results[i]["out"])
    return out
```

```python
import os
import numpy as np
import concourse.bass as bass
import concourse.mybir as mybir
from concourse.bass_utils import run_bass_kernel_spmd
import contextlib

F32 = mybir.dt.float32
BF16 = mybir.dt.bfloat16
AF = mybir.ActivationFunctionType
ALU = mybir.AluOpType
AX = mybir.AxisListType

ENGS = ("pe", "act", "dve", "pool", "sp")
SEM_WRAP = 30000


class Buf:
    __slots__ = ("name", "t", "writers", "readers", "dsem", "dcount")

    def __init__(self, name, t=None):
        self.name = name
        self.t = t
        self.writers = {}
        self.readers = {}
        self.dsem = None
        self.dcount = 0

    def __getitem__(self, idx):
        return self.t[idx]


class Op:
    __slots__ = ("eng", "fn", "deps", "sig", "is_dma", "needs_sig", "dma_sig", "idx")

    def __init__(self, eng, fn, is_dma):
        self.eng = eng
        self.fn = fn
        self.deps = []
        self.sig = None
        self.is_dma = is_dma
        self.needs_sig = False
        self.dma_sig = None
        self.idx = 0


class Prog:
    def __init__(self, nc, same_engine_sync=True):
        self.nc = nc
        self.ops = {e: [] for e in ENGS}
        self.stack = contextlib.ExitStack()
        self.same_engine_sync = same_engine_sync
        self.nsem = 0
        self.n_ops = 0
        self.pending = {e: [] for e in ENGS}
        self.open_dmas = {}
        import os
        self.chain_engs = [x for x in os.environ.get("MK_CHAIN", "act").split(",") if x]
        self.scopes = []
        self.scope_bufs = []
        self.free_dsems = []

    def sem(self, name):
        self.nsem += 1
        return self.stack.enter_context(self.nc.semaphore(name))

    def sbuf(self, name, shape, dt):
        st = self.scopes[-1] if self.scopes else self.stack
        self.nalloc = getattr(self, "nalloc", 0) + 1
        t = st.enter_context(self.nc.sbuf_tensor(f"{name}_{self.nalloc}", list(shape), dt))
        b = Buf(name, t)
        if self.scope_bufs:
            self.scope_bufs[-1].append(b)
        return b

    @contextlib.contextmanager
    def scope(self):
        st = contextlib.ExitStack()
        self.scopes.append(st)
        self.scope_bufs.append([])
        try:
            yield
        finally:
            self.barrier()
            self.scopes.pop()
            for b in self.scope_bufs.pop():
                if b.dsem is not None:
                    self.free_dsems.append((b.dsem, b.dcount))
                    b.dsem = None
            st.close()

    def barrier(self):
        lasts = [self.ops[e][-1] for e in ENGS if self.ops[e]]
        dmas = list(self.open_dmas.values())
        self.open_dmas = {}
        for e in ENGS:
            self.pending[e] = [o for o in lasts if o.eng != e or o.is_dma] + dmas

    def psum(self, name, shape, dt):
        t = self.stack.enter_context(self.nc.psum_tensor(name, list(shape), dt))
        return Buf(name, t)

    def dram(self, name, shape, dt, kind="Internal"):
        t = self.nc.dram_tensor(name, list(shape), dt, kind=kind)
        return Buf(name, t.ap())

    def view(self, name, ap):
        return Buf(name, ap)

    def _track(self, op, reads, writes, key):
        deps = op.deps
        for b in reads:
            for k, w in b.writers.items():
                deps.append(w)
            b.readers[key] = op
        for b in writes:
            if b.readers:
                for k, r in b.readers.items():
                    if r is not op:
                        deps.append(r)
                for k, w in b.writers.items():
                    deps.append(w)
                b.readers = {}
                b.writers = {key: op}
            else:
                for k, w in b.writers.items():
                    if not (op.is_dma and w.is_dma):
                        deps.append(w)
                b.writers[key] = op

    def op(self, eng, fn, reads=(), writes=()):
        o = Op(eng, fn, False)
        if self.pending[eng]:
            o.deps.extend(self.pending[eng])
            self.pending[eng] = []
        self._track(o, reads, writes, eng)
        if eng in self.chain_engs and self.ops[eng]:
            o.deps.append(self.ops[eng][-1])
        self.ops[eng].append(o)
        self.n_ops += 1
        return o

    def dma(self, eng, out_ap, in_ap, sb, reads=(), writes=(), custom=None, **kw):
        o = Op(eng, None, True)
        if sb.dsem is None:
            if self.free_dsems:
                sb.dsem, sb.dcount = self.free_dsems.pop()
            else:
                sb.dsem = self.sem(f"d_{sb.name}_{self.nsem}")
        sb.dcount += 16
        o.dma_sig = (sb.dsem, sb.dcount)
        dsem = sb.dsem

        def fn(e, out_ap=out_ap, in_ap=in_ap, kw=kw, dsem=dsem):
            if custom is not None:
                return custom(e).then_inc(dsem, 16)
            return e.dma_start(out=out_ap, in_=in_ap, **kw).then_inc(dsem, 16)
        o.fn = fn
        if self.pending[eng]:
            o.deps.extend(self.pending[eng])
            self.pending[eng] = []
        self.open_dmas[id(dsem)] = o
        self._track(o, reads, writes, ("dma", id(sb)))
        self.ops[eng].append(o)
        self.n_ops += 1
        return o

    def emit(self, final_waits=()):
        nc = self.nc
        final_waits = list(final_waits) + list(self.pending["sp"])
        for e in ENGS:
            for o in self.ops[e]:
                for d in o.deps:
                    if d.is_dma:
                        continue
                    if d.eng == o.eng and not o.is_dma and (not self.same_engine_sync or d.eng == "pe"):
                        continue
                    d.needs_sig = True
        for o in final_waits:
            if not o.is_dma:
                o.needs_sig = True
        for e in ENGS:
            cur = None
            cnt = 0
            for o in self.ops[e]:
                if o.is_dma or not o.needs_sig:
                    continue
                if cur is None or cnt >= SEM_WRAP:
                    cur = self.sem(f"s_{e}_{self.nsem}")
                    cnt = 0
                cnt += 1
                o.sig = (cur, cnt)
        engmap = {"pe": "tensor", "act": "scalar", "dve": "vector", "pool": "gpsimd", "sp": "sync"}
        with nc.Block() as block:
            for e in ENGS:
                ops = self.ops[e]
                if e == "sp":
                    ops = ops + []
                dec = getattr(block, engmap[e])

                def body(eng, ops=ops, e=e, last=(e == "sp")):
                    waited = {}

                    def wait(sem, val):
                        k = id(sem)
                        if waited.get(k, 0) >= val:
                            return
                        waited[k] = val
                        eng.wait_ge(sem, val)
                    for o in ops:
                        for d in o.deps:
                            if d.is_dma:
                                wait(*d.dma_sig)
                            else:
                                if d.sig is None:
                                    continue
                                wait(*d.sig)
                        ins = o.fn(eng)
                        if o.sig is not None:
                            ins.then_inc(o.sig[0], 1)
                    if last:
                        for o in final_waits:
                            if o.is_dma:
                                wait(*o.dma_sig)
                            else:
                                wait(*o.sig)
                dec(body)
        self.stack.close()


D = 4096
KC = 32
S = 2048
NT = 16
DIN = 10256
DFF = 16384
G = 1024
NGRP = S // G
EPS = 1e-6
DEPTH = 2
SCALE = 128 ** -0.5
BIG = 1.0e9

C_MQ, C_MK, C_MV, C_GQ, C_GK, C_GV, C_GG, C_GA, C_SQ, C_SK, C_SV = (
    0, 1536, 3072, 4608, 5120, 5632, 6656, 7680, 7696, 9232, 9744)

MOBA_SLOPES = [2.0 ** (-8.0 * h / 12) for h in range(1, 13)]
SWA_SLOPES = [2.0 ** (-8.0 * h / 12) for h in range(1, 13)]

CO_ID, CO_ONES, CO_TRI, CO_LTRI, CO_OH2, CO_PAST, CO_GMASK, CO_END = 0, 128, 256, 384, 512, 514, 642, 770
MO_DPL, MO_DBIG, MO_SEL, MO_END = 0, 512, 1408, 2432


def make_consts():
    c = np.zeros((128, CO_END), np.float32)
    c[:, CO_ID:CO_ID + 128] = np.eye(128, dtype=np.float32)
    c[:, CO_ONES:CO_ONES + 128] = 1.0
    s = np.arange(128)[:, None]
    t = np.arange(128)[None, :]
    tri = ((s // 64) == (t // 64)) & (s <= t)
    c[:, CO_TRI:CO_TRI + 128] = tri.astype(np.float32)
    c[:, CO_LTRI:CO_LTRI + 128] = -tri.astype(np.float32) / 16.0
    c[63, CO_OH2] = 1.0
    c[127, CO_OH2 + 1] = 1.0
    tt = np.arange(16)[:, None]
    nn = np.arange(8)[None, :]
    past = ((nn < tt // 2) & (tt >= 8)).astype(np.float32).reshape(1, 128)
    c[:, CO_PAST:CO_PAST + 128] = past
    gm = np.where(nn >= tt // 2, -1.0e30, 0.0).astype(np.float32).reshape(1, 128)
    c[:, CO_GMASK:CO_GMASK + 128] = gm
    m = np.zeros((128, MO_END), np.float32)
    sp = np.arange(128)[:, None].astype(np.float64)
    tp = np.arange(512)[None, :].astype(np.float64)
    m[:, MO_DPL:MO_DPL + 512] = tp - sp
    u = np.arange(896)[None, :].astype(np.float64)
    db = u - 384 - sp
    m[:, MO_DBIG:MO_DBIG + 896] = np.where(db >= 0, db, BIG)
    for n in range(8):
        m[n, MO_SEL + n * 128:MO_SEL + (n + 1) * 128] = 1.0
    sw = np.zeros((128, 12, 2, 128), np.float64)
    s1 = np.arange(128)[:, None]
    t1 = np.arange(128)[None, :]
    dprev = np.where(t1 < s1, 128.0 + t1 - s1, BIG)
    ddiag = np.where(t1 >= s1, (t1 - s1).astype(np.float64), BIG)
    for h in range(12):
        sw[:, h, 0, :] = -SWA_SLOPES[h] / SCALE * dprev
        sw[:, h, 1, :] = -SWA_SLOPES[h] / SCALE * ddiag
    return c, m, sw.reshape(128, 12 * 2 * 128).astype(np.float32)


class MK:
    def __init__(self, debug=False, upto="all", depth=DEPTH, only=None, v2=False):
        self.v2 = v2
        self.only = only
        self.debug = debug
        self.upto = upto
        self.depth = depth
        nc = bass.Bass("TRN2", target_bir_lowering=False)
        self.nc = nc
        P = Prog(nc)
        self.P = P
        self.outs = []
        self.build()

    def din(self, name, shape, dt=F32):
        if self.only is not None and name in ("w_ada", "w_in", "w_out", "w_mlp_in", "w_mlp_out", "x", "b_ada"):
            shape = [2, 2]
        return self.P.dram(name, shape, dt, kind="ExternalInput")

    def dscr(self, name, shape, dt):
        if self.only == "gla" and name in ("gq", "gk", "gv", "sgT", "gaT"):
            return self.P.dram(name, shape, dt, kind="ExternalInput")
        if self.debug:
            self.outs.append(name)
            return self.P.dram(name, shape, dt, kind="ExternalOutput")
        return self.P.dram(name, shape, dt, kind="Internal")

    def wload(self, src_ap, a, b, eng=None):
        P = self.P
        i = self.wi
        self.wi += 1
        st = self.wst[i % len(self.wst)]
        bf = self.wbf[i % len(self.wbf)]
        n = a * b
        stv = st.t[:, 0:n].rearrange("p (a b) -> p a b", b=b)
        bfv = bf.t[:, 0:n].rearrange("p (a b) -> p a b", b=b)
        P.dma("sp", stv, src_ap, st, writes=[st])
        ce = self.cast_engs[i % len(self.cast_engs)]
        if eng is not None:
            ce = eng[i % len(eng)]
        if ce == "act":
            P.op("act", lambda e: e.copy(out=bf.t[:, 0:n], in_=st.t[:, 0:n]), reads=[st], writes=[bf])
        else:
            P.op(ce, lambda e: e.tensor_copy(out=bf.t[:, 0:n], in_=st.t[:, 0:n]), reads=[st], writes=[bf])
        return bf, bfv

    def barrier(self):
        self.P.barrier()

    def build(self):
        P = self.P
        dbg = self.debug
        self.x_in = self.din("x", [S, D])
        self.cT = self.din("cT", [128, KC])
        self.w_ada = self.din("w_ada", [DEPTH, D, 6 * D])
        self.b_ada = self.din("b_ada", [DEPTH, 6 * D])
        self.normT = self.din("normT", [DEPTH, 128, 2 * KC])
        self.w_in = self.din("w_in", [DEPTH, D, DIN])
        self.gains = self.din("gains", [DEPTH, 128, 5])
        self.w_a_aug = self.din("w_a_aug", [DEPTH, 17, 512])
        self.esink_in = self.din("sinks_rep", [DEPTH, 128, 12])
        self.w_out = self.din("w_out", [DEPTH, D, D])
        self.w_mi = self.din("w_mlp_in", [DEPTH, D, DFF])
        self.w_mo = self.din("w_mlp_out", [DEPTH, DFF, D])
        self.consts_d = self.din("consts", [128, CO_END])
        self.mconsts_d = self.din("mconsts", [128, MO_END])
        self.swb_d = self.din("swbias", [128, 12 * 2 * 128])
        self.flags_d = self.din("flags", [128, 128 + 128 + 1 + 12 * 128])
        self.out = P.dram("out", [G if self.v2 else S, D], F32, kind="ExternalOutput")
        self.qmT = self.dscr("qmT", [12 * 128, S], BF16)
        self.kmT = self.dscr("kmT", [12 * 128, S], BF16)
        self.vm = self.dscr("vm", [S, 1536], BF16)
        self.gq = self.dscr("gq", [S, 512], F32)
        self.gk = self.dscr("gk", [S, 512], F32)
        self.gv = self.dscr("gv", [S, 1024], BF16)
        self.sgT = self.dscr("sgT", [8 * 128, S], F32)
        self.gaT = self.dscr("gaT", [16, S], F32)
        self.sqT = self.dscr("sqT", [12 * 128, S], BF16)
        self.skT = self.dscr("skT", [4 * 128, S], BF16)
        self.sv = self.dscr("sv", [S, 512], BF16)
        self.mixT = self.dscr("mixT", [32 * 128, S], BF16)
        self.xa = self.dscr("xa", [S, D], F32)
        self.xb = self.dscr("xb", [S, D], F32)
        if dbg:
            self.modT_d = self.dscr("modT_d", [DEPTH, 128, 6 * KC], F32)
            self.hT_d = self.dscr("hT_d", [KC * 128, G], BF16)
        self.consts = P.sbuf("consts", [128, CO_END], F32)
        self.ones_bf = P.sbuf("ones_bf", [128, 128], BF16)
        self.wst = [P.sbuf(f"wst{i}", [128, 2048], F32) for i in range(3)]
        self.wbf = [P.sbuf(f"wbf{i}", [128, 2048], BF16) for i in range(4)]
        self.wi = 0
        self.cast_engs = ["pool"]
        self.hT = P.sbuf("hT", [128, KC, G], BF16)
        self.modT = [P.sbuf(f"modT{l}", [128, 6, KC], F32) for l in range(DEPTH)]
        self.gsc = [P.sbuf(f"gsc{l}", [128, 2, KC], F32) for l in range(DEPTH)]
        self.gn = [P.sbuf(f"gn{l}", [128, 5], F32) for l in range(DEPTH)]
        self.kmean = P.sbuf("kmean", [128, 12, 8], F32)
        self.ps = [P.psum(f"ps{i}", [128, 512], F32) for i in range(8)]
        c = self.consts
        self.ident = c.t[:, CO_ID:CO_ID + 128]
        self.ones_f = c.t[:, CO_ONES:CO_ONES + 128]
        self.tri = c.t[:, CO_TRI:CO_TRI + 128]
        self.ltri = c.t[:, CO_LTRI:CO_LTRI + 128]
        self.oh2 = c.t[:, CO_OH2:CO_OH2 + 2]
        self.pastm = c.t[:, CO_PAST:CO_PAST + 128]
        self.gmask = c.t[:, CO_GMASK:CO_GMASK + 128]
        self.flags = P.sbuf("flags", [128, 257], F32)
        P.dma("sp", self.flags.t[:], self.flags_d.t[:, 0:257], self.flags, writes=[self.flags])
        self.gmask_p = self.flags.t[:, 0:128]
        self.flagb = self.flags.t[:, 128:256]
        self.fcol = self.flags.t[:, 256:257]

        P.dma("sp", c.t[:], self.consts_d[:], c, writes=[c])
        P.op("dve", lambda e: e.tensor_copy(out=self.ones_bf.t[:], in_=self.ones_f), reads=[c], writes=[self.ones_bf])
        for l in range(DEPTH):
            P.dma("sp", self.gn[l].t[:], self.gains.t[l], self.gn[l], writes=[self.gn[l]])

        if self.only == "gla":
            self.phase_gla(0)
            return self.finish()
        self.phase_mod()
        if self.upto == "mod":
            return self.finish()
        xsrc = self.x_in
        for l in range(self.depth):
            last = (l == self.depth - 1)
            xdst = self.out if last else self.xb
            half = self.v2 and last
            grps = [1] if half else list(range(NGRP))
            self.phase_proj(l, xsrc, a_kv_only=half)
            if self.upto == "proj":
                return self.finish()
            self.phase_moba(l, qgroups=(2, 3) if half else (0, 1, 2, 3))
            self.phase_gla(l, a_state_only=half)
            self.phase_swa(l, tiles=range(8, 16) if half else range(16))
            self.phase_oproj(l, xsrc, self.xa, grps)
            self.phase_mlp(l, self.xa, xdst, grps, dst_off=(G if half else 0))
            xsrc = xdst
        self.finish()

    def finish(self):
        P = self.P
        P.barrier()
        P.emit()

    def phase_mod(self):
        P = self.P
        ps = self.ps
        with P.scope():
            cT = P.sbuf("cT", [128, KC], F32)
            cond = P.sbuf("cond", [128, KC], BF16)
            row = [P.sbuf(f"mrow{i}", [1, 2048], F32) for i in range(2)]
            brow = [P.sbuf(f"brow{i}", [1, 2048], F32) for i in range(2)]
            nT = P.sbuf("nT", [128, 2 * KC], F32)
            g_wst, g_wbf = self.wst, self.wbf
            self.wst = g_wst + [P.sbuf(f"mwst{i}", [128, 2048], F32) for i in range(4)]
            self.wbf = g_wbf + [P.sbuf(f"mwbf{i}", [128, 2048], BF16) for i in range(3)]
            P.dma("sp", cT.t[:], self.cT.t[:], cT, writes=[cT])
            P.op("act", lambda e: e.activation(out=cond.t[:], in_=cT.t[:], func=AF.Silu), reads=[cT], writes=[cond])
            for l in range(self.depth):
                modT = self.modT[l]
                for g in range(12):
                    r = row[g % 2]
                    br = brow[g % 2]
                    P.dma("sp", br.t[:], self.b_ada.t[l:l + 1, g * 2048:(g + 1) * 2048], br, writes=[br])
                    for k in range(KC):
                        src = self.w_ada.t[l, k * 128:(k + 1) * 128, g * 2048:(g + 1) * 2048].rearrange("p (a b) -> p a b", b=512)
                        bf, bv = self.wload(src, 4, 512, eng=("dve", "act", "dve", "act", "pool"))
                        for n in range(4):
                            P.op("pe", lambda e, n=n, k=k, bv=bv: e.matmul(ps[n].t[0:1, :], lhsT=cond.t[:, k:k + 1], rhs=bv[:, n, :],
                                                                              start=(k == 0), stop=(k == KC - 1)),
                                 reads=[cond, bf], writes=[ps[n]])
                    for n in range(4):
                        P.op("dve", lambda e, n=n, r=r, br=br: e.tensor_tensor(out=r.t[0:1, n * 512:(n + 1) * 512], in0=ps[n].t[0:1, :],
                                                                                in1=br.t[0:1, n * 512:(n + 1) * 512], op=ALU.add),
                             reads=[ps[n], br], writes=[r])
                    kind, half = g // 2, g % 2
                    for j in range(16):
                        P.op("pe", lambda e, j=j, r=r: e.matmul(ps[4].t[:, j:j + 1], lhsT=r.t[0:1, j * 128:(j + 1) * 128],
                                                                 rhs=self.ones_f[0:1, 0:1], start=True, stop=True),
                             reads=[r, self.consts], writes=[ps[4]])
                    P.op("dve", lambda e, kind=kind, half=half, modT=modT: e.tensor_copy(out=modT.t[:, kind, half * 16:(half + 1) * 16],
                                                                                      in_=ps[4].t[:, 0:16]),
                         reads=[ps[4]], writes=[modT])
                P.dma("sp", nT.t[:], self.normT.t[l], nT, writes=[nT])
                for w, kind in ((0, 1), (1, 4)):
                    P.op("dve", lambda e, w=w, kind=kind, modT=modT, l=l: e.scalar_tensor_tensor(
                        out=self.gsc[l].t[:, w, :], in0=modT.t[:, kind, :], scalar=1.0, in1=nT.t[:, w * KC:(w + 1) * KC],
                        op0=ALU.add, op1=ALU.mult), reads=[modT, nT], writes=[self.gsc[l]])
                if self.debug:
                    P.dma("act", self.modT_d.t[l], modT.t[:].rearrange("p a b -> p (a b)"), modT, reads=[modT])
            self.wst, self.wbf = g_wst, g_wbf
            self.wi = 0
        P.barrier()

    def norm_group(self, xsrc, grp, gsc_ap, sh_ap):
        P = self.P
        ps = self.ps
        with P.scope():
            xts = [P.sbuf(f"xt{i}", [128, D], F32) for i in range(2)]
            junk = P.sbuf("junk", [128, D], BF16)
            st = P.sbuf("nstat", [128, 3, 8], F32)
            for t in range(G // 128):
                tok0 = grp * G + t * 128
                xt = xts[t % 2]
                P.dma("sp", xt.t[:], xsrc.t[tok0:tok0 + 128, :], xt, writes=[xt])
                P.op("act", lambda e, xt=xt, t=t: e.activation(out=junk.t[:], in_=xt.t[:], func=AF.Square, accum_out=st.t[:, 0, t:t + 1]),
                     reads=[xt], writes=[junk, st])
                P.op("act", lambda e, t=t: e.activation(out=st.t[:, 1, t:t + 1], in_=st.t[:, 0, t:t + 1], func=AF.Sqrt, bias=EPS, scale=1.0 / D),
                     reads=[st], writes=[st])
                P.op("dve", lambda e, t=t: e.reciprocal(out=st.t[:, 2, t:t + 1], in_=st.t[:, 1, t:t + 1]), reads=[st], writes=[st])
                P.op("dve", lambda e, xt=xt, t=t: e.tensor_scalar(out=xt.t[:], in0=xt.t[:], scalar1=st.t[:, 2, t:t + 1], scalar2=None, op0=ALU.mult),
                     reads=[xt, st], writes=[xt])
                for jb in range(8):
                    bank = ps[jb % 2]
                    for q in range(4):
                        j = jb * 4 + q
                        P.op("pe", lambda e, bank=bank, q=q, j=j, xt=xt: e.transpose(bank.t[:, q * 128:(q + 1) * 128], xt.t[:, j * 128:(j + 1) * 128], self.ident),
                             reads=[xt, self.consts], writes=[bank])
                    for q in range(4):
                        j = jb * 4 + q
                        if q % 2 == 0:
                            P.op("act", lambda e, bank=bank, q=q, j=j, t=t: e.activation(
                                out=self.hT.t[:, j, t * 128:(t + 1) * 128], in_=bank.t[:, q * 128:(q + 1) * 128], func=AF.Identity,
                                scale=gsc_ap[:, j:j + 1], bias=sh_ap[:, j:j + 1]), reads=[bank] + self.modbufs, writes=[self.hT])
                        else:
                            P.op("dve", lambda e, bank=bank, q=q, j=j, t=t: e.tensor_scalar(
                                out=self.hT.t[:, j, t * 128:(t + 1) * 128], in0=bank.t[:, q * 128:(q + 1) * 128],
                                scalar1=gsc_ap[:, j:j + 1], scalar2=sh_ap[:, j:j + 1], op0=ALU.mult, op1=ALU.add),
                                reads=[bank] + self.modbufs, writes=[self.hT])

    def fm_job(self, banks, wsrc_fn, nk, ncols, rhs_fn, rhs_bufs):
        P = self.P
        kk_per = 2048 // 128
        k = 0
        while k < nk:
            nkk = min(kk_per, nk - k)
            bf, bv = self.wload(wsrc_fn(k, nkk), nkk, ncols)
            for kk in range(nkk):
                kg = k + kk
                for hf in range(2):
                    P.op("pe", lambda e, hf=hf, kg=kg, kk=kk, bv=bv: e.matmul(banks[hf].t[0:ncols, :], lhsT=bv[:, kk, :], rhs=rhs_fn(kg, hf),
                                                                             start=(kg == 0), stop=(kg == nk - 1)),
                         reads=[bf] + rhs_bufs, writes=[banks[hf]])
            k += nkk
        self.flush_ep()

    def phase_proj(self, l, xsrc, a_kv_only=False):
        P = self.P
        ps = self.ps
        self.modbufs = [self.modT[l], self.gsc[l]]
        gn = self.gn[l]
        jobs = []
        for h in range(12):
            jobs.append(("fmn", C_MQ + h * 128, 128, (self.qmT, h, 0, None)))
        for h in range(12):
            jobs.append(("fmn", C_MK + h * 128, 128, (self.kmT, h, 1, h)))
        for h in range(12):
            jobs.append(("tm", C_MV + h * 128, 128, (self.vm, h * 128, BF16)))
        for j in range(4):
            jobs.append(("tm", C_GQ + j * 128, 128, (self.gq, j * 128, F32)))
        for j in range(4):
            jobs.append(("tm", C_GK + j * 128, 128, (self.gk, j * 128, F32)))
        for j in range(8):
            jobs.append(("tm", C_GV + j * 128, 128, (self.gv, j * 128, BF16)))
        for j in range(8):
            jobs.append(("fms", C_GG + j * 128, 128, (self.sgT, j)))
        jobs.append(("fma", C_GA, 16, None))
        for h in range(12):
            jobs.append(("fmn", C_SQ + h * 128, 128, (self.sqT, h, 2, None)))
        for h in range(4):
            jobs.append(("fmn", C_SK + h * 128, 128, (self.skT, h, 3, None)))
        for j in range(4):
            jobs.append(("tm", C_SV + j * 128, 128, (self.sv, j * 128, BF16)))
        if self.upto == "projq":
            jobs = jobs[:2]
        w = self.w_in
        all_jobs = jobs
        kv_cols = [(C_MK, C_GQ), (C_GK, C_GG), (C_GA, C_SQ), (C_SK, DIN)]
        kv_jobs = [j for j in all_jobs if any(a <= j[1] < b for a, b in kv_cols)]
        for grp in range(NGRP):
            jobs = kv_jobs if (a_kv_only and grp == 0) else all_jobs
            self.norm_group(xsrc, grp, self.gsc[l].t[:, 0, :], self.modT[l].t[:, 0, :])
            if self.debug and grp == 0 and l == 0:
                P.barrier()
                P.dma("act", self.hT_d.t[:].rearrange("(j p) t -> p j t", p=128), self.hT.t[:], self.hT, reads=[self.hT])
            t0 = grp * G
            with P.scope():
                sq = [P.sbuf(f"sq{i}", [128, 512], BF16) for i in range(2)]
                sd = [P.sbuf(f"sd{i}", [128, 512], F32) for i in range(2)]
                stg_bf = [P.sbuf(f"stgb{i}", [128, 1024], BF16) for i in range(2)]
                stg_f = [P.sbuf(f"stgf{i}", [128, 1024], F32) for i in range(2)]
                pending = None
                for ji, (kind, c0, ncols, meta) in enumerate(jobs):
                    banks = (ps[2 * (ji % 3)], ps[2 * (ji % 3) + 1])

                    def wsrc(k0, nkk, c0=c0, ncols=ncols):
                        return w.t[l, k0 * 128:(k0 + nkk) * 128, c0:c0 + ncols].rearrange("(k p) c -> p k c", p=128)
                    if kind != "tm":
                        self.fm_job(banks, wsrc, KC, ncols, lambda kg, hf: self.hT.t[:, kg, hf * 512:(hf + 1) * 512], [self.hT])
                    else:
                        wts = []
                        for k0 in (0, 16):
                            wts.append(self.wload(wsrc(k0, 16), 16, ncols))
                        for t in range(8):
                            for kg in range(KC):
                                bf, bv = wts[kg // 16]
                                P.op("pe", lambda e, t=t, kg=kg, bv=bv, banks=banks: e.matmul(
                                    banks[t // 4].t[:, (t % 4) * 128:(t % 4) * 128 + 128], lhsT=self.hT.t[:, kg, t * 128:(t + 1) * 128],
                                    rhs=bv[:, kg % 16, :], start=(kg == 0), stop=(kg == KC - 1)),
                                    reads=[bf, self.hT], writes=[banks[t // 4]])
                    if pending is not None:
                        pending()
                        pending = None
                    sb = stg_bf[ji % 2]
                    sf = stg_f[ji % 2]
                    if kind == "fmn":
                        dst, h, gi, kmh = meta
                        for hf in range(2):
                            P.op("act", lambda e, hf=hf, banks=banks: e.activation(out=sq[hf].t[:], in_=banks[hf].t[:], func=AF.Square),
                                 reads=[banks[hf]], writes=[sq[hf]])

                        def fin(banks=banks, sb=sb, dst=dst, h=h, gi=gi, kmh=kmh, t0=t0, grp=grp):
                            for hf in range(2):
                                P.op("pe", lambda e, hf=hf: e.matmul(ps[6 + hf].t[:], lhsT=self.ones_bf.t[:], rhs=sq[hf].t[:], start=True, stop=True),
                                     reads=[self.ones_bf, sq[hf]], writes=[ps[6 + hf]])
                                P.op("act", lambda e, hf=hf: e.activation(out=sd[hf].t[:], in_=ps[6 + hf].t[:], func=AF.Sqrt, bias=EPS, scale=1.0 / 128),
                                     reads=[ps[6 + hf]], writes=[sd[hf]])
                                P.op("dve", lambda e, hf=hf: e.reciprocal(out=sd[hf].t[:], in_=sd[hf].t[:]), reads=[sd[hf]], writes=[sd[hf]])
                                P.op("dve", lambda e, hf=hf: e.scalar_tensor_tensor(out=sb.t[:, hf * 512:(hf + 1) * 512], in0=banks[hf].t[:],
                                                                                   scalar=gn.t[:, gi:gi + 1], in1=sd[hf].t[:], op0=ALU.mult, op1=ALU.mult),
                                     reads=[banks[hf], sd[hf], gn], writes=[sb])
                            if kmh is not None:
                                P.op("dve", lambda e: e.tensor_reduce(out=self.kmean.t[:, kmh, grp * 4:(grp + 1) * 4],
                                                                      in_=sb.t[:].rearrange("p (n k) -> p n k", k=256), axis=AX.X, op=ALU.add),
                                     reads=[sb], writes=[self.kmean])
                            P.dma("act", dst.t[h * 128:(h + 1) * 128, t0:t0 + G], sb.t[:], sb, reads=[sb])
                        pending = fin
                    elif kind == "fms":
                        dst, j = meta
                        for hf in range(2):
                            P.op("act", lambda e, hf=hf, banks=banks, sf=sf: e.activation(out=sf.t[:, hf * 512:(hf + 1) * 512], in_=banks[hf].t[:], func=AF.Silu),
                                 reads=[banks[hf]], writes=[sf])
                        P.dma("act", dst.t[j * 128:(j + 1) * 128, t0:t0 + G], sf.t[:], sf, reads=[sf])
                    elif kind == "fma":
                        for hf in range(2):
                            P.op("dve", lambda e, hf=hf, banks=banks, sf=sf: e.tensor_copy(out=sf.t[0:16, hf * 512:(hf + 1) * 512], in_=banks[hf].t[0:16, :]),
                                 reads=[banks[hf]], writes=[sf])
                        P.dma("act", self.gaT.t[:, t0:t0 + G], sf.t[0:16, :], sf, reads=[sf])
                    elif kind == "tm":
                        dst, dc0, dt = meta
                        stg = sb if dt == BF16 else sf
                        for b2 in range(2):
                            eng = "act" if b2 == 0 else "dve"
                            if eng == "act":
                                P.op("act", lambda e, b2=b2, banks=banks, stg=stg: e.copy(out=stg.t[:, b2 * 512:(b2 + 1) * 512], in_=banks[b2].t[:]),
                                     reads=[banks[b2]], writes=[stg])
                            else:
                                P.op("dve", lambda e, b2=b2, banks=banks, stg=stg: e.tensor_copy(out=stg.t[:, b2 * 512:(b2 + 1) * 512], in_=banks[b2].t[:]),
                                     reads=[banks[b2]], writes=[stg])
                        P.dma("act", dst.t[t0:t0 + G, dc0:dc0 + 128].rearrange("(t p) c -> p t c", p=128),
                              stg.t[:].rearrange("p (t c) -> p t c", c=128), stg, reads=[stg])
                if pending is not None:
                    pending()
            P.barrier()

    def phase_moba(self, l, qgroups=(0, 1, 2, 3)):
        P = self.P
        ps = self.ps
        with P.scope():
            mc = P.sbuf("mconst", [128, MO_END], F32)
            P.dma("sp", mc.t[:], self.mconsts_d.t[:], mc, writes=[mc])
            sel_bf = P.sbuf("sel_bf", [8, 1024], BF16)
            P.op("dve", lambda e: e.tensor_copy(out=sel_bf.t[:], in_=mc.t[0:8, MO_SEL:MO_SEL + 1024]), reads=[mc], writes=[sel_bf])
            kmean_bf = P.sbuf("kmean_bf", [128, 12, 8], BF16)
            P.op("dve", lambda e: e.tensor_scalar(out=kmean_bf.t[:], in0=self.kmean.t[:], scalar1=1.0 / 256, scalar2=None, op0=ALU.mult),
                 reads=[self.kmean], writes=[kmean_bf])
            qT = [P.sbuf(f"mq{i}", [128, S], BF16) for i in range(2)]
            kT = [P.sbuf(f"mk{i}", [128, S], BF16) for i in range(2)]
            vt = [P.sbuf(f"mv{i}", [128, 16, 128], BF16) for i in range(2)]
            gate = P.sbuf("gate", [128, 128], F32)
            top8 = P.sbuf("top8", [128, 8, 8], F32)
            mb = P.sbuf("mbias", [128, 128], F32)
            maskT = P.sbuf("maskT", [8, 1024], BF16)
            pTs = [P.sbuf(f"pT{i}", [128, 512], BF16) for i in range(4)]
            tmps = [P.sbuf(f"mtmp{i}", [128, 512], F32) for i in range(4)]
            rec = P.sbuf("mrec", [128, 512], F32)
            osts = [P.sbuf(f"most{i}", [128, 512], BF16) for i in range(2)]
            P.op("dve", lambda e: e.memset(mb.t[:], 0.0), writes=[mb])
            it = 0
            for h in range(12):
                q, k, v = qT[h % 2], kT[h % 2], vt[h % 2]
                P.dma("sp", q.t[:], self.qmT.t[h * 128:(h + 1) * 128, :], q, writes=[q])
                P.dma("sp", k.t[:], self.kmT.t[h * 128:(h + 1) * 128, :], k, writes=[k])
                P.dma("sp", v.t[:], self.vm.t[:, h * 128:(h + 1) * 128].rearrange("(t p) c -> p t c", p=128), v, writes=[v])
                for t in range(16):
                    P.op("pe", lambda e, t=t, q=q, h=h: e.matmul(ps[7].t[:, t * 8:(t + 1) * 8], lhsT=q.t[:, t * 128:(t + 1) * 128],
                                                                 rhs=kmean_bf.t[:, h, :], start=True, stop=True),
                         reads=[q, kmean_bf], writes=[ps[7]])
                P.op("dve", lambda e: e.tensor_tensor(out=gate.t[:], in0=ps[7].t[:, 0:128], in1=self.gmask_p, op=ALU.add),
                     reads=[ps[7], self.flags], writes=[gate])
                for t in range(8, 16):
                    P.op("dve", lambda e, t=t: e.max(out=top8.t[:, t - 8, :], in_=gate.t[:, t * 8:(t + 1) * 8]), reads=[gate], writes=[top8])
                    P.op("dve", lambda e, t=t: e.tensor_scalar(out=mb.t[:, t * 8:(t + 1) * 8], in0=gate.t[:, t * 8:(t + 1) * 8],
                                                              scalar1=top8.t[:, t - 8, 2:3], scalar2=-30000.0, op0=ALU.is_lt, op1=ALU.mult),
                         reads=[gate, top8], writes=[mb])
                P.op("dve", lambda e: e.tensor_tensor(out=mb.t[:], in0=mb.t[:], in1=self.pastm, op=ALU.mult), reads=[mb, self.consts], writes=[mb])
                P.op("dve", lambda e: e.tensor_tensor(out=mb.t[:], in0=mb.t[:], in1=self.flagb, op=ALU.add), reads=[mb, self.flags], writes=[mb])
                for r in range(2):
                    for qq in range(4):
                        t = 8 + r * 4 + qq
                        P.op("pe", lambda e, t=t, qq=qq: e.transpose(ps[6].t[0:8, qq * 128:(qq + 1) * 128], mb.t[:, t * 8:(t + 1) * 8], self.ident),
                             reads=[mb, self.consts], writes=[ps[6]])
                    P.op("act", lambda e, r=r: e.copy(out=maskT.t[:, r * 512:(r + 1) * 512], in_=ps[6].t[0:8, :]), reads=[ps[6]], writes=[maskT])
                slope = MOBA_SLOPES[h]
                pairs = [(g, kt) for g in qgroups for kt in range(4 * g + 4)]
                LOOK = 3
                ctx = {}

                def emit_score(idx, h=h, q=q, k=k, slope=slope, ctx=ctx):
                    g, kt = pairs[idx]
                    sb = ps[2 + (idx % 4)]
                    tmp = tmps[idx % 4]
                    pT = pTs[idx % 4]
                    ctx[idx] = pT
                    n = kt // 2
                    need_mask = (g >= 2) and (n <= 2 * g)
                    P.op("pe", lambda e, sb=sb, kt=kt, g=g, k=k, q=q, need_mask=need_mask: e.matmul(
                        sb.t[:], lhsT=k.t[:, kt * 128:(kt + 1) * 128], rhs=q.t[:, g * 512:(g + 1) * 512], start=True, stop=not need_mask),
                        reads=[k, q], writes=[sb])
                    if need_mask:
                        P.op("pe", lambda e, sb=sb, n=n, g=g: e.matmul(sb.t[:], lhsT=sel_bf.t[0:8, n * 128:(n + 1) * 128],
                                                                       rhs=maskT.t[0:8, (g - 2) * 512:(g - 1) * 512], start=False, stop=True),
                             reads=[sel_bf, maskT], writes=[sb])
                    j = kt - 4 * g
                    if j >= 0:
                        c0 = MO_DBIG + 384 - 128 * j
                        delta = 0.0
                    else:
                        c0 = MO_DPL
                        delta = float(512 * g - 128 * kt)
                    P.op("dve", lambda e, tmp=tmp, sb=sb, c0=c0, slope=slope: e.scalar_tensor_tensor(
                        out=tmp.t[:], in0=mc.t[:, c0:c0 + 512], scalar=-slope / SCALE, in1=sb.t[:], op0=ALU.mult, op1=ALU.add),
                        reads=[mc, sb], writes=[tmp])
                    P.op("act", lambda e, tmp=tmp, pT=pT, slope=slope, delta=delta: e.activation(
                        out=pT.t[:], in_=tmp.t[:], func=AF.Exp, scale=SCALE, bias=-slope * delta), reads=[tmp], writes=[pT])

                def emit_pv(idx, h=h, v=v, ctx=ctx):
                    g, kt = pairs[idx]
                    nkt = 4 * g + 4
                    pT = ctx.pop(idx)
                    oT, den = ps[0], ps[1]
                    P.op("pe", lambda e, kt=kt, pT=pT, nkt=nkt: e.matmul(oT.t[:], lhsT=v.t[:, kt, :], rhs=pT.t[:],
                                                                        start=(kt == 0), stop=(kt == nkt - 1)),
                         reads=[v, pT], writes=[oT])
                    P.op("pe", lambda e, kt=kt, pT=pT, nkt=nkt: e.matmul(den.t[:], lhsT=self.ones_bf.t[:], rhs=pT.t[:],
                                                                        start=(kt == 0), stop=(kt == nkt - 1)),
                         reads=[self.ones_bf, pT], writes=[den])
                    if kt == nkt - 1:
                        ost = osts[g % 2]
                        P.op("dve", lambda e: e.reciprocal(out=rec.t[:], in_=den.t[:]), reads=[den], writes=[rec])
                        P.op("dve", lambda e, ost=ost: e.tensor_tensor(out=ost.t[:], in0=oT.t[:], in1=rec.t[:], op=ALU.mult),
                             reads=[oT, rec], writes=[ost])
                        P.dma("act", self.mixT.t[h * 128:(h + 1) * 128, g * 512:(g + 1) * 512], ost.t[:], ost, reads=[ost])

                for idx in range(len(pairs) + LOOK):
                    if idx < len(pairs):
                        emit_score(idx)
                    if idx >= LOOK:
                        emit_pv(idx - LOOK)

    def phase_swa(self, l, tiles=range(16)):
        P = self.P
        ps = self.ps
        with P.scope():
            swb = P.sbuf("swb", [128, 12 * 2 * 128], F32)
            P.dma("sp", swb.t[:], self.swb_d.t[:], swb, writes=[swb])
            swv = swb.t[:].rearrange("p (h w q) -> p h w q", w=2, q=128)
            swb8t = P.sbuf("swb8", [128, 12 * 128], F32)
            P.dma("sp", swb8t.t[:], self.flags_d.t[:, 257:257 + 12 * 128], swb8t, writes=[swb8t])
            swb8 = swb8t.t[:].rearrange("p (h q) -> p h q", q=128)
            esk = P.sbuf("esk", [128, 12], F32)
            P.dma("sp", esk.t[:], self.esink_in.t[l], esk, writes=[esk])
            P.op("act", lambda e: e.activation(out=esk.t[:], in_=esk.t[:], func=AF.Exp), reads=[esk], writes=[esk])
            qTs = [P.sbuf(f"sq{i}", [128, 3, S], BF16) for i in range(2)]
            kTs = [P.sbuf(f"sk{i}", [128, S], BF16) for i in range(2)]
            vts = [P.sbuf(f"sv{i}", [128, 16, 128], BF16) for i in range(2)]
            msts = [P.sbuf(f"smst{i}", [128, 3, S], BF16) for i in range(2)]
            tmps = [P.sbuf(f"stmp{i}", [128, 384], F32) for i in range(4)]
            pTs = [P.sbuf(f"spT{i}", [128, 384], BF16) for i in range(4)]
            rec = P.sbuf("srec", [128, 384], F32)
            it = 0
            for kv in range(4):
                q, k, v, mst = qTs[kv % 2], kTs[kv % 2], vts[kv % 2], msts[kv % 2]
                P.dma("sp", q.t[:], self.sqT.t[3 * kv * 128:(3 * kv + 3) * 128, :].rearrange("(g p) t -> p g t", p=128), q, writes=[q])
                P.dma("sp", k.t[:], self.skT.t[kv * 128:(kv + 1) * 128, :], k, writes=[k])
                P.dma("sp", v.t[:], self.sv.t[:, kv * 128:(kv + 1) * 128].rearrange("(t p) c -> p t c", p=128), v, writes=[v])
                parts = []
                for i in tiles:
                    pl = ([(i - 1, 0)] if i > 0 else []) + [(i, 1)]
                    for pi, (kt, which) in enumerate(pl):
                        parts.append((i, kt, which, pi == 0, pi == len(pl) - 1))
                LOOK = 2
                ctx = {}

                def emit_score(idx, q=q, k=k, kv=kv, ctx=ctx):
                    i, kt, which, first, lastp = parts[idx]
                    sb = ps[idx % 4]
                    tmp = tmps[idx % 4]
                    pT = pTs[idx % 4]
                    ctx[idx] = pT
                    qi = q.t[:, :, i * 128:(i + 1) * 128]
                    P.op("pe", lambda e, sb=sb, kt=kt, qi=qi, k=k: e.matmul(sb.t[:, 0:384], lhsT=k.t[:, kt * 128:(kt + 1) * 128], rhs=qi,
                                                                            start=True, stop=True), reads=[k, q], writes=[sb])
                    bias_ap = swb8[:, 3 * kv:3 * kv + 3, :] if (i == 8 and which == 0) else swv[:, 3 * kv:3 * kv + 3, which, :]
                    P.op("dve", lambda e, sb=sb, tmp=tmp, bias_ap=bias_ap: e.tensor_tensor(
                        out=tmp.t[:].rearrange("p (g q) -> p g q", q=128), in0=sb.t[:, 0:384].rearrange("p (g q) -> p g q", q=128),
                        in1=bias_ap, op=ALU.add), reads=[sb, swb, swb8t], writes=[tmp])
                    P.op("act", lambda e, tmp=tmp, pT=pT: e.activation(out=pT.t[:], in_=tmp.t[:], func=AF.Exp, scale=SCALE),
                         reads=[tmp], writes=[pT])

                def emit_pv(idx, v=v, kv=kv, mst=mst, ctx=ctx):
                    i, kt, which, first, lastp = parts[idx]
                    pT = ctx.pop(idx)
                    oT = ps[4 + (i % 2)]
                    den = ps[6 + (i % 2)]
                    P.op("pe", lambda e, oT=oT, kt=kt, pT=pT, first=first, lastp=lastp: e.matmul(
                        oT.t[:, 0:384], lhsT=v.t[:, kt, :], rhs=pT.t[:], start=first, stop=lastp), reads=[v, pT], writes=[oT])
                    P.op("pe", lambda e, den=den, pT=pT, first=first, lastp=lastp: e.matmul(
                        den.t[:, 0:384], lhsT=self.ones_bf.t[:], rhs=pT.t[:], start=first, stop=lastp), reads=[self.ones_bf, pT], writes=[den])
                    if lastp:
                        for g in range(3):
                            P.op("dve", lambda e, g=g, den=den: e.tensor_scalar(
                                out=rec.t[:, g * 128:(g + 1) * 128], in0=den.t[:, g * 128:(g + 1) * 128],
                                scalar1=esk.t[:, 3 * kv + g:3 * kv + g + 1], scalar2=None, op0=ALU.add), reads=[den, esk], writes=[rec])
                        P.op("dve", lambda e: e.reciprocal(out=rec.t[:], in_=rec.t[:]), reads=[rec], writes=[rec])
                        P.op("dve", lambda e, i=i, oT=oT: e.tensor_tensor(
                            out=mst.t[:, :, i * 128:(i + 1) * 128], in0=oT.t[:, 0:384].rearrange("p (g q) -> p g q", q=128),
                            in1=rec.t[:].rearrange("p (g q) -> p g q", q=128), op=ALU.mult), reads=[oT, rec], writes=[mst])

                for idx in range(len(parts) + LOOK):
                    if idx < len(parts):
                        emit_score(idx)
                    if idx >= LOOK:
                        emit_pv(idx - LOOK)
                r0 = (20 + 3 * kv) * 128
                c_lo, c_hi = tiles[0] * 128, (tiles[-1] + 1) * 128
                P.dma("act", self.mixT.t[r0:r0 + 384, c_lo:c_hi].rearrange("(g p) t -> p g t", p=128), mst.t[:, :, c_lo:c_hi], mst, reads=[mst])

    def phase_gla(self, l, a_state_only=False):
        P = self.P
        ps = self.ps
        gn = self.gn[l]
        with P.scope():
            waug = P.sbuf("waug", [17, 512], F32)
            P.dma("sp", waug.t[:], self.w_a_aug.t[l], waug, writes=[waug])
            ga1 = P.sbuf("ga1", [32, S], F32)
            P.op("dve", lambda e: e.memset(ga1.t[:], 1.0), writes=[ga1])
            P.dma("sp", ga1.t[0:16, :], self.gaT.t[:, :], ga1, writes=[ga1])
            state = [P.sbuf(f"gst{h}", [64, 128], F32) for h in range(8)]
            state_bf = [P.sbuf(f"gsb{h}", [64, 128], BF16) for h in range(8)]
            for h in range(8):
                P.op("dve", lambda e, h=h: e.memset(state[h].t[:], 0.0), writes=[state[h]])
                P.op("dve", lambda e, h=h: e.memset(state_bf[h].t[:], 0.0), writes=[state_bf[h]])

            def mk(name, shape, dt, n=2):
                return [P.sbuf(f"{name}{i}", shape, dt) for i in range(n)]
            q_sb = mk("gqs", [128, 512], F32)
            k_sb = mk("gks", [128, 512], F32)
            v_sb = mk("gvs", [128, 1024], BF16)
            sg_sb = mk("gsg", [128, 8, 128], F32)
            e_sb = mk("ges", [128, 512], F32, 1)[0]
            sp_sb = mk("gsp", [128, 512], F32, 1)[0]
            lam_sb = mk("glam", [128, 512], F32)
            elam = mk("gel", [128, 512], F32, 1)[0]
            enlam = mk("gen", [128, 512], F32, 1)[0]
            qe = mk("gqe", [128, 512], F32)
            ke = mk("gke", [128, 512], F32)
            ke_bf = mk("gkeb", [128, 512], BF16)
            E_sb = mk("gE", [64, 16], F32)
            qkT = mk("gqkT", [64, 256], BF16)
            attT = mk("gatt", [128, 128], BF16)
            tmpst = mk("gtmp", [64, 128], F32, 4)
            osq = mk("gosq", [128, 128], BF16)
            ord_ = mk("gord", [128, 128], F32)
            ytmp = mk("gyt", [128, 128], F32)
            yst = mk("gyst", [128, 8, 128], BF16)
            hh = 0
            import os
            GSTOP = int(os.environ.get("MK_GLA_STOP", "9"))
            for t in range(int(os.environ.get("MK_GLA_TILES", "16"))):
                tb = t % 2
                r0 = t * 128
                so = a_state_only and t < 8
                if t == 8:
                    for h in range(8):
                        P.op("dve", lambda e, h=h: e.tensor_scalar(out=state[h].t[:], in0=state[h].t[:], scalar1=self.fcol[0:64, :], scalar2=None, op0=ALU.mult),
                             reads=[state[h], self.flags], writes=[state[h]])
                        P.op("dve", lambda e, h=h: e.tensor_scalar(out=state_bf[h].t[:], in0=state_bf[h].t[:], scalar1=self.fcol[0:64, :], scalar2=None, op0=ALU.mult),
                             reads=[state_bf[h], self.flags], writes=[state_bf[h]])
                if not so:
                    P.dma("sp", q_sb[tb].t[:], self.gq.t[r0:r0 + 128, :], q_sb[tb], writes=[q_sb[tb]])
                    P.dma("sp", sg_sb[tb].t[:], self.sgT.t[:, r0:r0 + 128].rearrange("(h p) t -> p h t", p=128), sg_sb[tb], writes=[sg_sb[tb]])
                P.dma("sp", k_sb[tb].t[:], self.gk.t[r0:r0 + 128, :], k_sb[tb], writes=[k_sb[tb]])
                P.dma("sp", v_sb[tb].t[:], self.gv.t[r0:r0 + 128, :], v_sb[tb], writes=[v_sb[tb]])
                P.op("pe", lambda e, r0=r0: e.matmul(ps[0].t[:], lhsT=ga1.t[0:17, r0:r0 + 128], rhs=waug.t[0:17, :], start=True, stop=True),
                     reads=[ga1, waug], writes=[ps[0]])
                P.op("act", lambda e: e.activation(out=e_sb.t[:], in_=ps[0].t[:], func=AF.Exp, scale=-1.0), reads=[ps[0]], writes=[e_sb])
                P.op("act", lambda e: e.activation(out=sp_sb.t[:], in_=e_sb.t[:], func=AF.Ln, bias=1.0, scale=1.0), reads=[e_sb], writes=[sp_sb])
                if GSTOP <= 0:
                    continue
                SUB = int(os.environ.get("MK_GLA_SUB", "99"))
                lam = lam_sb[tb]
                E = E_sb[tb]
                if SUB >= 1:
                    P.op("pe", lambda e: e.matmul(ps[1].t[:], lhsT=self.ltri, rhs=sp_sb.t[:], start=True, stop=True),
                         reads=[self.consts, sp_sb], writes=[ps[1]])
                if SUB >= 2:
                    P.op("dve", lambda e, lam=lam: e.tensor_copy(out=lam.t[:], in_=ps[1].t[:]), reads=[ps[1]], writes=[lam])
                if SUB >= 3:
                    FV = os.environ.get("MK_GLA_F", "expsb")
                    if FV == "exp":
                        P.op("act", lambda e: e.activation(out=elam.t[:], in_=ps[1].t[:], func=AF.Exp), reads=[ps[1]], writes=[elam])
                    elif FV == "exps":
                        P.op("act", lambda e: e.activation(out=elam.t[:], in_=ps[1].t[:], func=AF.Exp, scale=1.0, bias=0.0), reads=[ps[1]], writes=[elam])
                    elif FV == "copy":
                        P.op("act", lambda e: e.activation(out=elam.t[:], in_=ps[1].t[:], func=AF.Identity), reads=[ps[1]], writes=[elam])
                    elif FV == "toesb":
                        P.op("act", lambda e: e.activation(out=e_sb.t[:], in_=lam.t[:], func=AF.Exp), reads=[lam], writes=[e_sb])
                    elif FV == "dvew":
                        P.op("dve", lambda e: e.memset(elam.t[:], 1.0), writes=[elam])
                    elif FV == "expsb":
                        P.op("act", lambda e, lam=lam: e.activation(out=elam.t[:], in_=lam.t[:], func=AF.Exp), reads=[lam], writes=[elam])
                if SUB >= 4:
                    P.op("act", lambda e, lam=lam: e.activation(out=enlam.t[:], in_=lam.t[:], func=AF.Exp, scale=-1.0), reads=[lam], writes=[enlam])
                if SUB >= 5 and not so:
                    P.op("dve", lambda e, tb=tb: e.scalar_tensor_tensor(out=qe[tb].t[:], in0=q_sb[tb].t[:], scalar=0.125, in1=elam.t[:],
                                                                         op0=ALU.mult, op1=ALU.mult), reads=[q_sb[tb], elam], writes=[qe[tb]])
                if SUB >= 6:
                    P.op("dve", lambda e, tb=tb: e.tensor_tensor(out=ke[tb].t[:], in0=k_sb[tb].t[:], in1=enlam.t[:], op=ALU.mult),
                         reads=[k_sb[tb], enlam], writes=[ke[tb]])
                if SUB >= 7:
                    P.op("pool", lambda e, tb=tb: e.tensor_copy(out=ke_bf[tb].t[:], in_=ke[tb].t[:]), reads=[ke[tb]], writes=[ke_bf[tb]])
                if SUB >= 8:
                    for h in range(8):
                        P.op("pe", lambda e, h=h, lam=lam: e.matmul(ps[2].t[0:64, 2 * h:2 * h + 2], lhsT=lam.t[:, h * 64:(h + 1) * 64], rhs=self.oh2,
                                                                    start=True, stop=True), reads=[lam, self.consts], writes=[ps[2]])
                if SUB >= 9:
                    P.op("act", lambda e, E=E: e.activation(out=E.t[:], in_=ps[2].t[0:64, 0:16], func=AF.Exp), reads=[ps[2]], writes=[E])
                if GSTOP <= 1:
                    continue
                v = v_sb[tb]

                def front(h, hb, tb=tb):
                    qk = qkT[hb]
                    P.op("pe", lambda e, h=h, tb=tb: e.transpose(ps[3].t[0:64, 0:128], qe[tb].t[:, h * 64:(h + 1) * 64], self.ident),
                         reads=[qe[tb], self.consts], writes=[ps[3]])
                    P.op("pe", lambda e, h=h, tb=tb: e.transpose(ps[3].t[0:64, 128:256], ke[tb].t[:, h * 64:(h + 1) * 64], self.ident),
                         reads=[ke[tb], self.consts], writes=[ps[3]])
                    P.op("act", lambda e, qk=qk: e.copy(out=qk.t[:], in_=ps[3].t[0:64, 0:256]), reads=[ps[3]], writes=[qk])
                    P.op("pe", lambda e, qk=qk: e.matmul(ps[4].t[:, 0:128], lhsT=qk.t[:, 128:256], rhs=qk.t[:, 0:128], start=True, stop=True),
                         reads=[qk], writes=[ps[4]])
                    at = attT[hb]
                    P.op("dve", lambda e, at=at: e.tensor_tensor(out=at.t[:], in0=ps[4].t[:, 0:128], in1=self.tri, op=ALU.mult),
                         reads=[ps[4], self.consts], writes=[at])
                    return qk, at

                def half_step(h, hb, qk, half, tb=tb, v=v, E=E):
                    bank = ps[5 + hb]
                    c0 = half * 64
                    if qk is not None:
                        P.op("pe", lambda e, bank=bank, h=h, qk=qk, c0=c0, half=half: e.matmul(
                            bank.t[:, c0:c0 + 64], lhsT=state_bf[h].t[:], rhs=qk.t[:, c0:c0 + 64], start=False, stop=(half == 1)),
                            reads=[state_bf[h], qk], writes=[bank])
                    P.op("pe", lambda e, h=h, tb=tb, v=v, c0=c0: e.matmul(
                        ps[7].t[0:64, 0:128], lhsT=ke_bf[tb].t[c0:c0 + 64, h * 64:(h + 1) * 64], rhs=v.t[c0:c0 + 64, h * 128:(h + 1) * 128],
                        start=True, stop=True), reads=[ke_bf[tb], v], writes=[ps[7]])
                    tm = tmpst[(2 * h + half) % 4]
                    P.op("dve", lambda e, h=h, tm=tm: e.tensor_tensor(out=tm.t[:], in0=ps[7].t[0:64, 0:128], in1=state[h].t[:], op=ALU.add),
                         reads=[ps[7], state[h]], writes=[tm])
                    P.op("dve", lambda e, h=h, tm=tm, E=E, half=half: e.tensor_scalar(
                        out=state_bf[h].t[:], in0=tm.t[:], scalar1=E.t[:, 2 * h + half:2 * h + half + 1], scalar2=None, op0=ALU.mult),
                        reads=[tm, E], writes=[state_bf[h]])
                    P.op("pool", lambda e, h=h, tm=tm, E=E, half=half: e.tensor_scalar(
                        out=state[h].t[:], in0=tm.t[:], scalar1=E.t[:, 2 * h + half:2 * h + half + 1], scalar2=1.0, op0=ALU.mult, op1=ALU.mult),
                        reads=[tm, E], writes=[state[h]])

                def mid(h, hb, qk, at, v=v):
                    bank = ps[5 + hb]
                    P.op("pe", lambda e, bank=bank, v=v, h=h, at=at: e.matmul(bank.t[:, 0:128], lhsT=v.t[:, h * 128:(h + 1) * 128], rhs=at.t[:],
                                                                             start=True, stop=False), reads=[v, at], writes=[bank])
                    half_step(h, hb, qk, 0)

                def back(h, hb, qk, tb=tb):
                    bank = ps[5 + hb]
                    half_step(h, hb, qk, 1)
                    P.op("act", lambda e, bank=bank, hb=hb: e.activation(out=osq[hb].t[:], in_=bank.t[:, 0:128], func=AF.Square),
                         reads=[bank], writes=[osq[hb]])
                    P.op("pe", lambda e, hb=hb: e.matmul(ps[2].t[:, 256:384], lhsT=self.ones_bf.t[:], rhs=osq[hb].t[:], start=True, stop=True),
                         reads=[self.ones_bf, osq[hb]], writes=[ps[2]])
                    P.op("act", lambda e, hb=hb: e.activation(out=ord_[hb].t[:], in_=ps[2].t[:, 256:384], func=AF.Sqrt, bias=EPS, scale=1.0 / 128),
                         reads=[ps[2]], writes=[ord_[hb]])
                    P.op("dve", lambda e, hb=hb: e.reciprocal(out=ord_[hb].t[:], in_=ord_[hb].t[:]), reads=[ord_[hb]], writes=[ord_[hb]])
                    P.op("dve", lambda e, hb=hb, bank=bank: e.scalar_tensor_tensor(out=ytmp[hb].t[:], in0=bank.t[:, 0:128], scalar=gn.t[:, 4:5],
                                                                                     in1=ord_[hb].t[:], op0=ALU.mult, op1=ALU.mult),
                         reads=[bank, ord_[hb], gn], writes=[ytmp[hb]])
                    P.op("pool", lambda e, hb=hb, h=h, tb=tb: e.tensor_tensor(out=yst[tb].t[:, h, :], in0=ytmp[hb].t[:], in1=sg_sb[tb].t[:, h, :], op=ALU.mult),
                         reads=[ytmp[hb], sg_sb[tb]], writes=[yst[tb]])

                if so:
                    for h in range(8):
                        half_step(h, h % 2, None, 0)
                        half_step(h, h % 2, None, 1)
                    continue
                fr = {0: front(0, 0)}
                for h in range(8):
                    hb = h % 2
                    mid(h, hb, *fr[h])
                    if h + 1 < 8:
                        fr[h + 1] = front(h + 1, (h + 1) % 2)
                    back(h, hb, fr[h][0])
                P.dma("act", self.mixT.t[12 * 128:20 * 128, r0:r0 + 128].rearrange("(h p) t -> p h t", p=128), yst[tb].t[:], yst[tb], reads=[yst[tb]])

    def resid_epilogue(self, banks, gcol, xsrc, xdst, t0, c0, ji, xbuf, src_off=0, dst_off=0):
        P = self.P
        ps = self.ps
        yT, xin, xo = self.yT[ji % 2], self.xin[ji % 2], self.xo[ji % 2]
        P.dma("sp", xin.t[:], xsrc.t[t0 - src_off:t0 - src_off + G, c0:c0 + 128].rearrange("(t p) c -> p t c", p=128), xin, reads=[xbuf] if xbuf else [], writes=[xin])
        for hf in range(2):
            P.op("act", lambda e, hf=hf: e.activation(out=yT.t[:, hf * 512:(hf + 1) * 512], in_=banks[hf].t[:], func=AF.Identity, scale=gcol),
                 reads=[banks[hf]] + self.modbufs, writes=[yT])
        def part_b(yT=yT, xin=xin, xo=xo):
            for t in range(8):
                P.op("pe", lambda e, t=t: e.transpose(ps[6 + t // 4].t[:, (t % 4) * 128:(t % 4 + 1) * 128], yT.t[:, t * 128:(t + 1) * 128], self.ident),
                     reads=[yT, self.consts], writes=[ps[6 + t // 4]])
            for b2 in range(2):
                P.op("dve", lambda e, b2=b2: e.tensor_tensor(out=xo.t[:, b2 * 4:(b2 + 1) * 4, :], in0=ps[6 + b2].t[:].rearrange("p (t c) -> p t c", c=128),
                                                              in1=xin.t[:, b2 * 4:(b2 + 1) * 4, :], op=ALU.add), reads=[ps[6 + b2], xin], writes=[xo])
            P.dma("act", xdst.t[t0 - dst_off:t0 - dst_off + G, c0:c0 + 128].rearrange("(t p) c -> p t c", p=128), xo.t[:], xo, reads=[xo], writes=[xbuf] if xbuf else [])

        self.pending_ep = part_b

    def flush_ep(self):
        f = getattr(self, "pending_ep", None)
        if f is not None:
            self.pending_ep = None
            f()

    def phase_oproj(self, l, xsrc, xdst, grps):
        P = self.P
        ps = self.ps
        self.modbufs = [self.modT[l], self.gsc[l]]
        w = self.w_out
        for grp in grps:
            t0 = grp * G
            with P.scope():
                self.yT = [P.sbuf(f"yT{i}", [128, G], F32) for i in range(2)]
                self.xin = [P.sbuf(f"xin{i}", [128, 8, 128], F32) for i in range(2)]
                self.xo = [P.sbuf(f"xo{i}", [128, 8, 128], F32) for i in range(2)]
                P.dma("sp", self.hT.t[:], self.mixT.t[:, t0:t0 + G].rearrange("(j p) t -> p j t", p=128), self.hT, writes=[self.hT])
                for j in range(KC):
                    banks = (ps[2 * (j % 3)], ps[2 * (j % 3) + 1])

                    def wsrc(k0, nkk, j=j):
                        return w.t[l, k0 * 128:(k0 + nkk) * 128, j * 128:(j + 1) * 128].rearrange("(k p) c -> p k c", p=128)
                    self.fm_job(banks, wsrc, KC, 128, lambda kg, hf: self.hT.t[:, kg, hf * 512:(hf + 1) * 512], [self.hT])
                    self.resid_epilogue(banks, self.modT[l].t[:, 2, j:j + 1], xsrc, xdst, t0, j * 128, j, None)
                self.flush_ep()
            P.barrier()

    def phase_mlp(self, l, xsrc, xdst, grps, dst_off=0):
        P = self.P
        ps = self.ps
        self.modbufs = [self.modT[l], self.gsc[l]]
        NE = 4
        CE = DFF // 128 // NE
        for grp in grps:
            t0 = grp * G
            self.norm_group(xsrc, grp, self.gsc[l].t[:, 1, :], self.modT[l].t[:, 3, :])
            with P.scope():
                h1T = P.sbuf("h1T", [128, CE, G], BF16)
                sqs = [P.sbuf(f"fsq{i}", [128, 512], F32) for i in range(2)]
                self.yT = [P.sbuf(f"yT{i}", [128, G], F32) for i in range(2)]
                self.xin = [P.sbuf(f"xin{i}", [128, 8, 128], F32) for i in range(2)]
                self.xo = [P.sbuf(f"xo{i}", [128, 8, 128], F32) for i in range(2)]
                xcols = [Buf(f"xcol{j}") for j in range(KC)]
                ji = 0
                for ep in range(NE):
                    for c in range(CE):
                        banks = (ps[2 * (ji % 3)], ps[2 * (ji % 3) + 1])
                        ff0 = (ep * CE + c) * 128

                        def wsrc(k0, nkk, ff0=ff0):
                            return self.w_mi.t[l, k0 * 128:(k0 + nkk) * 128, ff0:ff0 + 128].rearrange("(k p) c -> p k c", p=128)
                        self.fm_job(banks, wsrc, KC, 128, lambda kg, hf: self.hT.t[:, kg, hf * 512:(hf + 1) * 512], [self.hT])
                        for hf in range(2):
                            sq = sqs[hf]
                            P.op("act", lambda e, sq=sq, hf=hf, banks=banks: e.activation(out=sq.t[:], in_=banks[hf].t[:], func=AF.Square),
                                 reads=[banks[hf]], writes=[sq])
                            P.op("dve", lambda e, sq=sq, hf=hf, banks=banks, c=c: e.scalar_tensor_tensor(
                                out=h1T.t[:, c, hf * 512:(hf + 1) * 512], in0=banks[hf].t[:], scalar=0.0, in1=sq.t[:], op0=ALU.is_gt, op1=ALU.mult),
                                reads=[banks[hf], sq], writes=[h1T])
                        ji += 1
                    for j in range(KC):
                        banks = (ps[2 * (ji % 3)], ps[2 * (ji % 3) + 1])

                        def wsrc2(k0, nkk, j=j, ep=ep):
                            r0 = (ep * CE + k0) * 128
                            return self.w_mo.t[l, r0:r0 + nkk * 128, j * 128:(j + 1) * 128].rearrange("(k p) c -> p k c", p=128)
                        self.fm_job(banks, wsrc2, CE, 128, lambda kg, hf: h1T.t[:, kg, hf * 512:(hf + 1) * 512], [h1T])
                        self.resid_epilogue(banks, self.modT[l].t[:, 5, j:j + 1], xsrc if ep == 0 else xdst, xdst, t0, j * 128, ji, xcols[j],
                                            src_off=(0 if ep == 0 else dst_off), dst_off=dst_off)
                        ji += 1
                self.flush_ep()
            P.barrier()


NCORES = 8


def _make_flags(p, sw):
    fl = np.zeros((128, 257 + 12 * 128), np.float32)
    tt = np.arange(16)[:, None]
    nn = np.arange(8)[None, :]
    gm = np.where(nn >= tt // 2, -1.0e30, 0.0).astype(np.float32)
    sel = (tt >= 8) & (nn < 4)
    fb = np.zeros((16, 8), np.float32)
    if p == 0:
        gm = np.where(sel, -1.0e30, gm).astype(np.float32)
        fb = np.where(sel, -30000.0, 0.0).astype(np.float32)
    fl[:, 0:128] = gm.reshape(1, 128)
    fl[:, 128:256] = fb.reshape(1, 128)
    fl[:, 256] = float(p)
    swp = sw.reshape(128, 12, 2, 128)[:, :, 0, :].copy()
    if p == 0:
        swp = swp - 1.0e10
    fl[:, 257:] = swp.reshape(128, 12 * 128)
    return fl


def _prep(inp, b, p, consts):
    c, m, sw = consts
    d = {}
    xb = inp["x"][b]
    d["x"] = np.ascontiguousarray(xb if p == 1 else np.concatenate([xb[G:], xb[:G]], axis=0))
    d["cT"] = np.ascontiguousarray(inp["c"][b].reshape(32, 128).T)
    for k in ("w_ada", "b_ada", "w_in", "w_out", "w_mlp_in", "w_mlp_out"):
        d[k] = inp[k]
    na = inp["norm_attn"].reshape(DEPTH, 32, 128).transpose(0, 2, 1)
    nm = inp["norm_mlp"].reshape(DEPTH, 32, 128).transpose(0, 2, 1)
    d["normT"] = np.ascontiguousarray(np.concatenate([na, nm], axis=2))
    d["gains"] = np.ascontiguousarray(np.stack([inp["moba_q_norm"], inp["moba_k_norm"], inp["swa_q_norm"], inp["swa_k_norm"],
                                                inp["gla_out_norm"]], axis=2))
    d["w_a_aug"] = np.ascontiguousarray(np.concatenate([inp["gla_w_a"], inp["gla_b_a"][:, None, :]], axis=1))
    d["sinks_rep"] = np.ascontiguousarray(np.broadcast_to(inp["swa_sinks"][:, None, :], (DEPTH, 128, 12)))
    d["consts"] = c
    d["mconsts"] = m
    d["swbias"] = sw
    d["flags"] = _make_flags(p, sw)
    return d


def kernel(**inputs):
    inp = {k: np.asarray(v, dtype=np.float32) for k, v in inputs.items()}
    consts = make_consts()
    K = MK(debug=False, v2=True)
    in_maps = [_prep(inp, i // 2, i % 2, consts) for i in range(NCORES)]
    res = run_bass_kernel_spmd(K.nc, in_maps, core_ids=list(range(NCORES)))
    B = inp["x"].shape[0]
    out = np.empty((B, S, D), np.float32)
    for i in range(NCORES):
        b, p = i // 2, i % 2
        out[b, p * G:(p + 1) * G] = np.asarray(res.results[i]["out"])
    return out
```
